# Optimizing a Trainium2 kernel written in Bass

```python
import math
import jax, jax.numpy as jnp
from jax import lax
import numpy as np

D_MODEL = 1024
BATCH = 16
SEQ = 2048
DEPTH = 2

CHUNK = 64
N_A = DEPTH // 2
N_B = DEPTH - N_A
N_DENSE = (DEPTH + 1) // 2
N_MOE = DEPTH // 2
GMLP_CHUNK = 128
A_FFN = 6 * D_MODEL
A_HALF = A_FFN // 2
A_GROUPS = 8
A_GROUP_DIM = A_HALF // A_GROUPS
B_HEAD_DIM = 64
B_HEADS = D_MODEL // B_HEAD_DIM
Q_BLOCK = 128
D_FF = ((8 * D_MODEL // 3 + 255) // 256) * 256
N_EXPERTS = 8
TOP_K = 2
D_EXPERT = 7 * D_MODEL // 2
RMS_EPS = 1e-6
NEG_INF = -1e30

kernel_name = "yoco_gmlp_fox_moe_trunk"


def rms_norm(x, g):
    xf = x.astype(jnp.float32)
    y = xf * lax.rsqrt(jnp.mean(xf * xf, axis=-1, keepdims=True) + RMS_EPS)
    return (y * g.astype(jnp.float32)).astype(x.dtype)


def swiglu(x, w_in, w_out):
    a, b = jnp.split(x @ w_in, 2, axis=-1)
    return (jax.nn.silu(a) * b) @ w_out


def mixer_a(x, w_in, v_norm_g, w_spatial, b_spatial, w_out):
    B, S, _ = x.shape
    uv = jax.nn.gelu(x @ w_in, approximate=False)
    u, v = jnp.split(uv, 2, axis=-1)
    v = rms_norm(v, v_norm_g)
    v = v.reshape(B, S // GMLP_CHUNK, GMLP_CHUNK, A_GROUPS, A_GROUP_DIM)
    pos = jnp.arange(GMLP_CHUNK)
    mask = (pos[None, :] // CHUNK) <= (pos[:, None] // CHUNK)
    w = jnp.where(mask[None], w_spatial, jnp.zeros_like(w_spatial))
    v = jnp.einsum('gij,bcjgd->bcigd', w, v) + b_spatial.T[None, None, :, :, None]
    return (u * v.reshape(B, S, A_HALF)) @ w_out


def shared_kv(h, norm_g, w_kvf, b_f, k_norm_g):
    B, S, _ = h.shape
    s = rms_norm(h, norm_g)
    kvf = s @ w_kvf
    k = kvf[..., :D_MODEL].reshape(B, S, B_HEADS, B_HEAD_DIM)
    v = kvf[..., D_MODEL:2 * D_MODEL].reshape(B, S, B_HEADS, B_HEAD_DIM)
    f = kvf[..., 2 * D_MODEL:]
    k = rms_norm(k, k_norm_g).transpose(0, 2, 1, 3)
    v = v.transpose(0, 2, 1, 3)
    log_f = jax.nn.log_sigmoid((f + b_f).astype(jnp.float32))
    cum_log_f = jnp.cumsum(log_f, axis=1).transpose(0, 2, 1)
    return k, v, cum_log_f


def mixer_b(x, k, v, cum_log_f, w_in, q_norm_g, w_out):
    B, S, _ = x.shape
    qg = x @ w_in
    q, gate = jnp.split(qg, 2, axis=-1)
    q = rms_norm(q.reshape(B, S, B_HEADS, B_HEAD_DIM), q_norm_g).transpose(0, 2, 1, 3)
    scale = B_HEAD_DIM ** -0.5
    outs = []
    for blk in range(S // Q_BLOCK):
        qs, qe = blk * Q_BLOCK, (blk + 1) * Q_BLOCK
        logits = jnp.einsum('bhqd,bhkd->bhqk', q[:, :, qs:qe], k[:, :, :qe]).astype(jnp.float32) * scale
        logits = logits + cum_log_f[:, :, qs:qe, None] - cum_log_f[:, :, None, :qe]
        causal = jnp.arange(qs, qe)[:, None] >= jnp.arange(qe)[None, :]
        logits = jnp.where(causal[None, None], logits, NEG_INF)
        p = jax.nn.softmax(logits, axis=-1).astype(v.dtype)
        outs.append(jnp.einsum('bhqk,bhkd->bhqd', p, v[:, :, :qe]))
    o = jnp.concatenate(outs, axis=2).transpose(0, 2, 1, 3).reshape(B, S, D_MODEL)
    o = o * jax.nn.sigmoid(gate)
    return o @ w_out


def moe_swiglu(x, w_router, w_in, w_out):
    B, S, D = x.shape
    xt = x.reshape(B * S, D)
    logits = (xt @ w_router).astype(jnp.float32)
    top_v, top_i = lax.top_k(logits, TOP_K)
    top_w = jax.nn.softmax(top_v, axis=-1)
    gates = jnp.sum(jax.nn.one_hot(top_i, N_EXPERTS, dtype=jnp.float32) * top_w[..., None], axis=1)
    y = jnp.zeros_like(xt)
    for e in range(N_EXPERTS):
        y = y + gates[:, e:e + 1].astype(xt.dtype) * swiglu(xt, w_in[e], w_out[e])
    return y.reshape(B, S, D)


def setup_inputs(seed: int = 0) -> dict:
    key = jax.random.key(seed)
    ks = jax.random.split(key, 24)

    def dense(k, shape, fan_in, s=1.0):
        return jax.random.normal(k, shape, jnp.float32) * (s * fan_in ** -0.5)

    def gain(k, shape):
        return 1.0 + 0.02 * jax.random.normal(k, shape, jnp.float32)

    return {
        "x": jax.random.normal(ks[0], (BATCH, SEQ, D_MODEL), jnp.float32),
        "a_norm_g": gain(ks[1], (N_A, D_MODEL)),
        "a_w_in": dense(ks[2], (N_A, D_MODEL, A_FFN), D_MODEL),
        "a_v_norm_g": gain(ks[3], (N_A, A_HALF)),
        "a_w_spatial": dense(ks[4], (N_A, A_GROUPS, GMLP_CHUNK, GMLP_CHUNK), GMLP_CHUNK, 0.5),
        "a_b_spatial": 1.0 + 0.1 * jax.random.normal(ks[5], (N_A, A_GROUPS, GMLP_CHUNK), jnp.float32),
        "a_w_out": dense(ks[6], (N_A, A_HALF, D_MODEL), A_HALF),
        "f_norm_g": gain(ks[7], (N_DENSE, D_MODEL)),
        "f_w_in": dense(ks[8], (N_DENSE, D_MODEL, 2 * D_FF), D_MODEL),
        "f_w_out": dense(ks[9], (N_DENSE, D_FF, D_MODEL), D_FF),
        "kv_norm_g": gain(ks[10], (D_MODEL,)),
        "kv_w": dense(ks[11], (D_MODEL, 2 * D_MODEL + B_HEADS), D_MODEL),
        "kv_b_f": 4.0 + 0.1 * jax.random.normal(ks[12], (B_HEADS,), jnp.float32),
        "k_norm_g": gain(ks[13], (B_HEAD_DIM,)),
        "b_norm_g": gain(ks[14], (N_B, D_MODEL)),
        "b_w_in": dense(ks[15], (N_B, D_MODEL, 2 * D_MODEL), D_MODEL),
        "q_norm_g": gain(ks[16], (N_B, B_HEAD_DIM)),
        "b_w_out": dense(ks[17], (N_B, D_MODEL, D_MODEL), D_MODEL),
        "m_norm_g": gain(ks[18], (N_MOE, D_MODEL)),
        "m_w_router": dense(ks[19], (N_MOE, D_MODEL, N_EXPERTS), D_MODEL),
        "m_w_in": dense(ks[20], (N_MOE, N_EXPERTS, D_MODEL, 2 * D_EXPERT), D_MODEL),
        "m_w_out": dense(ks[21], (N_MOE, N_EXPERTS, D_EXPERT, D_MODEL), D_EXPERT),
    }


def reference(x, a_norm_g, a_w_in, a_v_norm_g, a_w_spatial, a_b_spatial, a_w_out,
              f_norm_g, f_w_in, f_w_out,
              kv_norm_g, kv_w, kv_b_f, k_norm_g,
              b_norm_g, b_w_in, q_norm_g, b_w_out,
              m_norm_g, m_w_router, m_w_in, m_w_out):
    h = x
    k = v = cum_log_f = None
    for layer in range(DEPTH):
        if layer < N_A:
            i = layer
            h = h + mixer_a(rms_norm(h, a_norm_g[i]), a_w_in[i], a_v_norm_g[i],
                            a_w_spatial[i], a_b_spatial[i], a_w_out[i])
        else:
            if layer == N_A:
                k, v, cum_log_f = shared_kv(h, kv_norm_g, kv_w, kv_b_f, k_norm_g)
            i = layer - N_A
            h = h + mixer_b(rms_norm(h, b_norm_g[i]), k, v, cum_log_f,
                            b_w_in[i], q_norm_g[i], b_w_out[i])
        if layer % 2 == 0:
            j = layer // 2
            h = h + swiglu(rms_norm(h, f_norm_g[j]), f_w_in[j], f_w_out[j])
        else:
            j = layer // 2
            h = h + moe_swiglu(rms_norm(h, m_norm_g[j]), m_w_router[j], m_w_in[j], m_w_out[j])
    return h
```

```python
from contextlib import ExitStack

import numpy as np
import concourse.bass as bass
import concourse.mybir as mybir
from concourse.bass_utils import run_bass_kernel_spmd

F32 = mybir.dt.float32
BF16 = mybir.dt.bfloat16
AF = mybir.ActivationFunctionType
ALU = mybir.AluOpType
AX = mybir.AxisListType

ENG = ("pe", "act", "dve", "pool", "sp")
D = 1024
KC = 8
ST = 1024
NTT = ST // 512
NT = ST // 128
SEQ = 2048
NSEQ = 2
A_HALF = 3072
D_FF = 2816
D_EXP = 3584
NEXP = 8
EPS = 1e-6
SM_BOUND = 12.0


class Phase:
    def __init__(self, nc, name):
        self.nc = nc
        self.name = name
        self.es = ExitStack()
        self.ops = {e: [] for e in ENG}
        self.sem = {}
        self.cnt = {}
        self.all_sems = []
        for e in ENG:
            self.sem[e] = nc.alloc_semaphore(name=f"{name}_{e}")
            self.all_sems.append(self.sem[e])
            self.cnt[e] = 0
        self.dsem = {}
        self.waited = {e: {} for e in ENG}

    def alloc(self, name, shape, dt):
        return self.es.enter_context(self.nc.sbuf_tensor(f"{self.name}_{name}", shape, dt))

    def psum(self, name="ps"):
        return self.es.enter_context(self.nc.psum_tensor(f"{self.name}_{name}", [128, 8, 512], F32))

    def _waits(self, eng, waits):
        out = []
        for w in waits:
            if w is None:
                continue
            sem, val = w
            key = sem.num
            if self.waited[eng].get(key, 0) >= val:
                continue
            self.waited[eng][key] = val
            out.append((sem, val))
        return out

    def op(self, eng, fn, waits=(), sig=True):
        ws = self._waits(eng, waits)
        tok = None
        inc = None
        if sig:
            self.cnt[eng] += 1
            tok = (self.sem[eng], self.cnt[eng])
            inc = (self.sem[eng], 1)
        self.ops[eng].append((ws, fn, inc))
        return tok

    def dma(self, eng, fn, key, waits=()):
        if key not in self.dsem:
            s = self.nc.alloc_semaphore(name=f"{self.name}_d{len(self.dsem)}")
            self.all_sems.append(s)
            self.dsem[key] = [s, 0, eng]
        d = self.dsem[key]
        assert d[2] == eng
        d[1] += 16
        ws = self._waits(eng, waits)
        self.ops[eng].append((ws, fn, (d[0], 16)))
        return (d[0], d[1])

    def emit(self):
        nc = self.nc
        for key, (s, v, eng) in self.dsem.items():
            ws = self._waits(eng, [(s, v)])
            if ws:
                self.ops[eng].append((ws, None, None))

        def run(engobj, name):
            for ws, fn, inc in self.ops[name]:
                for sem, val in ws:
                    engobj.wait_ge(sem, val)
                if fn is None:
                    continue
                ins = fn(engobj)
                if inc is not None:
                    ins.then_inc(inc[0], inc[1])

        with nc.Block() as block:
            block.tensor(lambda e: run(e, "pe"))
            block.scalar(lambda e: run(e, "act"))
            block.vector(lambda e: run(e, "dve"))
            block.gpsimd(lambda e: run(e, "pool"))
            block.sync(lambda e: run(e, "sp"))
        nc.all_engine_barrier()
        nc.clear_and_free_semaphores(self.all_sems)
        nc.all_engine_barrier()
        self.es.close()


class Banks:
    def __init__(self, ps, ids):
        self.ps = ps
        self.ids = list(ids)
        self.free = {b: None for b in self.ids}
        self.i = 0

    def next(self):
        b = self.ids[self.i % len(self.ids)]
        self.i += 1
        return b, self.free[b]

    def release(self, b, tok):
        self.free[b] = tok


class WStream:
    def __init__(self, ph, name, shape, nbuf, dt=BF16, eng="pool", bufs=None, free0=None):
        self.ph = ph
        self.name = name
        self.eng = eng
        self.bufs = bufs if bufs is not None else [ph.alloc(f"{name}{i}", shape, dt) for i in range(nbuf)]
        self.free = [free0] * len(self.bufs)
        self.i = 0

    def load(self, fns):
        slot = self.i % len(self.bufs)
        self.i += 1
        buf = self.bufs[slot]
        toks = []
        for j, fn in enumerate(fns):
            toks.append(self.ph.dma(self.eng, (lambda e, fn=fn, buf=buf: fn(e, buf)), f"{self.name}{slot}",
                                    waits=[self.free[slot]]))
        return slot, buf, toks[-1:]

    def release(self, slot, tok):
        self.free[slot] = tok


def bcast_mid(ap2d, rep):
    a = ap2d.ap
    return bass.AP(ap2d.tensor, ap2d.offset, [list(a[0]), [0, rep], list(a[1])])


def bcast_last(ap2d, n):
    a = ap2d.ap
    return bass.AP(ap2d.tensor, ap2d.offset, [list(a[0]), [0, n]])


class Kern:
    def __init__(self, cfg):
        self.cfg = cfg
        self.nc = bass.Bass("TRN2", target_bir_lowering=False)
        self.pid = 0

    def phase(self, name):
        self.pid += 1
        return Phase(self.nc, f"p{self.pid}{name}")

    def declare(self):
        nc = self.nc
        I = {}

        def inp(name, shape):
            I[name] = nc.dram_tensor(name, list(shape), F32, kind="ExternalInput").ap()

        inp("x", (NSEQ, SEQ, D))
        inp("a_norm_g", (1, D)); inp("a_w_in", (1, D, 2 * A_HALF)); inp("a_v_norm_g", (1, A_HALF))
        inp("a_w_spatial", (1, 8, 128, 128)); inp("a_b_spatial", (1, 8, 128)); inp("a_w_out", (1, A_HALF, D))
        inp("f_norm_g", (1, D)); inp("f_w_in", (1, D, 2 * D_FF)); inp("f_w_out", (1, D_FF, D))
        inp("kv_norm_g", (D,)); inp("kv_w", (D, 2 * D + 16)); inp("kv_b_f", (16,)); inp("k_norm_g", (64,))
        inp("b_norm_g", (1, D)); inp("b_w_in", (1, D, 2 * D)); inp("q_norm_g", (1, 64)); inp("b_w_out", (1, D, D))
        inp("m_norm_g", (1, D)); inp("m_w_router", (1, D, NEXP)); inp("m_w_in", (1, NEXP, D, 2 * D_EXP))
        inp("m_w_out", (1, NEXP, D_EXP, D))
        self.I = I
        self.out = nc.dram_tensor("out", [NSEQ, SEQ, D], F32, kind="ExternalOutput").ap()
        if self.cfg.get("dbg"):
            self.dbg = nc.dram_tensor("dbg", [128, KC, ST], F32, kind="ExternalOutput").ap()
        self.KT_d = nc.dram_tensor("kt_scr", [128, KC, SEQ], BF16).ap()
        self.V_d = nc.dram_tensor("v_scr", [128, KC, SEQ // 128, 128], BF16).ap()

    def alloc_persist(self, es):
        nc = self.nc

        def sb(name, shape, dt=F32):
            return es.enter_context(nc.sbuf_tensor(name, shape, dt))

        self.hT = sb("hT", [128, KC, ST])
        self.ident = sb("ident", [128, 128])
        self.ones_bf = sb("ones_bf", [128, 128], BF16)
        self.blk_bf = sb("blk_bf", [128, 128], BF16)
        self.tri_f = sb("tri_f", [128, 128])
        self.ones_f = sb("ones_f", [128, 128])
        self.trimask = sb("trimask", [128, 128], BF16)
        self.swap_f = sb("swap_f", [128, 128])
        self.gains = sb("gains", [128, 5, KC])
        self.vgain = sb("vgain", [128, 24])
        self.kqg = sb("kqg", [128, 2])
        self.biasbc = sb("biasbc", [128, 8, 128])
        self.bf_bc = sb("bf_bc", [128, 16])
        self.wTsp = sb("wTsp", [128, 8, 128])
        self.gwr = sb("gwr", [128, KC, NEXP])
        self.sel8 = sb("sel8", [8, NEXP, 128])
        self.epsc = sb("epsc", [128, 1])
        self.onec = sb("onec", [128, 1])
        self.logf = sb("logf", [128, SEQ // 128, 16])
        self.Fcum = sb("Fcum", [128, SEQ // 128, 16])

    def ph_setup(self):
        I = self.I
        ph = self.phase("setup")
        nc = self.nc
        wsp = ph.alloc("wsp", [128, 8, 128], F32)
        rt = ph.alloc("rt", [128, KC, NEXP], F32)
        ps = ph.psum()
        toks = []
        with nc.allow_non_contiguous_dma(reason="tiny one-time parameter loads"):
            def ld(out, in_, key):
                return ph.dma("sp", lambda e: e.dma_start(out=out, in_=in_, allow_slow_non_contiguous=True), key)
            for i, nm in enumerate(["a_norm_g", "f_norm_g", "kv_norm_g", "b_norm_g", "m_norm_g"]):
                src = I[nm] if nm == "kv_norm_g" else I[nm][0]
                toks.append(ld(self.gains[:, i, :], src.rearrange("(c p) -> p c", p=128), "g"))
            toks.append(ld(self.vgain[:], I["a_v_norm_g"][0].rearrange("(c p) -> p c", p=128), "g"))
            for half in range(2):
                toks.append(ld(self.kqg[half * 64:(half + 1) * 64, 0:1], I["k_norm_g"].rearrange("(p o) -> p o", o=1), "g"))
                toks.append(ld(self.kqg[half * 64:(half + 1) * 64, 1:2], I["q_norm_g"][0].rearrange("(p o) -> p o", o=1), "g"))
            toks.append(ld(self.biasbc[:], bass.AP(I["a_b_spatial"].tensor, 0, [[0, 128], [128, 8], [1, 128]]), "g"))
            toks.append(ld(self.bf_bc[:], bass.AP(I["kv_b_f"].tensor, 0, [[0, 128], [1, 16]]), "g"))
            toks.append(ld(wsp[:], I["a_w_spatial"][0].rearrange("g i j -> i g j"), "g"))
            toks.append(ld(rt[:], I["m_w_router"][0].rearrange("(c p) e -> p c e", p=128), "g"))
        tl = toks[-1]
        pl = []
        def P(fn, waits=()):
            t = ph.op("pool", fn, waits=waits)
            pl.append(t)
            return t
        P(lambda e: e.memset(self.ident[:], 0.0))
        t_id = P(lambda e: e.affine_select(out=self.ident[:], in_=self.ident[:], pattern=[[-1, 128]],
                                           compare_op=ALU.not_equal, fill=1.0, base=0, channel_multiplier=1), waits=[pl[-1]])
        P(lambda e: e.memset(self.ones_bf[:], 1.0))
        P(lambda e: e.memset(self.epsc[:], EPS))
        P(lambda e: e.memset(self.onec[:], 1.0))
        P(lambda e: e.memset(self.ones_f[:], 1.0))
        P(lambda e: e.memset(self.blk_bf[:], 0.0))
        P(lambda e: e.memset(self.blk_bf[0:64, 0:64], 1.0), waits=[pl[-1]])
        P(lambda e: e.memset(self.blk_bf[64:128, 64:128], 1.0), waits=[pl[-2]])
        P(lambda e: e.memset(self.tri_f[:], 1.0))
        P(lambda e: e.affine_select(out=self.tri_f[:], in_=self.tri_f[:], pattern=[[1, 128]],
                                    compare_op=ALU.is_ge, fill=0.0, base=0, channel_multiplier=-1), waits=[pl[-1]])
        t_tri = pl[-1]
        P(lambda e: e.tensor_copy(out=self.trimask[:], in_=self.tri_f[:]), waits=[t_tri])
        P(lambda e: e.memset(self.swap_f[:], 0.0))
        t0 = pl[-1]
        P(lambda e: e.affine_select(out=self.swap_f[:, 0:64], in_=self.swap_f[:, 0:64], pattern=[[-1, 64]],
                                    compare_op=ALU.not_equal, fill=1.0, base=-64, channel_multiplier=1), waits=[t0])
        P(lambda e: e.affine_select(out=self.swap_f[:, 64:128], in_=self.swap_f[:, 64:128], pattern=[[-1, 64]],
                                    compare_op=ALU.not_equal, fill=1.0, base=0, channel_multiplier=1), waits=[t0])
        P(lambda e: e.memset(self.sel8[:], 0.0))
        P(lambda e: e.affine_select(out=self.sel8[:], in_=self.sel8[:], pattern=[[-1, NEXP], [0, 128]],
                                    compare_op=ALU.not_equal, fill=1.0, base=0, channel_multiplier=1), waits=[pl[-1]])
        t_pool = pl[-1]
        t1 = ph.op("dve", lambda e: e.tensor_scalar(out=self.kqg[:, 1:2], in0=self.kqg[:, 1:2], scalar1=0.125, scalar2=None,
                                                    op0=ALU.mult), waits=[tl])
        tg = None
        for c in range(KC):
            tg = ph.op("dve", lambda e, c=c: e.tensor_scalar(out=self.gwr[:, c, :], in0=rt[:, c, :],
                                                             scalar1=self.gains[:, 4, c:c + 1], scalar2=None, op0=ALU.mult),
                       waits=[tl])
        tp = None
        for g in range(8):
            tp = ph.op("pe", lambda e, g=g: e.transpose(ps[:, g // 4, (g % 4) * 128:(g % 4 + 1) * 128], wsp[:, g, :], self.ident[:]),
                       waits=[tl, t_id])
        tc = None
        for b in range(2):
            tc = ph.op("dve", lambda e, b=b: e.tensor_copy(out=self.wTsp[:, b * 4:(b + 1) * 4, :],
                                                           in_=ps[:, b, :].rearrange("p (g i) -> p g i", g=4)), waits=[tp])
        ph.op("dve", lambda e: e.memset(self.wTsp[64:128, :, 0:64], 0.0), waits=[tc])
        ph.emit()

    def ph_load(self, seq, st):
        ph = self.phase("load")
        xt = [ph.alloc(f"xt{i}", [128, D], F32) for i in range(2)]
        ps = ph.psum()
        banks = Banks(ps, range(8))
        xfree = [None, None]
        for t in range(NT):
            sl = t % 2
            r0 = st * ST + t * 128
            tl = ph.dma("sp", lambda e, sl=sl, r0=r0: e.dma_start(out=xt[sl][:], in_=self.I["x"][seq, r0:r0 + 128, :]),
                        f"x{sl}", waits=[xfree[sl]])
            for half in range(2):
                b, bf = banks.next()
                tp = None
                for j in range(4):
                    c = half * 4 + j
                    tp = ph.op("pe", lambda e, b=b, j=j, c=c, sl=sl: e.transpose(ps[:, b, j * 128:(j + 1) * 128],
                                                                                 xt[sl][:, c * 128:(c + 1) * 128], self.ident[:]),
                               waits=[tl, bf], sig=(j == 3))
                eng = "act" if half == 0 else "dve"
                if eng == "act":
                    tcp = ph.op("act", lambda e, b=b, half=half, t=t: e.activation(
                        out=self.hT[:, half * 4:(half + 1) * 4, t * 128:(t + 1) * 128],
                        in_=ps[:, b, :].rearrange("p (c i) -> p c i", c=4), func=AF.Copy), waits=[tp])
                else:
                    tcp = ph.op("dve", lambda e, b=b, half=half, t=t: e.tensor_copy(
                        out=self.hT[:, half * 4:(half + 1) * 4, t * 128:(t + 1) * 128],
                        in_=ps[:, b, :].rearrange("p (c i) -> p c i", c=4)), waits=[tp])
                banks.release(b, tcp)
                if half == 1:
                    xfree[sl] = tp
        ph.emit()

    def ph_store(self, seq, st):
        ph = self.phase("store")
        ot = [ph.alloc(f"ot{i}", [128, D], F32) for i in range(2)]
        ps = ph.psum()
        banks = Banks(ps, range(8))
        ofree = [None, None]
        for t in range(NT):
            sl = t % 2
            r0 = st * ST + t * 128
            cps = []
            for half in range(2):
                b, bf = banks.next()
                tp = None
                for j in range(4):
                    c = half * 4 + j
                    tp = ph.op("pe", lambda e, b=b, j=j, c=c, t=t: e.transpose(ps[:, b, j * 128:(j + 1) * 128],
                                                                               self.hT[:, c, t * 128:(t + 1) * 128], self.ident[:]),
                               waits=[bf], sig=(j == 3))
                if half == 0:
                    tcp = ph.op("act", lambda e, b=b, sl=sl: e.activation(out=ot[sl][:, 0:512], in_=ps[:, b, :], func=AF.Copy),
                                waits=[tp, ofree[sl]])
                else:
                    tcp = ph.op("dve", lambda e, b=b, sl=sl: e.tensor_copy(out=ot[sl][:, 512:1024], in_=ps[:, b, :]),
                                waits=[tp, ofree[sl]])
                banks.release(b, tcp)
                cps.append(tcp)
            ofree[sl] = ph.dma("sp", lambda e, sl=sl, r0=r0: e.dma_start(out=self.out[seq, r0:r0 + 128, :], in_=ot[sl][:]),
                               f"o{sl}", waits=cps)
        ph.emit()

    def emit_norm(self, ph, ps, banks, gidx, xn, sq, rstd_bufs, want_rstd=False):
        toks = []
        rstd_toks = []
        for tt in range(NTT):
            cs = slice(tt * 512, (tt + 1) * 512)
            b, bf = banks.next()
            tm = None
            for c in range(KC):
                eng = "act" if c % 2 == 0 else "dve"
                if eng == "act":
                    tsq = ph.op("act", lambda e, c=c, cs=cs: e.activation(out=sq[:, c, :], in_=self.hT[:, c, cs], func=AF.Square),
                                waits=[self._sq_free.get(c)])
                else:
                    tsq = ph.op("dve", lambda e, c=c, cs=cs: e.tensor_tensor(out=sq[:, c, :], in0=self.hT[:, c, cs], in1=self.hT[:, c, cs],
                                                                            op=ALU.mult), waits=[self._sq_free.get(c)])
                tm = ph.op("pe", lambda e, c=c, b=b: e.matmul(ps[:, b, :], lhsT=self.ones_bf[:], rhs=sq[:, c, :],
                                                              start=(c == 0), stop=(c == KC - 1)), waits=[tsq, bf])
                self._sq_free[c] = tm
            rs = rstd_bufs[tt]
            t1, t2 = self.emit_rsqrt(ph, rs[:], ps[:, b, :], 1.0 / D, [tm, self._rs_free.get(tt)])
            banks.release(b, t1)
            rstd_toks.append(t2)
            last = []
            for c in range(KC):
                tx = ph.op("dve", lambda e, c=c, cs=cs, rs=rs: e.scalar_tensor_tensor(
                    out=xn[:, c, cs], in0=self.hT[:, c, cs], scalar=self.gains[:, gidx, c:c + 1], in1=rs[:],
                    op0=ALU.mult, op1=ALU.mult), waits=[t2, self._xn_free])
                last.append(tx)
            toks.append(last[-1])
            self._rs_free[tt] = last[-1]
        return toks, rstd_toks

    def emit_rsqrt(self, ph, out_ap, in_ap, scale, waits):
        n = out_ap.shape[0]
        t1 = ph.op("act", lambda e: e.activation(out=out_ap, in_=in_ap, func=AF.Ln, bias=self.epsc[0:n, :], scale=scale), waits=waits)
        t2 = ph.op("act", lambda e: e.activation(out=out_ap, in_=out_ap, func=AF.Exp, scale=-0.5), waits=[t1])
        return t1, t2

    def norm_state(self):
        self._sq_free = {}
        self._rs_free = {}
        self._xn_free = None

    def ph_mixer_a(self):
        I = self.I
        ph = self.phase("mixA")
        self.norm_state()
        ps = ph.psum()
        banks = Banks(ps, range(8))
        xn = ph.alloc("xn", [128, KC, ST], BF16)
        rstd = [ph.alloc(f"rstd{i}", [128, 512], F32) for i in range(NTT)]
        big1 = ph.alloc("big1", [128, NT * A_HALF], BF16)
        big2 = ph.alloc("big2", [128, 24 * ST], BF16)
        vg = big1[:].rearrange("p (t n) -> p t n", t=NT)
        pT = big2[:].rearrange("p (m t) -> p m t", m=24)
        sq = big2[:, 0:KC * 512].rearrange("p (c n) -> p c n", c=KC)
        junk = big2[:, 8192:8192 + A_HALF]
        wts = ph.alloc("wts", [128, NT, 8 * 128], BF16)
        ssv = ph.alloc("ssv", [128, NT], F32)
        rsv = ph.alloc("rsv", [128, NT], F32)
        usb = [ph.alloc(f"usb{i}", [128, 512], BF16) for i in range(4)]
        t1b = [ph.alloc(f"t1b{i}", [128, 512], F32) for i in range(2)]
        ws = WStream(ph, "w", [128, KC, 512], 2)
        w_in = I["a_w_in"][0]
        w_out = I["a_w_out"][0]

        xtoks, _ = self.emit_norm(ph, ps, banks, 0, xn, sq, rstd)
        xall = xtoks[-1]

        def load_in(col0):
            return ws.load([lambda e, buf, col0=col0: e.dma_start(
                out=buf[:], in_=w_in[:, col0:col0 + 512].rearrange("(k p) n -> p k n", p=128))])

        pend = load_in(A_HALF)
        for n in range(6):
            slot, wb, wt = pend
            if n + 1 < 6:
                pend = load_in(A_HALF + (n + 1) * 512)
            else:
                pend = load_in(0)
            tm = None
            for t in range(NT):
                b, bf = banks.next()
                for k in range(KC):
                    tm = ph.op("pe", lambda e, b=b, k=k, t=t, wb=wb: e.matmul(
                        ps[:, b, :], lhsT=xn[:, k, t * 128:(t + 1) * 128], rhs=wb[:, k, :], start=(k == 0), stop=(k == KC - 1)),
                        waits=[xall, bf] + wt, sig=(k == KC - 1))
                tg = ph.op("act", lambda e, b=b, t=t, n=n: e.activation(out=vg[:, t, n * 512:(n + 1) * 512], in_=ps[:, b, :],
                                                                        func=AF.Gelu), waits=[tm])
                banks.release(b, tg)
                vg_last = tg
            ws.release(slot, tm)
        tz = ph.op("dve", lambda e: e.memset(ssv[:], 0.0))
        tsq = None
        for t in range(NT):
            tsq = ph.op("act", lambda e, t=t: e.activation(out=junk, in_=vg[:, t, :], func=AF.Square,
                                                           accum_out=ssv[:, t:t + 1]), waits=[vg_last, tz])
        _, tr = self.emit_rsqrt(ph, rsv[:], ssv[:], 1.0 / A_HALF, [tsq])
        tw = None
        for t in range(NT):
            tw = ph.op("dve", lambda e, t=t: e.tensor_scalar(out=wts[:, t, :], in0=self.wTsp[:].rearrange("p g i -> p (g i)"),
                                                             scalar1=rsv[:, t:t + 1], scalar2=None, op0=ALU.mult), waits=[tr])
        ufree = [None] * 4
        t1free = [None] * 2
        ui = 0
        ti = 0
        for piece in range(6):
            slot, wb, wt = pend
            if piece + 1 < 6:
                pend = load_in((piece + 1) * 512)
            tlast = None
            for mm in range(4):
                m = piece * 4 + mm
                g = m // 3
                for tt in range(NTT):
                    cs = slice(tt * 512, (tt + 1) * 512)
                    bu, buf_ = banks.next()
                    tm = None
                    for k in range(KC):
                        tm = ph.op("pe", lambda e, bu=bu, k=k, mm=mm, cs=cs, wb=wb: e.matmul(
                            ps[:, bu, :], lhsT=wb[:, k, mm * 128:(mm + 1) * 128], rhs=xn[:, k, cs],
                            start=(k == 0), stop=(k == KC - 1)), waits=[buf_] + wt, sig=(k == KC - 1))
                    tlast = tm
                    us = ui % 4
                    ui += 1
                    tu = ph.op("act", lambda e, bu=bu, us=us: e.activation(out=usb[us][:], in_=ps[:, bu, :], func=AF.Gelu),
                               waits=[tm, ufree[us]])
                    banks.release(bu, tu)
                    bs, bsf = banks.next()
                    tsm = None
                    for q in range(4):
                        t = tt * 4 + q
                        tsm = ph.op("pe", lambda e, bs=bs, q=q, t=t, m=m, g=g: e.matmul(
                            ps[:, bs, q * 128:(q + 1) * 128], lhsT=vg[:, t, m * 128:(m + 1) * 128],
                            rhs=wts[:, t, g * 128:(g + 1) * 128], start=True, stop=True), waits=[bsf, tw], sig=(q == 3))
                    tlast = tsm
                    t1s = ti % 2
                    ti += 1
                    ta = ph.op("dve", lambda e, bs=bs, m=m, g=g, t1s=t1s: e.scalar_tensor_tensor(
                        out=t1b[t1s][:].rearrange("p (r i) -> p r i", r=4), in0=ps[:, bs, :].rearrange("p (r i) -> p r i", r=4),
                        scalar=self.vgain[:, m:m + 1], in1=bcast_mid(self.biasbc[:, g, :], 4), op0=ALU.mult, op1=ALU.add),
                        waits=[tsm, t1free[t1s]])
                    banks.release(bs, ta)
                    tb = ph.op("dve", lambda e, m=m, cs=cs, t1s=t1s, us=us: e.tensor_tensor(
                        out=pT[:, m, cs], in0=t1b[t1s][:], in1=usb[us][:], op=ALU.mult), waits=[ta, tu])
                    ufree[us] = tb
                    t1free[t1s] = tb
                    p_last = tb
            ws.release(slot, tlast)
        ws2 = WStream(ph, "w2", None, 2, bufs=[big1[:, i * 6144:(i + 1) * 6144].rearrange("p (k n) -> p k n", k=24) for i in range(2)],
                      free0=tlast)

        def load_out(c0):
            return ws2.load([lambda e, buf, c0=c0: e.dma_start(
                out=buf, in_=w_out[:, c0:c0 + 256].rearrange("(k p) n -> p k n", p=128))])
        pend2 = load_out(0)
        for piece in range(4):
            slot, wb, wt = pend2
            if piece + 1 < 4:
                pend2 = load_out((piece + 1) * 256)
            tm = None
            for mm in range(2):
                mo = piece * 2 + mm
                for tt in range(NTT):
                    cs = slice(tt * 512, (tt + 1) * 512)
                    b, bf = banks.next()
                    for k in range(24):
                        tm = ph.op("pe", lambda e, b=b, k=k, mm=mm, cs=cs, wb=wb: e.matmul(
                            ps[:, b, :], lhsT=wb[:, k, mm * 128:(mm + 1) * 128], rhs=pT[:, k, cs],
                            start=(k == 0), stop=(k == 23)), waits=[bf, p_last] + wt, sig=(k == 23))
                    ta = ph.op("dve", lambda e, b=b, mo=mo, cs=cs: e.tensor_tensor(
                        out=self.hT[:, mo, cs], in0=self.hT[:, mo, cs], in1=ps[:, b, :], op=ALU.add), waits=[tm])
                    banks.release(b, ta)
            ws2.release(slot, tm)
        ph.emit()

    def emit_swiglu(self, ph, ps, banks, xn, xall, w_in, w_out, dff, hbuf, ws, ws2, gate_bc=None, gate_tok=None, tmpb=None):
        nj = dff // 128
        npiece = nj // 2

        def load_in(p):
            c0 = p * 256
            return ws.load([
                lambda e, buf, c0=c0: e.dma_start(out=buf[:, :, 0:256], in_=w_in[:, c0:c0 + 256].rearrange("(k p) n -> p k n", p=128)),
                lambda e, buf, c0=c0: e.dma_start(out=buf[:, :, 256:512],
                                                  in_=w_in[:, dff + c0:dff + c0 + 256].rearrange("(k p) n -> p k n", p=128)),
            ])
        pend = load_in(0)
        si = 0
        h_last = None
        for p in range(npiece):
            slot, wb, wt = pend
            if p + 1 < npiece:
                pend = load_in(p + 1)
            tlast = None
            for jj in range(2):
                j = p * 2 + jj
                for tt in range(NTT):
                    cs = slice(tt * 512, (tt + 1) * 512)
                    ba, baf = banks.next()
                    bb, bbf = banks.next()
                    ta_ = tb_ = None
                    for k in range(KC):
                        ta_ = ph.op("pe", lambda e, ba=ba, k=k, jj=jj, cs=cs, wb=wb: e.matmul(
                            ps[:, ba, :], lhsT=wb[:, k, jj * 128:(jj + 1) * 128], rhs=xn[:, k, cs],
                            start=(k == 0), stop=(k == KC - 1)), waits=[baf, xall] + wt, sig=(k == KC - 1))
                    for k in range(KC):
                        tb_ = ph.op("pe", lambda e, bb=bb, k=k, jj=jj, cs=cs, wb=wb: e.matmul(
                            ps[:, bb, :], lhsT=wb[:, k, 256 + jj * 128:256 + (jj + 1) * 128], rhs=xn[:, k, cs],
                            start=(k == 0), stop=(k == KC - 1)), waits=[bbf], sig=(k == KC - 1))
                    tlast = tb_
                    s = si % 2
                    si += 1
                    tsl = ph.op("act", lambda e, ba=ba, s=s: e.activation(out=self._silu[s][:], in_=ps[:, ba, :], func=AF.Silu),
                                waits=[ta_, self._silu_free[s]])
                    banks.release(ba, tsl)
                    th = ph.op("dve", lambda e, bb=bb, s=s, j=j, cs=cs: e.tensor_tensor(
                        out=hbuf[:, j, cs], in0=self._silu[s][:], in1=ps[:, bb, :], op=ALU.mult),
                        waits=[tsl, tb_, self._h_free])
                    banks.release(bb, th)
                    self._silu_free[s] = th
                    h_last = th
            ws.release(slot, tlast)

        def load_out(c0):
            return ws2.load([lambda e, buf, c0=c0: e.dma_start(
                out=buf[:, 0:nj, :], in_=w_out[:, c0:c0 + 256].rearrange("(k p) n -> p k n", p=128))])
        pend2 = load_out(0)
        tm = None
        for piece in range(4):
            slot, wb, wt = pend2
            if piece + 1 < 4:
                pend2 = load_out((piece + 1) * 256)
            for mm in range(2):
                mo = piece * 2 + mm
                for tt in range(NTT):
                    cs = slice(tt * 512, (tt + 1) * 512)
                    b, bf = banks.next()
                    for k in range(nj):
                        tm = ph.op("pe", lambda e, b=b, k=k, mm=mm, cs=cs, wb=wb: e.matmul(
                            ps[:, b, :], lhsT=wb[:, k, mm * 128:(mm + 1) * 128], rhs=hbuf[:, k, cs],
                            start=(k == 0), stop=(k == nj - 1)), waits=[bf, h_last] + wt, sig=(k == nj - 1))
                    if gate_bc is None:
                        ta = ph.op("dve", lambda e, b=b, mo=mo, cs=cs: e.tensor_tensor(
                            out=self.hT[:, mo, cs], in0=self.hT[:, mo, cs], in1=ps[:, b, :], op=ALU.add), waits=[tm])
                        banks.release(b, ta)
                    else:
                        s = self._tmp_i % 2
                        self._tmp_i += 1
                        tq = ph.op("dve", lambda e, b=b, cs=cs, s=s: e.tensor_tensor(
                            out=tmpb[s][:], in0=ps[:, b, :], in1=gate_bc[:, cs], op=ALU.mult),
                            waits=[tm, gate_tok, self._tmp_free[s]])
                        banks.release(b, tq)
                        ta = ph.op("dve", lambda e, mo=mo, cs=cs, s=s: e.tensor_tensor(
                            out=self.hT[:, mo, cs], in0=self.hT[:, mo, cs], in1=tmpb[s][:], op=ALU.add), waits=[tq])
                        self._tmp_free[s] = ta
            ws2.release(slot, tm)
        self._h_free = tm
        return tm

    def ph_ffn(self):
        I = self.I
        ph = self.phase("ffn")
        self.norm_state()
        ps = ph.psum()
        banks = Banks(ps, range(8))
        xn = ph.alloc("xn", [128, KC, ST], BF16)
        sq = ph.alloc("sq", [128, KC, 512], BF16)
        rstd = [ph.alloc(f"rstd{i}", [128, 512], F32) for i in range(NTT)]
        hbuf = ph.alloc("hbuf", [128, D_FF // 128, ST], BF16)
        self._silu = [ph.alloc(f"silu{i}", [128, 512], F32) for i in range(2)]
        self._silu_free = [None, None]
        self._h_free = None
        ws = WStream(ph, "w", [128, KC, 512], 2)
        ws2 = WStream(ph, "w2", [128, D_FF // 128, 256], 2)
        xtoks, _ = self.emit_norm(ph, ps, banks, 1, xn, sq, rstd)
        self.emit_swiglu(ph, ps, banks, xn, xtoks[-1], I["f_w_in"][0], I["f_w_out"][0], D_FF, hbuf, ws, ws2)
        ph.emit()

    def emit_headnorm(self, ph, ps, banks, src_bank, src_tok, gcol, out_ap, raw, sqb, rsb, free_tok):
        t_raw = ph.op("act", lambda e: e.activation(out=raw[:], in_=ps[:, src_bank, :], func=AF.Copy), waits=[src_tok, free_tok])
        t_sq = ph.op("act", lambda e: e.activation(out=sqb[:], in_=ps[:, src_bank, :], func=AF.Square), waits=[src_tok, free_tok])
        banks.release(src_bank, t_sq)
        b, bf = banks.next()
        tm = ph.op("pe", lambda e: e.matmul(ps[:, b, :], lhsT=self.blk_bf[:], rhs=sqb[:], start=True, stop=True), waits=[t_sq, bf])
        t1, t2 = self.emit_rsqrt(ph, rsb[:], ps[:, b, :], 1.0 / 64, [tm, free_tok])
        banks.release(b, t1)
        t3 = ph.op("dve", lambda e: e.scalar_tensor_tensor(out=out_ap, in0=raw[:], scalar=self.kqg[:, gcol:gcol + 1], in1=rsb[:],
                                                           op0=ALU.mult, op1=ALU.mult), waits=[t2, t_raw])
        return t3

    def ph_kv(self, st):
        I = self.I
        ph = self.phase("kv")
        self.norm_state()
        ps = ph.psum()
        banks = Banks(ps, range(8))
        xn = ph.alloc("xn", [128, KC, ST], BF16)
        sq = ph.alloc("sq", [128, KC, 512], BF16)
        rstd = [ph.alloc(f"rstd{i}", [128, 512], F32) for i in range(NTT)]
        KT = ph.alloc("KT", [128, KC, ST], BF16)
        Vs = ph.alloc("Vs", [128, NT, D], BF16)
        wf = ph.alloc("wf", [128, KC, 16], BF16)
        raws = [ph.alloc(f"raw{i}", [128, 512], F32) for i in range(2)]
        sqbs = [ph.alloc(f"sqb{i}", [128, 512], BF16) for i in range(2)]
        rsbs = [ph.alloc(f"rsb{i}", [128, 512], F32) for i in range(2)]
        ef = ph.alloc("ef", [128, NT, 16], F32)
        ws = WStream(ph, "w", [128, KC, 512], 2)
        kv_w = I["kv_w"]
        xtoks, _ = self.emit_norm(ph, ps, banks, 2, xn, sq, rstd)
        xall = xtoks[-1]
        tf_w = ph.dma("pool", lambda e: e.dma_start(out=wf[:], in_=kv_w[:, 2 * D:2 * D + 16].rearrange("(k p) n -> p k n", p=128)), "wf")

        def load(col0):
            return ws.load([lambda e, buf, col0=col0: e.dma_start(
                out=buf[:], in_=kv_w[:, col0:col0 + 512].rearrange("(k p) n -> p k n", p=128))])
        pend = load(0)
        hfree = [None, None]
        hi = 0
        k_last = None
        for piece in range(2):
            slot, wb, wt = pend
            pend = load((piece + 1) * 512) if piece == 0 else load(D)
            tm = None
            for mm in range(4):
                m = piece * 4 + mm
                for tt in range(NTT):
                    cs = slice(tt * 512, (tt + 1) * 512)
                    b, bf = banks.next()
                    for k in range(KC):
                        tm = ph.op("pe", lambda e, b=b, k=k, mm=mm, cs=cs, wb=wb: e.matmul(
                            ps[:, b, :], lhsT=wb[:, k, mm * 128:(mm + 1) * 128], rhs=xn[:, k, cs],
                            start=(k == 0), stop=(k == KC - 1)), waits=[bf, xall] + wt, sig=(k == KC - 1))
                    s = hi % 2
                    hi += 1
                    k_last = self.emit_headnorm(ph, ps, banks, b, tm, 0, KT[:, m, cs], raws[s], sqbs[s], rsbs[s], hfree[s])
                    hfree[s] = k_last
            ws.release(slot, tm)
        tk_st = ph.dma("sp", lambda e: e.dma_start(out=self.KT_d[:, :, st * ST:(st + 1) * ST], in_=KT[:]), "kst", waits=[k_last])
        v_last = None
        for n in range(2):
            slot, wb, wt = pend
            if n == 0:
                pend = load(D + 512)
            tm = None
            for t in range(NT):
                b, bf = banks.next()
                for k in range(KC):
                    tm = ph.op("pe", lambda e, b=b, k=k, t=t, wb=wb: e.matmul(
                        ps[:, b, :], lhsT=xn[:, k, t * 128:(t + 1) * 128], rhs=wb[:, k, :], start=(k == 0), stop=(k == KC - 1)),
                        waits=[bf, xall] + wt, sig=(k == KC - 1))
                if t % 2 == 0:
                    v_last = ph.op("act", lambda e, b=b, t=t, n=n: e.activation(out=Vs[:, t, n * 512:(n + 1) * 512], in_=ps[:, b, :],
                                                                                func=AF.Copy), waits=[tm])
                else:
                    v_last = ph.op("dve", lambda e, b=b, t=t, n=n: e.tensor_copy(out=Vs[:, t, n * 512:(n + 1) * 512], in_=ps[:, b, :]),
                                   waits=[tm])
                banks.release(b, v_last)
                if t == NT - 2:
                    v_prev = v_last
            ws.release(slot, tm)
        for c in range(KC):
            ph.dma("sp", lambda e, c=c: e.dma_start(out=self.V_d[:, c, st * NT:(st + 1) * NT, :], in_=Vs[:, :, c * 128:(c + 1) * 128]),
                   "vst", waits=[v_last, v_prev])
        b, bf = banks.next()
        tm = None
        for t in range(NT):
            for k in range(KC):
                tm = ph.op("pe", lambda e, b=b, k=k, t=t: e.matmul(
                    ps[:, b, t * 16:(t + 1) * 16], lhsT=xn[:, k, t * 128:(t + 1) * 128], rhs=wf[:, k, :],
                    start=(k == 0), stop=(k == KC - 1)), waits=[bf, xall, tf_w], sig=(k == KC - 1 and t == NT - 1))
        g0 = st * NT
        t1 = ph.op("dve", lambda e: e.tensor_tensor(out=ef[:], in0=ps[:, b, 0:NT * 16].rearrange("p (t h) -> p t h", t=NT),
                                                    in1=bcast_mid(self.bf_bc[:], NT), op=ALU.add), waits=[tm])
        banks.release(b, t1)
        t2 = ph.op("act", lambda e: e.activation(out=ef[:], in_=ef[:], func=AF.Exp, scale=-1.0), waits=[t1])
        t3 = ph.op("act", lambda e: e.activation(out=ef[:], in_=ef[:], func=AF.Ln, bias=self.onec[:], scale=1.0), waits=[t2])
        t4 = ph.op("dve", lambda e: e.tensor_scalar(out=self.logf[:, g0:g0 + NT, :], in0=ef[:], scalar1=-1.0, scalar2=None, op0=ALU.mult),
                   waits=[t3])
        b2, b2f = banks.next()
        tm = None
        for t in range(NT):
            T = g0 + t
            for tp in range(T + 1):
                tm = ph.op("pe", lambda e, b2=b2, t=t, tp=tp, T=T: e.matmul(
                    ps[:, b2, t * 16:(t + 1) * 16], lhsT=(self.tri_f[:] if tp == T else self.ones_f[:]), rhs=self.logf[:, tp, :],
                    start=(tp == 0), stop=(tp == T)), waits=[b2f, t4], sig=(tp == T and t == NT - 1))
        t5 = ph.op("dve", lambda e: e.tensor_copy(out=self.Fcum[:, g0:g0 + NT, :],
                                                  in_=ps[:, b2, 0:NT * 16].rearrange("p (t h) -> p t h", t=NT)), waits=[tm])
        banks.release(b2, t5)
        ph.emit()

    def ph_mixer_b(self, st):
        I = self.I
        ph = self.phase("mixB")
        self.norm_state()
        ps = ph.psum()
        banks = Banks(ps, range(6))
        xn = ph.alloc("xn", [128, KC, ST], BF16)
        rstd = [ph.alloc(f"rstd{i}", [128, 512], F32) for i in range(NTT)]
        QT = ph.alloc("QT", [128, KC, ST], BF16)
        SG = ph.alloc("SG", [128, KC, ST], BF16)
        OT = ph.alloc("OT", [128, KC, ST], BF16)
        sq = OT[:, 0:4, :].rearrange("p a (b n) -> p (a b) n", b=2)
        Vraw = [ph.alloc(f"Vraw{i}", [128, SEQ // 128, 128], BF16) for i in range(2)]
        raws = [ph.alloc(f"raw{i}", [128, 512], F32) for i in range(2)]
        sqbs = [ph.alloc(f"sqb{i}", [128, 512], BF16) for i in range(2)]
        rsbs = [ph.alloc(f"rsb{i}", [128, 512], F32) for i in range(2)]
        ntk = (st + 1) * NT
        L = ntk * 128
        Kb = [ph.alloc(f"Kb{i}", [128, SEQ], BF16) for i in range(2)]
        Vb = [ph.alloc(f"Vb{i}", [128, SEQ // 128, 2, 128], BF16) for i in range(2)]
        nb = ph.alloc("nb", [128, NTT, SEQ // 128, 16], F32)
        cq = ph.alloc("cq", [128, NTT, 16], F32)
        PT = [ph.alloc(f"PT{i}", [128, 512], BF16) for i in range(3)]
        Rt = [ph.alloc(f"Rt{i}", [128, 512], F32) for i in range(2)]
        Rs = [ph.alloc(f"Rs{i}", [128, 512], F32) for i in range(2)]
        ws = WStream(ph, "w", [128, KC, 512], 2)
        wo = ph.alloc("wo", [128, KC, D], BF16)
        w_in = I["b_w_in"][0]
        xtoks, _ = self.emit_norm(ph, ps, banks, 3, xn, sq, rstd)
        xall = xtoks[-1]
        t_wo = ph.dma("pool", lambda e: e.dma_start(out=wo[:], in_=I["b_w_out"][0].rearrange("(k p) n -> p k n", p=128)), "wo")
        tvo = None
        for i in range(2):
            ph.op("dve", lambda e, i=i: e.memset(Vb[i][:, :, 0, 64:128], 1.0), sig=False)
            tvo = ph.op("dve", lambda e, i=i: e.memset(Vb[i][:, :, 1, 0:64], 1.0))

        def load(col0):
            return ws.load([lambda e, buf, col0=col0: e.dma_start(
                out=buf[:], in_=w_in[:, col0:col0 + 512].rearrange("(k p) n -> p k n", p=128))])
        pend = load(0)
        hfree = [None, None]
        hi = 0
        q_last = None
        g_last = None
        for piece in range(4):
            slot, wb, wt = pend
            if piece + 1 < 4:
                pend = load((piece + 1) * 512)
            tm = None
            for mm in range(4):
                m = (piece % 2) * 4 + mm
                for tt in range(NTT):
                    cs = slice(tt * 512, (tt + 1) * 512)
                    b, bf = banks.next()
                    for k in range(KC):
                        tm = ph.op("pe", lambda e, b=b, k=k, mm=mm, cs=cs, wb=wb: e.matmul(
                            ps[:, b, :], lhsT=wb[:, k, mm * 128:(mm + 1) * 128], rhs=xn[:, k, cs],
                            start=(k == 0), stop=(k == KC - 1)), waits=[bf, xall] + wt, sig=(k == KC - 1))
                    if piece < 2:
                        s = hi % 2
                        hi += 1
                        q_last = self.emit_headnorm(ph, ps, banks, b, tm, 1, QT[:, m, cs], raws[s], sqbs[s], rsbs[s], hfree[s])
                        hfree[s] = q_last
                    else:
                        g_last = ph.op("act", lambda e, b=b, m=m, cs=cs: e.activation(out=SG[:, m, cs], in_=ps[:, b, :], func=AF.Sigmoid),
                                       waits=[tm])
                        banks.release(b, g_last)
            ws.release(slot, tm)
        bq, bqf = banks.next()
        tm = None
        for Q in range(NTT):
            Qa = st * NTT + Q
            npre = Qa * 4
            if npre == 0:
                continue
            for tp in range(npre):
                tm = ph.op("pe", lambda e, Q=Q, tp=tp, npre=npre: e.matmul(
                    ps[:, bq, Q * 16:(Q + 1) * 16], lhsT=self.ones_f[:], rhs=self.logf[:, tp, :], start=(tp == 0), stop=(tp == npre - 1)),
                    waits=[bqf])
        tcq = None
        for Q in range(NTT):
            Qa = st * NTT + Q
            if Qa == 0:
                tcq = ph.op("dve", lambda e, Q=Q: e.memset(cq[:, Q, :], 0.0))
            else:
                tcq = ph.op("dve", lambda e, Q=Q: e.tensor_copy(out=cq[:, Q, :], in_=ps[:, bq, Q * 16:(Q + 1) * 16]), waits=[tm])
        banks.release(bq, tcq)
        tnb = None
        for Q in range(NTT):
            tnb0 = ph.op("dve", lambda e, Q=Q: e.tensor_scalar(out=nb[:, Q, 0:ntk, :], in0=self.Fcum[:, 0:ntk, :], scalar1=-1.0,
                                                               scalar2=-SM_BOUND, op0=ALU.mult, op1=ALU.add), waits=[tcq])
            if st * NTT + Q == 0:
                tnb = tnb0
                continue
            tnb = ph.op("dve", lambda e, Q=Q: e.tensor_tensor(out=nb[:, Q, 0:ntk, :], in0=nb[:, Q, 0:ntk, :],
                                                              in1=bcast_mid(cq[:, Q, :], ntk), op=ALU.add), waits=[tnb0])
        kvfree = [None, None]
        pfree = [None] * 3
        rfree = [None, None]
        pi = 0
        o_last = None
        for c in range(KC):
            sl = c % 2
            tk = ph.dma("sp", lambda e, c=c, sl=sl: e.dma_start(out=Kb[sl][:, 0:L], in_=self.KT_d[:, c, 0:L]), f"kb{sl}",
                        waits=[kvfree[sl]])
            tvr = ph.dma("sp", lambda e, c=c, sl=sl: e.dma_start(out=Vraw[sl][:, 0:ntk, :], in_=self.V_d[:, c, 0:ntk, :]), f"vb{sl}",
                         waits=[kvfree[sl]])
            tv = None
            for hh in range(2):
                off = 0 if hh == 0 else 64
                tv = ph.op("pool", lambda e, sl=sl, hh=hh, off=off: e.tensor_copy(
                    out=Vb[sl][:, 0:ntk, hh, off:off + 64], in_=Vraw[sl][:, 0:ntk, off:off + 64]), waits=[tvr, tvo, kvfree[sl]])
            for Q in range(NTT):
                Qa = st * NTT + Q
                nk = (Qa + 1) * 4
                cs0 = Q * 512
                acc_tok = []
                for hh in range(2):
                    h = c * 2 + hh
                    pb = 6 + hh
                    rows = slice(hh * 64, (hh + 1) * 64)
                    tpv = None
                    for t in range(nk):
                        r = t - Qa * 4
                        c0 = max(r, 0) * 128
                        b, bf = banks.next()
                        tqk = ph.op("pe", lambda e, b=b, t=t, c0=c0, rows=rows, sl=sl, c=c, cs0=cs0: e.matmul(
                            ps[:, b, c0:512], lhsT=Kb[sl][rows, t * 128:(t + 1) * 128], rhs=QT[rows, c, cs0 + c0:cs0 + 512],
                            start=True, stop=True), waits=[bf, tk, q_last])
                        s = pi % 3
                        pi += 1
                        tex = ph.op("act", lambda e, b=b, t=t, c0=c0, s=s, Q=Q, h=h: e.activation(
                            out=PT[s][:, c0:512], in_=ps[:, b, c0:512], func=AF.Exp, bias=nb[:, Q, t, h:h + 1], scale=1.0),
                            waits=[tqk, pfree[s], tnb])
                        banks.release(b, tex)
                        tready = tex
                        if r >= 0:
                            tready = ph.op("dve", lambda e, s=s, c0=c0: e.tensor_tensor(
                                out=PT[s][:, c0:c0 + 128], in0=PT[s][:, c0:c0 + 128], in1=self.trimask[:], op=ALU.mult), waits=[tex])
                        tpv = ph.op("pe", lambda e, pb=pb, t=t, c0=c0, s=s, sl=sl, hh=hh, nk=nk: e.matmul(
                            ps[:, pb, c0:512], lhsT=Vb[sl][:, t, hh, :], rhs=PT[s][:, c0:512], start=(t == 0), stop=(t == nk - 1)),
                            waits=[tready, tv, rfree[hh]])
                        pfree[s] = tpv
                    acc_tok.append(tpv)
                rs_ = Q % 2
                ta = ph.op("dve", lambda e, rs_=rs_: e.reciprocal(out=Rt[rs_][0:64, :], in_=ps[0:64, 7, :]), waits=[acc_tok[1], self._rt_free[rs_]])
                tb = ph.op("dve", lambda e, rs_=rs_: e.reciprocal(out=Rt[rs_][64:128, :], in_=ps[64:128, 6, :]), waits=[acc_tok[0], self._rt_free[rs_]])
                b, bf = banks.next()
                tsw = ph.op("pe", lambda e, b=b, rs_=rs_: e.matmul(ps[:, b, :], lhsT=self.swap_f[:], rhs=Rt[rs_][:], start=True, stop=True),
                            waits=[ta, tb, bf])
                self._rt_free[rs_] = tsw
                tcp = ph.op("act", lambda e, b=b, rs_=rs_: e.activation(out=Rs[rs_][:], in_=ps[:, b, :], func=AF.Copy),
                            waits=[tsw, self._rs2_free[rs_]])
                banks.release(b, tcp)
                tg = ph.op("dve", lambda e, rs_=rs_, c=c, cs0=cs0: e.tensor_tensor(out=Rs[rs_][:], in0=Rs[rs_][:], in1=SG[:, c, cs0:cs0 + 512],
                                                                                  op=ALU.mult), waits=[tcp, g_last])
                to0 = ph.op("dve", lambda e, rs_=rs_, c=c, cs0=cs0: e.tensor_tensor(out=OT[0:64, c, cs0:cs0 + 512], in0=ps[0:64, 6, :],
                                                                                   in1=Rs[rs_][0:64, :], op=ALU.mult), waits=[tg])
                to1 = ph.op("dve", lambda e, rs_=rs_, c=c, cs0=cs0: e.tensor_tensor(out=OT[64:128, c, cs0:cs0 + 512], in0=ps[64:128, 7, :],
                                                                                   in1=Rs[rs_][64:128, :], op=ALU.mult), waits=[tg])
                rfree[0] = to0
                rfree[1] = to1
                self._rs2_free[rs_] = to1
                o_last = to1
            kvfree[sl] = acc_tok[1]
        if self.cfg.get("dbg") and st == 0:
            tdd = None
            for c in range(KC):
                for tt in range(NTT):
                    cs = slice(tt * 512, (tt + 1) * 512)
                    td = ph.op("act", lambda e, c=c, cs=cs: e.activation(out=raws[0][:], in_={'OT': OT, 'QT': QT, 'SG': SG, 'XN': xn}[self.cfg['dbg']][:, c, cs], func=AF.Copy), waits=[o_last, tdd])
                    tdd = ph.dma("sp", lambda e, c=c, cs=cs: e.dma_start(out=self.dbg[:, c, cs], in_=raws[0][:]), "dbg", waits=[td])
        tm = None
        for mo in range(KC):
            for tt in range(NTT):
                cs = slice(tt * 512, (tt + 1) * 512)
                b, bf = banks.next()
                for k in range(KC):
                    tm = ph.op("pe", lambda e, b=b, k=k, mo=mo, cs=cs: e.matmul(
                        ps[:, b, :], lhsT=wo[:, k, mo * 128:(mo + 1) * 128], rhs=OT[:, k, cs], start=(k == 0), stop=(k == KC - 1)),
                        waits=[bf, o_last, t_wo], sig=(k == KC - 1))
                ta = ph.op("dve", lambda e, b=b, mo=mo, cs=cs: e.tensor_tensor(
                    out=self.hT[:, mo, cs], in0=self.hT[:, mo, cs], in1=ps[:, b, :], op=ALU.add), waits=[tm])
                banks.release(b, ta)
        ph.emit()

    def ph_moe(self):
        I = self.I
        ph = self.phase("moe")
        self.norm_state()
        ps = ph.psum()
        banks = Banks(ps, range(8))
        xn = ph.alloc("xn", [128, KC, ST], BF16)
        sq = ph.alloc("sq", [128, KC, 512], BF16)
        rstd = [ph.alloc(f"rstd{i}", [128, 512], F32) for i in range(NTT)]
        hbuf = ph.alloc("hbuf", [128, D_EXP // 128, ST], BF16)
        self._silu = [ph.alloc(f"silu{i}", [128, 512], F32) for i in range(2)]
        self._silu_free = [None, None]
        self._h_free = None
        tmpb = [ph.alloc(f"tmpb{i}", [128, 512], F32) for i in range(2)]
        self._tmp_free = [None, None]
        self._tmp_i = 0
        lgT = ph.alloc("lgT", [8, ST], F32)
        lg = ph.alloc("lg", [128, NT, NEXP], F32)
        m1 = ph.alloc("m1", [128, NT], F32)
        m2 = ph.alloc("m2", [128, NT], F32)
        mk1 = ph.alloc("mk1", [128, NT, NEXP], F32)
        mk2 = ph.alloc("mk2", [128, NT, NEXP], F32)
        lg2 = ph.alloc("lg2", [128, NT, NEXP], F32)
        g1 = ph.alloc("g1", [128, NT], F32)
        g2 = ph.alloc("g2", [128, NT], F32)
        gates = ph.alloc("gates", [128, NT, NEXP], F32)
        gT = ph.alloc("gT", [8, ST], F32)
        gbc = [ph.alloc(f"gbc{i}", [128, ST], F32) for i in range(2)]
        ws = WStream(ph, "w", [128, KC, 512], 2)
        ws2 = WStream(ph, "w2", [128, D_EXP // 128, 256], 2)
        xtoks, rtoks = self.emit_norm(ph, ps, banks, 4, xn, sq, rstd)
        xall = xtoks[-1]
        tl = None
        for tt in range(NTT):
            cs = slice(tt * 512, (tt + 1) * 512)
            b, bf = banks.next()
            tm = None
            for k in range(KC):
                tm = ph.op("pe", lambda e, b=b, k=k, cs=cs: e.matmul(ps[0:8, b, :], lhsT=self.gwr[:, k, :], rhs=self.hT[:, k, cs],
                                                                     start=(k == 0), stop=(k == KC - 1)), waits=[bf], sig=(k == KC - 1))
            tl = ph.op("dve", lambda e, b=b, cs=cs, tt=tt: e.tensor_tensor(out=lgT[:, cs], in0=ps[0:8, b, :], in1=rstd[tt][0:8, :],
                                                                           op=ALU.mult), waits=[tm, rtoks[tt]])
            banks.release(b, tl)
        b, bf = banks.next()
        tp = None
        for t in range(NT):
            tp = ph.op("pe", lambda e, b=b, t=t: e.transpose(ps[:, b, t * 8:(t + 1) * 8], lgT[:, t * 128:(t + 1) * 128], self.ident[0:8, 0:8]),
                       waits=[bf, tl], sig=(t == NT - 1))
        t0 = ph.op("dve", lambda e, b=b: e.tensor_copy(out=lg[:], in_=ps[:, b, 0:NT * 8].rearrange("p (t x) -> p t x", t=NT)), waits=[tp])
        banks.release(b, t0)
        t1 = ph.op("dve", lambda e: e.tensor_reduce(out=m1[:], in_=lg[:], axis=AX.X, op=ALU.max), waits=[t0])
        tk1 = None
        for t in range(NT):
            tk1 = ph.op("dve", lambda e, t=t: e.tensor_scalar(out=mk1[:, t, :], in0=lg[:, t, :], scalar1=m1[:, t:t + 1], scalar2=None,
                                                              op0=ALU.is_equal), waits=[t1])
        t2 = ph.op("dve", lambda e: e.scalar_tensor_tensor(out=lg2[:], in0=mk1[:], scalar=-1e30, in1=lg[:], op0=ALU.mult, op1=ALU.add),
                   waits=[tk1])
        t3 = ph.op("dve", lambda e: e.tensor_reduce(out=m2[:], in_=lg2[:], axis=AX.X, op=ALU.max), waits=[t2])
        tk2 = None
        for t in range(NT):
            tk2 = ph.op("dve", lambda e, t=t: e.tensor_scalar(out=mk2[:, t, :], in0=lg2[:, t, :], scalar1=m2[:, t:t + 1], scalar2=None,
                                                              op0=ALU.is_equal), waits=[t3])
        t4 = ph.op("dve", lambda e: e.tensor_tensor(out=g1[:], in0=m1[:], in1=m2[:], op=ALU.subtract), waits=[t3])
        t5 = ph.op("act", lambda e: e.activation(out=g1[:], in_=g1[:], func=AF.Sigmoid), waits=[t4])
        t6 = ph.op("dve", lambda e: e.tensor_scalar(out=g2[:], in0=g1[:], scalar1=-1.0, scalar2=1.0, op0=ALU.mult, op1=ALU.add), waits=[t5])
        tg = None
        for t in range(NT):
            ta = ph.op("dve", lambda e, t=t: e.tensor_scalar(out=mk1[:, t, :], in0=mk1[:, t, :], scalar1=g1[:, t:t + 1], scalar2=None,
                                                             op0=ALU.mult), waits=[t5, tk2])
            tb = ph.op("dve", lambda e, t=t: e.scalar_tensor_tensor(out=gates[:, t, :], in0=mk2[:, t, :], scalar=g2[:, t:t + 1],
                                                                    in1=mk1[:, t, :], op0=ALU.mult, op1=ALU.add), waits=[ta, t6])
            tg = tb
        tgt = None
        for half in range(2):
            b, bf = banks.next()
            tp = None
            for q in range(4):
                t = half * 4 + q
                tp = ph.op("pe", lambda e, b=b, q=q, t=t: e.transpose(ps[0:8, b, q * 128:(q + 1) * 128], gates[:, t, :], self.ident[:]),
                           waits=[bf, tg], sig=(q == 3))
            tgt = ph.op("dve", lambda e, b=b, half=half: e.tensor_copy(out=gT[:, half * 512:(half + 1) * 512], in_=ps[0:8, b, :]), waits=[tp])
            banks.release(b, tgt)
        gfree = [None, None]
        for ex in range(NEXP):
            gs = ex % 2
            tgb = None
            for tt in range(NTT):
                cs = slice(tt * 512, (tt + 1) * 512)
                b, bf = banks.next()
                tm = ph.op("pe", lambda e, b=b, ex=ex, cs=cs: e.matmul(ps[:, b, :], lhsT=self.sel8[:, ex, :], rhs=gT[:, cs], start=True, stop=True),
                           waits=[bf, tgt])
                tgb = ph.op("act", lambda e, b=b, gs=gs, cs=cs: e.activation(out=gbc[gs][:, cs], in_=ps[:, b, :], func=AF.Copy),
                            waits=[tm, gfree[gs]])
                banks.release(b, tgb)
            last = self.emit_swiglu(ph, ps, banks, xn, xall, I["m_w_in"][0, ex], I["m_w_out"][0, ex], D_EXP, hbuf, ws, ws2,
                                    gate_bc=gbc[gs], gate_tok=tgb, tmpb=tmpb)
            gfree[gs] = self._tmp_free[(self._tmp_i - 1) % 2]
        ph.emit()

    def build(self):
        cfg = self.cfg
        self.declare()
        with ExitStack() as es:
            self.alloc_persist(es)
            self._rt_free = [None, None]
            self._rs2_free = [None, None]
            self.ph_setup()
            for seq in range(cfg.get("nseq", NSEQ)):
                for st in range(cfg.get("nst", SEQ // ST)):
                    self._rt_free = [None, None]
                    self._rs2_free = [None, None]
                    self.ph_load(seq, st)
                    if cfg.get("mixa", True):
                        self.ph_mixer_a()
                    if cfg.get("ffn", True):
                        self.ph_ffn()
                    if cfg.get("kv", True):
                        self.ph_kv(st)
                    if cfg.get("mixb", True):
                        self._rt_free = [None, None]
                        self._rs2_free = [None, None]
                        self.ph_mixer_b(st)
                    if cfg.get("moe", True):
                        self.ph_moe()
                    self.ph_store(seq, st)
        return self.nc


INPUT_NAMES = ["x", "a_norm_g", "a_w_in", "a_v_norm_g", "a_w_spatial", "a_b_spatial", "a_w_out",
               "f_norm_g", "f_w_in", "f_w_out", "kv_norm_g", "kv_w", "kv_b_f", "k_norm_g",
               "b_norm_g", "b_w_in", "q_norm_g", "b_w_out", "m_norm_g", "m_w_router", "m_w_in", "m_w_out"]


def kernel(**inputs):
    n = 8
    k = Kern({})
    nc = k.build()
    shared = {nm: np.ascontiguousarray(np.asarray(inputs[nm], dtype=np.float32)) for nm in INPUT_NAMES if nm != "x"}
    x = np.asarray(inputs["x"], dtype=np.float32)
    in_maps = []
    for i in range(n):
        m = dict(shared)
        m["x"] = np.ascontiguousarray(x[i * NSEQ:(i + 1) * NSEQ])
        in_maps.append(m)
    res = run_bass_kernel_spmd(nc, in_maps, core_ids=list(range(n)))
    return np.concatenate([r["out"] for r in res.results], axis=0)
```

```python
from contextlib import ExitStack

import numpy as np
import concourse.bass as bass
import concourse.mybir as mybir
from concourse.bass_utils import run_bass_kernel_spmd

F32 = mybir.dt.float32
BF16 = mybir.dt.bfloat16
AF = mybir.ActivationFunctionType
ALU = mybir.AluOpType
AX = mybir.AxisListType

ENG = ("pe", "act", "dve", "pool", "sp")
D = 1024
KC = 8
ST = 1024
NTT = ST // 512
NT = ST // 128
SEQ = 2048
NSEQ = 2
A_HALF = 3072
D_FF = 2816
D_EXP = 3584
NEXP = 8
EPS = 1e-6
SM_BOUND = 12.0


class Phase:
    def __init__(self, nc, name):
        self.nc = nc
        self.name = name
        self.es = ExitStack()
        self.ops = {e: [] for e in ENG}
        self.sem = {}
        self.cnt = {}
        self.all_sems = []
        for e in ENG:
            self.sem[e] = nc.alloc_semaphore(name=f"{name}_{e}")
            self.all_sems.append(self.sem[e])
            self.cnt[e] = 0
        self.dsem = {}
        self.waited = {e: {} for e in ENG}

    def alloc(self, name, shape, dt):
        return self.es.enter_context(self.nc.sbuf_tensor(f"{self.name}_{name}", shape, dt))

    def psum(self, name="ps"):
        return self.es.enter_context(self.nc.psum_tensor(f"{self.name}_{name}", [128, 8, 512], F32))

    def _waits(self, eng, waits):
        out = []
        for w in waits:
            if w is None:
                continue
            sem, val = w
            key = sem.num
            if self.waited[eng].get(key, 0) >= val:
                continue
            self.waited[eng][key] = val
            out.append((sem, val))
        return out

    def op(self, eng, fn, waits=(), sig=True):
        ws = self._waits(eng, waits)
        tok = None
        inc = None
        if sig:
            self.cnt[eng] += 1
            tok = (self.sem[eng], self.cnt[eng])
            inc = (self.sem[eng], 1)
        self.ops[eng].append((ws, fn, inc))
        return tok

    def dma(self, eng, fn, key, waits=()):
        if key not in self.dsem:
            s = self.nc.alloc_semaphore(name=f"{self.name}_d{len(self.dsem)}")
            self.all_sems.append(s)
            self.dsem[key] = [s, 0, eng]
        d = self.dsem[key]
        assert d[2] == eng
        d[1] += 16
        ws = self._waits(eng, waits)
        self.ops[eng].append((ws, fn, (d[0], 16)))
        return (d[0], d[1])

    def emit(self):
        nc = self.nc
        for key, (s, v, eng) in self.dsem.items():
            ws = self._waits(eng, [(s, v)])
            if ws:
                self.ops[eng].append((ws, None, None))

        def run(engobj, name):
            for ws, fn, inc in self.ops[name]:
                for sem, val in ws:
                    engobj.wait_ge(sem, val)
                if fn is None:
                    continue
                ins = fn(engobj)
                if inc is not None:
                    ins.then_inc(inc[0], inc[1])

        with nc.Block() as block:
            block.tensor(lambda e: run(e, "pe"))
            block.scalar(lambda e: run(e, "act"))
            block.vector(lambda e: run(e, "dve"))
            block.gpsimd(lambda e: run(e, "pool"))
            block.sync(lambda e: run(e, "sp"))
        nc.all_engine_barrier()
        nc.clear_and_free_semaphores(self.all_sems)
        nc.all_engine_barrier()
        self.es.close()


class Banks:
    def __init__(self, ps, ids):
        self.ps = ps
        self.ids = list(ids)
        self.free = {b: None for b in self.ids}
        self.i = 0

    def next(self):
        b = self.ids[self.i % len(self.ids)]
        self.i += 1
        return b, self.free[b]

    def release(self, b, tok):
        self.free[b] = tok


class WStream:
    def __init__(self, ph, name, shape, nbuf, dt=BF16, eng="pool", bufs=None, free0=None):
        self.ph = ph
        self.name = name
        self.eng = eng
        self.bufs = bufs if bufs is not None else [ph.alloc(f"{name}{i}", shape, dt) for i in range(nbuf)]
        self.free = [free0] * len(self.bufs)
        self.i = 0

    def load(self, fns):
        slot = self.i % len(self.bufs)
        self.i += 1
        buf = self.bufs[slot]
        toks = []
        for j, fn in enumerate(fns):
            toks.append(self.ph.dma(self.eng, (lambda e, fn=fn, buf=buf: fn(e, buf)), f"{self.name}{slot}",
                                    waits=[self.free[slot]]))
        return slot, buf, toks[-1:]

    def release(self, slot, tok):
        self.free[slot] = tok


def bcast_mid(ap2d, rep):
    a = ap2d.ap
    return bass.AP(ap2d.tensor, ap2d.offset, [list(a[0]), [0, rep], list(a[1])])


def bcast_last(ap2d, n):
    a = ap2d.ap
    return bass.AP(ap2d.tensor, ap2d.offset, [list(a[0]), [0, n]])


class Kern:
    def __init__(self, cfg):
        self.cfg = cfg
        self.nc = bass.Bass("TRN2", target_bir_lowering=False)
        self.pid = 0

    def phase(self, name):
        self.pid += 1
        return Phase(self.nc, f"p{self.pid}{name}")

    def declare(self):
        nc = self.nc
        I = {}

        def inp(name, shape):
            I[name] = nc.dram_tensor(name, list(shape), F32, kind="ExternalInput").ap()

        inp("x", (NSEQ, SEQ, D))
        inp("a_norm_g", (1, D)); inp("a_w_in", (1, D, 2 * A_HALF)); inp("a_v_norm_g", (1, A_HALF))
        inp("a_w_spatial", (1, 8, 128, 128)); inp("a_b_spatial", (1, 8, 128)); inp("a_w_out", (1, A_HALF, D))
        inp("f_norm_g", (1, D)); inp("f_w_in", (1, D, 2 * D_FF)); inp("f_w_out", (1, D_FF, D))
        inp("kv_norm_g", (D,)); inp("kv_w", (D, 2 * D + 16)); inp("kv_b_f", (16,)); inp("k_norm_g", (64,))
        inp("b_norm_g", (1, D)); inp("b_w_in", (1, D, 2 * D)); inp("q_norm_g", (1, 64)); inp("b_w_out", (1, D, D))
        inp("m_norm_g", (1, D)); inp("m_w_router", (1, D, NEXP)); inp("m_w_in", (1, NEXP, D, 2 * D_EXP))
        inp("m_w_out", (1, NEXP, D_EXP, D))
        self.I = I
        self.out = nc.dram_tensor("out", [NSEQ, SEQ, D], F32, kind="ExternalOutput").ap()
        if self.cfg.get("dbg"):
            self.dbg = nc.dram_tensor("dbg", [128, KC, ST], F32, kind="ExternalOutput").ap()
        self.KT_d = nc.dram_tensor("kt_scr", [128, KC, SEQ], BF16).ap()
        self.V_d = nc.dram_tensor("v_scr", [128, KC, SEQ // 128, 128], BF16).ap()

    def alloc_persist(self, es):
        nc = self.nc

        def sb(name, shape, dt=F32):
            return es.enter_context(nc.sbuf_tensor(name, shape, dt))

        self.hT = sb("hT", [128, KC, ST])
        self.ident = sb("ident", [128, 128])
        self.ones_bf = sb("ones_bf", [128, 128], BF16)
        self.blk_bf = sb("blk_bf", [128, 128], BF16)
        self.tri_f = sb("tri_f", [128, 128])
        self.ones_f = sb("ones_f", [128, 128])
        self.trimask = sb("trimask", [128, 128], BF16)
        self.swap_f = sb("swap_f", [128, 128])
        self.gains = sb("gains", [128, 5, KC])
        self.vgain = sb("vgain", [128, 24])
        self.kqg = sb("kqg", [128, 2])
        self.biasbc = sb("biasbc", [128, 8, 128])
        self.bf_bc = sb("bf_bc", [128, 16])
        self.wTsp = sb("wTsp", [128, 8, 128])
        self.gwr = sb("gwr", [128, KC, NEXP])
        self.sel8 = sb("sel8", [8, NEXP, 128])
        self.epsc = sb("epsc", [128, 1])
        self.onec = sb("onec", [128, 1])
        self.logf = sb("logf", [128, SEQ // 128, 16])
        self.Fcum = sb("Fcum", [128, SEQ // 128, 16])

    def ph_setup(self):
        I = self.I
        ph = self.phase("setup")
        nc = self.nc
        wsp = ph.alloc("wsp", [128, 8, 128], F32)
        rt = ph.alloc("rt", [128, KC, NEXP], F32)
        ps = ph.psum()
        toks = []
        with nc.allow_non_contiguous_dma(reason="tiny one-time parameter loads"):
            def ld(out, in_, key):
                return ph.dma("sp", lambda e: e.dma_start(out=out, in_=in_, allow_slow_non_contiguous=True), key)
            for i, nm in enumerate(["a_norm_g", "f_norm_g", "kv_norm_g", "b_norm_g", "m_norm_g"]):
                src = I[nm] if nm == "kv_norm_g" else I[nm][0]
                toks.append(ld(self.gains[:, i, :], src.rearrange("(c p) -> p c", p=128), "g"))
            toks.append(ld(self.vgain[:], I["a_v_norm_g"][0].rearrange("(c p) -> p c", p=128), "g"))
            for half in range(2):
                toks.append(ld(self.kqg[half * 64:(half + 1) * 64, 0:1], I["k_norm_g"].rearrange("(p o) -> p o", o=1), "g"))
                toks.append(ld(self.kqg[half * 64:(half + 1) * 64, 1:2], I["q_norm_g"][0].rearrange("(p o) -> p o", o=1), "g"))
            toks.append(ld(self.biasbc[:], bass.AP(I["a_b_spatial"].tensor, 0, [[0, 128], [128, 8], [1, 128]]), "g"))
            toks.append(ld(self.bf_bc[:], bass.AP(I["kv_b_f"].tensor, 0, [[0, 128], [1, 16]]), "g"))
            toks.append(ld(wsp[:], I["a_w_spatial"][0].rearrange("g i j -> i g j"), "g"))
            toks.append(ld(rt[:], I["m_w_router"][0].rearrange("(c p) e -> p c e", p=128), "g"))
        tl = toks[-1]
        pl = []
        def P(fn, waits=()):
            t = ph.op("pool", fn, waits=waits)
            pl.append(t)
            return t
        P(lambda e: e.memset(self.ident[:], 0.0))
        t_id = P(lambda e: e.affine_select(out=self.ident[:], in_=self.ident[:], pattern=[[-1, 128]],
                                           compare_op=ALU.not_equal, fill=1.0, base=0, channel_multiplier=1), waits=[pl[-1]])
        P(lambda e: e.memset(self.ones_bf[:], 1.0))
        P(lambda e: e.memset(self.epsc[:], EPS))
        P(lambda e: e.memset(self.onec[:], 1.0))
        P(lambda e: e.memset(self.ones_f[:], 1.0))
        P(lambda e: e.memset(self.blk_bf[:], 0.0))
        P(lambda e: e.memset(self.blk_bf[0:64, 0:64], 1.0), waits=[pl[-1]])
        P(lambda e: e.memset(self.blk_bf[64:128, 64:128], 1.0), waits=[pl[-2]])
        P(lambda e: e.memset(self.tri_f[:], 1.0))
        P(lambda e: e.affine_select(out=self.tri_f[:], in_=self.tri_f[:], pattern=[[1, 128]],
                                    compare_op=ALU.is_ge, fill=0.0, base=0, channel_multiplier=-1), waits=[pl[-1]])
        t_tri = pl[-1]
        P(lambda e: e.tensor_copy(out=self.trimask[:], in_=self.tri_f[:]), waits=[t_tri])
        P(lambda e: e.memset(self.swap_f[:], 0.0))
        t0 = pl[-1]
        P(lambda e: e.affine_select(out=self.swap_f[:, 0:64], in_=self.swap_f[:, 0:64], pattern=[[-1, 64]],
                                    compare_op=ALU.not_equal, fill=1.0, base=-64, channel_multiplier=1), waits=[t0])
        P(lambda e: e.affine_select(out=self.swap_f[:, 64:128], in_=self.swap_f[:, 64:128], pattern=[[-1, 64]],
                                    compare_op=ALU.not_equal, fill=1.0, base=0, channel_multiplier=1), waits=[t0])
        P(lambda e: e.memset(self.sel8[:], 0.0))
        P(lambda e: e.affine_select(out=self.sel8[:], in_=self.sel8[:], pattern=[[-1, NEXP], [0, 128]],
                                    compare_op=ALU.not_equal, fill=1.0, base=0, channel_multiplier=1), waits=[pl[-1]])
        t_pool = pl[-1]
        t1 = ph.op("dve", lambda e: e.tensor_scalar(out=self.kqg[:, 1:2], in0=self.kqg[:, 1:2], scalar1=0.125, scalar2=None,
                                                    op0=ALU.mult), waits=[tl])
        tg = None
        for c in range(KC):
            tg = ph.op("dve", lambda e, c=c: e.tensor_scalar(out=self.gwr[:, c, :], in0=rt[:, c, :],
                                                             scalar1=self.gains[:, 4, c:c + 1], scalar2=None, op0=ALU.mult),
                       waits=[tl])
        tp = None
        for g in range(8):
            tp = ph.op("pe", lambda e, g=g: e.transpose(ps[:, g // 4, (g % 4) * 128:(g % 4 + 1) * 128], wsp[:, g, :], self.ident[:]),
                       waits=[tl, t_id])
        tc = None
        for b in range(2):
            tc = ph.op("dve", lambda e, b=b: e.tensor_copy(out=self.wTsp[:, b * 4:(b + 1) * 4, :],
                                                           in_=ps[:, b, :].rearrange("p (g i) -> p g i", g=4)), waits=[tp])
        ph.op("dve", lambda e: e.memset(self.wTsp[64:128, :, 0:64], 0.0), waits=[tc])
        ph.emit()

    def ph_load(self, seq, st):
        ph = self.phase("load")
        xt = [ph.alloc(f"xt{i}", [128, D], F32) for i in range(2)]
        ps = ph.psum()
        banks = Banks(ps, range(8))
        xfree = [None, None]
        for t in range(NT):
            sl = t % 2
            r0 = st * ST + t * 128
            tl = ph.dma("sp", lambda e, sl=sl, r0=r0: e.dma_start(out=xt[sl][:], in_=self.I["x"][seq, r0:r0 + 128, :]),
                        f"x{sl}", waits=[xfree[sl]])
            for half in range(2):
                b, bf = banks.next()
                tp = None
                for j in range(4):
                    c = half * 4 + j
                    tp = ph.op("pe", lambda e, b=b, j=j, c=c, sl=sl: e.transpose(ps[:, b, j * 128:(j + 1) * 128],
                                                                                 xt[sl][:, c * 128:(c + 1) * 128], self.ident[:]),
                               waits=[tl, bf], sig=(j == 3))
                eng = "act" if half == 0 else "dve"
                if eng == "act":
                    tcp = ph.op("act", lambda e, b=b, half=half, t=t: e.activation(
                        out=self.hT[:, half * 4:(half + 1) * 4, t * 128:(t + 1) * 128],
                        in_=ps[:, b, :].rearrange("p (c i) -> p c i", c=4), func=AF.Copy), waits=[tp])
                else:
                    tcp = ph.op("dve", lambda e, b=b, half=half, t=t: e.tensor_copy(
                        out=self.hT[:, half * 4:(half + 1) * 4, t * 128:(t + 1) * 128],
                        in_=ps[:, b, :].rearrange("p (c i) -> p c i", c=4)), waits=[tp])
                banks.release(b, tcp)
                if half == 1:
                    xfree[sl] = tp
        ph.emit()

    def ph_store(self, seq, st):
        ph = self.phase("store")
        ot = [ph.alloc(f"ot{i}", [128, D], F32) for i in range(2)]
        ps = ph.psum()
        banks = Banks(ps, range(8))
        ofree = [None, None]
        for t in range(NT):
            sl = t % 2
            r0 = st * ST + t * 128
            cps = []
            for half in range(2):
                b, bf = banks.next()
                tp = None
                for j in range(4):
                    c = half * 4 + j
                    tp = ph.op("pe", lambda e, b=b, j=j, c=c, t=t: e.transpose(ps[:, b, j * 128:(j + 1) * 128],
                                                                               self.hT[:, c, t * 128:(t + 1) * 128], self.ident[:]),
                               waits=[bf], sig=(j == 3))
                if half == 0:
                    tcp = ph.op("act", lambda e, b=b, sl=sl: e.activation(out=ot[sl][:, 0:512], in_=ps[:, b, :], func=AF.Copy),
                                waits=[tp, ofree[sl]])
                else:
                    tcp = ph.op("dve", lambda e, b=b, sl=sl: e.tensor_copy(out=ot[sl][:, 512:1024], in_=ps[:, b, :]),
                                waits=[tp, ofree[sl]])
                banks.release(b, tcp)
                cps.append(tcp)
            ofree[sl] = ph.dma("sp", lambda e, sl=sl, r0=r0: e.dma_start(out=self.out[seq, r0:r0 + 128, :], in_=ot[sl][:]),
                               f"o{sl}", waits=cps)
        ph.emit()

    def emit_norm(self, ph, ps, banks, gidx, xn, sq, rstd_bufs, want_rstd=False):
        toks = []
        rstd_toks = []
        for tt in range(NTT):
            cs = slice(tt * 512, (tt + 1) * 512)
            b, bf = banks.next()
            tm = None
            for c in range(KC):
                eng = "act" if c % 2 == 0 else "dve"
                if eng == "act":
                    tsq = ph.op("act", lambda e, c=c, cs=cs: e.activation(out=sq[:, c, :], in_=self.hT[:, c, cs], func=AF.Square),
                                waits=[self._sq_free.get(c)])
                else:
                    tsq = ph.op("dve", lambda e, c=c, cs=cs: e.tensor_tensor(out=sq[:, c, :], in0=self.hT[:, c, cs], in1=self.hT[:, c, cs],
                                                                            op=ALU.mult), waits=[self._sq_free.get(c)])
                tm = ph.op("pe", lambda e, c=c, b=b: e.matmul(ps[:, b, :], lhsT=self.ones_bf[:], rhs=sq[:, c, :],
                                                              start=(c == 0), stop=(c == KC - 1)), waits=[tsq, bf])
                self._sq_free[c] = tm
            rs = rstd_bufs[tt]
            t1, t2 = self.emit_rsqrt(ph, rs[:], ps[:, b, :], 1.0 / D, [tm, self._rs_free.get(tt)])
            banks.release(b, t1)
            rstd_toks.append(t2)
            last = []
            for c in range(KC):
                tx = ph.op("dve", lambda e, c=c, cs=cs, rs=rs: e.scalar_tensor_tensor(
                    out=xn[:, c, cs], in0=self.hT[:, c, cs], scalar=self.gains[:, gidx, c:c + 1], in1=rs[:],
                    op0=ALU.mult, op1=ALU.mult), waits=[t2, self._xn_free])
                last.append(tx)
            toks.append(last[-1])
            self._rs_free[tt] = last[-1]
        return toks, rstd_toks

    def emit_rsqrt(self, ph, out_ap, in_ap, scale, waits):
        n = out_ap.shape[0]
        t1 = ph.op("act", lambda e: e.activation(out=out_ap, in_=in_ap, func=AF.Ln, bias=self.epsc[0:n, :], scale=scale), waits=waits)
        t2 = ph.op("act", lambda e: e.activation(out=out_ap, in_=out_ap, func=AF.Exp, scale=-0.5), waits=[t1])
        return t1, t2

    def norm_state(self):
        self._sq_free = {}
        self._rs_free = {}
        self._xn_free = None

    def ph_mixer_a(self):
        I = self.I
        ph = self.phase("mixA")
        self.norm_state()
        ps = ph.psum()
        banks = Banks(ps, range(8))
        xn = ph.alloc("xn", [128, KC, ST], BF16)
        rstd = [ph.alloc(f"rstd{i}", [128, 512], F32) for i in range(NTT)]
        big1 = ph.alloc("big1", [128, NT * A_HALF], BF16)
        big2 = ph.alloc("big2", [128, 24 * ST], BF16)
        vg = big1[:].rearrange("p (t n) -> p t n", t=NT)
        pT = big2[:].rearrange("p (m t) -> p m t", m=24)
        sq = big2[:, 0:KC * 512].rearrange("p (c n) -> p c n", c=KC)
        junk = big2[:, 8192:8192 + A_HALF]
        wts = ph.alloc("wts", [128, NT, 8 * 128], BF16)
        ssv = ph.alloc("ssv", [128, NT], F32)
        rsv = ph.alloc("rsv", [128, NT], F32)
        usb = [ph.alloc(f"usb{i}", [128, 512], BF16) for i in range(4)]
        t1b = [ph.alloc(f"t1b{i}", [128, 512], F32) for i in range(2)]
        ws = WStream(ph, "w", [128, KC, 512], 2)
        w_in = I["a_w_in"][0]
        w_out = I["a_w_out"][0]

        xtoks, _ = self.emit_norm(ph, ps, banks, 0, xn, sq, rstd)
        xall = xtoks[-1]

        def load_in(col0):
            return ws.load([lambda e, buf, col0=col0: e.dma_start(
                out=buf[:], in_=w_in[:, col0:col0 + 512].rearrange("(k p) n -> p k n", p=128))])

        pend = load_in(A_HALF)
        for n in range(6):
            slot, wb, wt = pend
            if n + 1 < 6:
                pend = load_in(A_HALF + (n + 1) * 512)
            else:
                pend = load_in(0)
            tm = None
            for t in range(NT):
                b, bf = banks.next()
                for k in range(KC):
                    tm = ph.op("pe", lambda e, b=b, k=k, t=t, wb=wb: e.matmul(
                        ps[:, b, :], lhsT=xn[:, k, t * 128:(t + 1) * 128], rhs=wb[:, k, :], start=(k == 0), stop=(k == KC - 1)),
                        waits=[xall, bf] + wt, sig=(k == KC - 1))
                tg = ph.op("act", lambda e, b=b, t=t, n=n: e.activation(out=vg[:, t, n * 512:(n + 1) * 512], in_=ps[:, b, :],
                                                                        func=AF.Gelu), waits=[tm])
                banks.release(b, tg)
                vg_last = tg
            ws.release(slot, tm)
        tz = ph.op("dve", lambda e: e.memset(ssv[:], 0.0))
        tsq = None
        for t in range(NT):
            tsq = ph.op("act", lambda e, t=t: e.activation(out=junk, in_=vg[:, t, :], func=AF.Square,
                                                           accum_out=ssv[:, t:t + 1]), waits=[vg_last, tz])
        _, tr = self.emit_rsqrt(ph, rsv[:], ssv[:], 1.0 / A_HALF, [tsq])
        tw = None
        for t in range(NT):
            tw = ph.op("dve", lambda e, t=t: e.tensor_scalar(out=wts[:, t, :], in0=self.wTsp[:].rearrange("p g i -> p (g i)"),
                                                             scalar1=rsv[:, t:t + 1], scalar2=None, op0=ALU.mult), waits=[tr])
        ufree = [None] * 4
        t1free = [None] * 2
        ui = 0
        ti = 0
        for piece in range(6):
            slot, wb, wt = pend
            if piece + 1 < 6:
                pend = load_in((piece + 1) * 512)
            tlast = None
            for mm in range(4):
                m = piece * 4 + mm
                g = m // 3
                for tt in range(NTT):
                    cs = slice(tt * 512, (tt + 1) * 512)
                    bu, buf_ = banks.next()
                    tm = None
                    for k in range(KC):
                        tm = ph.op("pe", lambda e, bu=bu, k=k, mm=mm, cs=cs, wb=wb: e.matmul(
                            ps[:, bu, :], lhsT=wb[:, k, mm * 128:(mm + 1) * 128], rhs=xn[:, k, cs],
                            start=(k == 0), stop=(k == KC - 1)), waits=[buf_] + wt, sig=(k == KC - 1))
                    tlast = tm
                    us = ui % 4
                    ui += 1
                    tu = ph.op("act", lambda e, bu=bu, us=us: e.activation(out=usb[us][:], in_=ps[:, bu, :], func=AF.Gelu),
                               waits=[tm, ufree[us]])
                    banks.release(bu, tu)
                    bs, bsf = banks.next()
                    tsm = None
                    for q in range(4):
                        t = tt * 4 + q
                        tsm = ph.op("pe", lambda e, bs=bs, q=q, t=t, m=m, g=g: e.matmul(
                            ps[:, bs, q * 128:(q + 1) * 128], lhsT=vg[:, t, m * 128:(m + 1) * 128],
                            rhs=wts[:, t, g * 128:(g + 1) * 128], start=True, stop=True), waits=[bsf, tw], sig=(q == 3))
                    tlast = tsm
                    t1s = ti % 2
                    ti += 1
                    ta = ph.op("dve", lambda e, bs=bs, m=m, g=g, t1s=t1s: e.scalar_tensor_tensor(
                        out=t1b[t1s][:].rearrange("p (r i) -> p r i", r=4), in0=ps[:, bs, :].rearrange("p (r i) -> p r i", r=4),
                        scalar=self.vgain[:, m:m + 1], in1=bcast_mid(self.biasbc[:, g, :], 4), op0=ALU.mult, op1=ALU.add),
                        waits=[tsm, t1free[t1s]])
                    banks.release(bs, ta)
                    tb = ph.op("dve", lambda e, m=m, cs=cs, t1s=t1s, us=us: e.tensor_tensor(
                        out=pT[:, m, cs], in0=t1b[t1s][:], in1=usb[us][:], op=ALU.mult), waits=[ta, tu])
                    ufree[us] = tb
                    t1free[t1s] = tb
                    p_last = tb
            ws.release(slot, tlast)
        ws2 = WStream(ph, "w2", None, 2, bufs=[big1[:, i * 6144:(i + 1) * 6144].rearrange("p (k n) -> p k n", k=24) for i in range(2)],
                      free0=tlast)

        def load_out(c0):
            return ws2.load([lambda e, buf, c0=c0: e.dma_start(
                out=buf, in_=w_out[:, c0:c0 + 256].rearrange("(k p) n -> p k n", p=128))])
        pend2 = load_out(0)
        for piece in range(4):
            slot, wb, wt = pend2
            if piece + 1 < 4:
                pend2 = load_out((piece + 1) * 256)
            tm = None
            for mm in range(2):
                mo = piece * 2 + mm
                for tt in range(NTT):
                    cs = slice(tt * 512, (tt + 1) * 512)
                    b, bf = banks.next()
                    for k in range(24):
                        tm = ph.op("pe", lambda e, b=b, k=k, mm=mm, cs=cs, wb=wb: e.matmul(
                            ps[:, b, :], lhsT=wb[:, k, mm * 128:(mm + 1) * 128], rhs=pT[:, k, cs],
                            start=(k == 0), stop=(k == 23)), waits=[bf, p_last] + wt, sig=(k == 23))
                    ta = ph.op("dve", lambda e, b=b, mo=mo, cs=cs: e.tensor_tensor(
                        out=self.hT[:, mo, cs], in0=self.hT[:, mo, cs], in1=ps[:, b, :], op=ALU.add), waits=[tm])
                    banks.release(b, ta)
            ws2.release(slot, tm)
        ph.emit()

    def emit_swiglu(self, ph, ps, banks, xn, xall, w_in, w_out, dff, hbuf, ws, ws2, gate_bc=None, gate_tok=None, tmpb=None,
                    next_w_in=None):
        nj = dff // 128
        npiece = nj // 2

        def load_in(p, w_in=w_in):
            c0 = p * 256
            return ws.load([
                lambda e, buf, c0=c0: e.dma_start(out=buf[:, :, 0:256], in_=w_in[:, c0:c0 + 256].rearrange("(k p) n -> p k n", p=128)),
                lambda e, buf, c0=c0: e.dma_start(out=buf[:, :, 256:512],
                                                  in_=w_in[:, dff + c0:dff + c0 + 256].rearrange("(k p) n -> p k n", p=128)),
            ])

        def load_out(c0):
            return ws2.load([lambda e, buf, c0=c0: e.dma_start(
                out=buf[:, 0:nj, :], in_=w_out[:, c0:c0 + 256].rearrange("(k p) n -> p k n", p=128))])
        pre = getattr(self, "_pre_in", None)
        self._pre_in = None
        pend = pre if pre is not None else load_in(0)
        pend2 = None
        si = 0
        h_last = None
        for p in range(npiece):
            slot, wb, wt = pend
            if p + 1 < npiece:
                pend = load_in(p + 1)
            if p == 0:
                pend2 = load_out(0)
            tlast = None
            for jj in range(2):
                j = p * 2 + jj
                for tt in range(NTT):
                    cs = slice(tt * 512, (tt + 1) * 512)
                    ba, baf = banks.next()
                    bb, bbf = banks.next()
                    ta_ = tb_ = None
                    for k in range(KC):
                        ta_ = ph.op("pe", lambda e, ba=ba, k=k, jj=jj, cs=cs, wb=wb: e.matmul(
                            ps[:, ba, :], lhsT=wb[:, k, jj * 128:(jj + 1) * 128], rhs=xn[:, k, cs],
                            start=(k == 0), stop=(k == KC - 1)), waits=[baf, xall] + wt, sig=(k == KC - 1))
                    for k in range(KC):
                        tb_ = ph.op("pe", lambda e, bb=bb, k=k, jj=jj, cs=cs, wb=wb: e.matmul(
                            ps[:, bb, :], lhsT=wb[:, k, 256 + jj * 128:256 + (jj + 1) * 128], rhs=xn[:, k, cs],
                            start=(k == 0), stop=(k == KC - 1)), waits=[bbf], sig=(k == KC - 1))
                    tlast = tb_
                    s = si % 2
                    si += 1
                    tsl = ph.op("act", lambda e, ba=ba, s=s: e.activation(out=self._silu[s][:], in_=ps[:, ba, :], func=AF.Silu),
                                waits=[ta_, self._silu_free[s]])
                    banks.release(ba, tsl)
                    th = ph.op("dve", lambda e, bb=bb, s=s, j=j, cs=cs: e.tensor_tensor(
                        out=hbuf[:, j, cs], in0=self._silu[s][:], in1=ps[:, bb, :], op=ALU.mult),
                        waits=[tsl, tb_, self._h_free])
                    banks.release(bb, th)
                    self._silu_free[s] = th
                    h_last = th
            ws.release(slot, tlast)
        if next_w_in is not None:
            self._pre_in = load_in(0, next_w_in)
        tm = None
        for piece in range(4):
            slot, wb, wt = pend2
            if piece + 1 < 4:
                pend2 = load_out((piece + 1) * 256)
            for mm in range(2):
                mo = piece * 2 + mm
                for tt in range(NTT):
                    cs = slice(tt * 512, (tt + 1) * 512)
                    b, bf = banks.next()
                    for k in range(nj):
                        tm = ph.op("pe", lambda e, b=b, k=k, mm=mm, cs=cs, wb=wb: e.matmul(
                            ps[:, b, :], lhsT=wb[:, k, mm * 128:(mm + 1) * 128], rhs=hbuf[:, k, cs],
                            start=(k == 0), stop=(k == nj - 1)), waits=[bf, h_last] + wt, sig=(k == nj - 1))
                    if gate_bc is None:
                        ta = ph.op("dve", lambda e, b=b, mo=mo, cs=cs: e.tensor_tensor(
                            out=self.hT[:, mo, cs], in0=self.hT[:, mo, cs], in1=ps[:, b, :], op=ALU.add), waits=[tm])
                        banks.release(b, ta)
                    else:
                        s = self._tmp_i % 2
                        self._tmp_i += 1
                        tq = ph.op("dve", lambda e, b=b, cs=cs, s=s: e.tensor_tensor(
                            out=tmpb[s][:], in0=ps[:, b, :], in1=gate_bc[:, cs], op=ALU.mult),
                            waits=[tm, gate_tok, self._tmp_free[s]])
                        banks.release(b, tq)
                        ta = ph.op("dve", lambda e, mo=mo, cs=cs, s=s: e.tensor_tensor(
                            out=self.hT[:, mo, cs], in0=self.hT[:, mo, cs], in1=tmpb[s][:], op=ALU.add), waits=[tq])
                        self._tmp_free[s] = ta
            ws2.release(slot, tm)
        self._h_free = tm
        return tm

    def ph_ffn(self):
        I = self.I
        ph = self.phase("ffn")
        self.norm_state()
        ps = ph.psum()
        banks = Banks(ps, range(8))
        xn = ph.alloc("xn", [128, KC, ST], BF16)
        sq = ph.alloc("sq", [128, KC, 512], BF16)
        rstd = [ph.alloc(f"rstd{i}", [128, 512], F32) for i in range(NTT)]
        hbuf = ph.alloc("hbuf", [128, D_FF // 128, ST], BF16)
        self._silu = [ph.alloc(f"silu{i}", [128, 512], F32) for i in range(2)]
        self._silu_free = [None, None]
        self._h_free = None
        ws = WStream(ph, "w", [128, KC, 512], 2)
        ws2 = WStream(ph, "w2", [128, D_FF // 128, 256], 2)
        xtoks, _ = self.emit_norm(ph, ps, banks, 1, xn, sq, rstd)
        self.emit_swiglu(ph, ps, banks, xn, xtoks[-1], I["f_w_in"][0], I["f_w_out"][0], D_FF, hbuf, ws, ws2)
        ph.emit()

    def emit_headnorm(self, ph, ps, banks, src_bank, src_tok, gcol, out_ap, raw, sqb, rsb, free_tok):
        t_raw = ph.op("act", lambda e: e.activation(out=raw[:], in_=ps[:, src_bank, :], func=AF.Copy), waits=[src_tok, free_tok])
        t_sq = ph.op("act", lambda e: e.activation(out=sqb[:], in_=ps[:, src_bank, :], func=AF.Square), waits=[src_tok, free_tok])
        banks.release(src_bank, t_sq)
        b, bf = banks.next()
        tm = ph.op("pe", lambda e: e.matmul(ps[:, b, :], lhsT=self.blk_bf[:], rhs=sqb[:], start=True, stop=True), waits=[t_sq, bf])
        t1, t2 = self.emit_rsqrt(ph, rsb[:], ps[:, b, :], 1.0 / 64, [tm, free_tok])
        banks.release(b, t1)
        t3 = ph.op("dve", lambda e: e.scalar_tensor_tensor(out=out_ap, in0=raw[:], scalar=self.kqg[:, gcol:gcol + 1], in1=rsb[:],
                                                           op0=ALU.mult, op1=ALU.mult), waits=[t2, t_raw])
        return t3

    def ph_kv(self, st):
        I = self.I
        ph = self.phase("kv")
        self.norm_state()
        ps = ph.psum()
        banks = Banks(ps, range(8))
        xn = ph.alloc("xn", [128, KC, ST], BF16)
        sq = ph.alloc("sq", [128, KC, 512], BF16)
        rstd = [ph.alloc(f"rstd{i}", [128, 512], F32) for i in range(NTT)]
        KT = ph.alloc("KT", [128, KC, ST], BF16)
        Vs = ph.alloc("Vs", [128, NT, D], BF16)
        wf = ph.alloc("wf", [128, KC, 16], BF16)
        raws = [ph.alloc(f"raw{i}", [128, 512], F32) for i in range(2)]
        sqbs = [ph.alloc(f"sqb{i}", [128, 512], BF16) for i in range(2)]
        rsbs = [ph.alloc(f"rsb{i}", [128, 512], F32) for i in range(2)]
        ef = ph.alloc("ef", [128, NT, 16], F32)
        ws = WStream(ph, "w", [128, KC, 512], 2)
        kv_w = I["kv_w"]
        xtoks, _ = self.emit_norm(ph, ps, banks, 2, xn, sq, rstd)
        xall = xtoks[-1]
        tf_w = ph.dma("pool", lambda e: e.dma_start(out=wf[:], in_=kv_w[:, 2 * D:2 * D + 16].rearrange("(k p) n -> p k n", p=128)), "wf")

        def load(col0):
            return ws.load([lambda e, buf, col0=col0: e.dma_start(
                out=buf[:], in_=kv_w[:, col0:col0 + 512].rearrange("(k p) n -> p k n", p=128))])
        pend = load(0)
        hfree = [None, None]
        hi = 0
        k_last = None
        for piece in range(2):
            slot, wb, wt = pend
            pend = load((piece + 1) * 512) if piece == 0 else load(D)
            tm = None
            for mm in range(4):
                m = piece * 4 + mm
                for tt in range(NTT):
                    cs = slice(tt * 512, (tt + 1) * 512)
                    b, bf = banks.next()
                    for k in range(KC):
                        tm = ph.op("pe", lambda e, b=b, k=k, mm=mm, cs=cs, wb=wb: e.matmul(
                            ps[:, b, :], lhsT=wb[:, k, mm * 128:(mm + 1) * 128], rhs=xn[:, k, cs],
                            start=(k == 0), stop=(k == KC - 1)), waits=[bf, xall] + wt, sig=(k == KC - 1))
                    s = hi % 2
                    hi += 1
                    k_last = self.emit_headnorm(ph, ps, banks, b, tm, 0, KT[:, m, cs], raws[s], sqbs[s], rsbs[s], hfree[s])
                    hfree[s] = k_last
            ws.release(slot, tm)
        tk_st = ph.dma("sp", lambda e: e.dma_start(out=self.KT_d[:, :, st * ST:(st + 1) * ST], in_=KT[:]), "kst", waits=[k_last])
        v_last = None
        for n in range(2):
            slot, wb, wt = pend
            if n == 0:
                pend = load(D + 512)
            tm = None
            for t in range(NT):
                b, bf = banks.next()
                for k in range(KC):
                    tm = ph.op("pe", lambda e, b=b, k=k, t=t, wb=wb: e.matmul(
                        ps[:, b, :], lhsT=xn[:, k, t * 128:(t + 1) * 128], rhs=wb[:, k, :], start=(k == 0), stop=(k == KC - 1)),
                        waits=[bf, xall] + wt, sig=(k == KC - 1))
                if t % 2 == 0:
                    v_last = ph.op("act", lambda e, b=b, t=t, n=n: e.activation(out=Vs[:, t, n * 512:(n + 1) * 512], in_=ps[:, b, :],
                                                                                func=AF.Copy), waits=[tm])
                else:
                    v_last = ph.op("dve", lambda e, b=b, t=t, n=n: e.tensor_copy(out=Vs[:, t, n * 512:(n + 1) * 512], in_=ps[:, b, :]),
                                   waits=[tm])
                banks.release(b, v_last)
                if t == NT - 2:
                    v_prev = v_last
            ws.release(slot, tm)
        for c in range(KC):
            ph.dma("sp", lambda e, c=c: e.dma_start(out=self.V_d[:, c, st * NT:(st + 1) * NT, :], in_=Vs[:, :, c * 128:(c + 1) * 128]),
                   "vst", waits=[v_last, v_prev])
        b, bf = banks.next()
        tm = None
        for t in range(NT):
            for k in range(KC):
                tm = ph.op("pe", lambda e, b=b, k=k, t=t: e.matmul(
                    ps[:, b, t * 16:(t + 1) * 16], lhsT=xn[:, k, t * 128:(t + 1) * 128], rhs=wf[:, k, :],
                    start=(k == 0), stop=(k == KC - 1)), waits=[bf, xall, tf_w], sig=(k == KC - 1 and t == NT - 1))
        g0 = st * NT
        t1 = ph.op("dve", lambda e: e.tensor_tensor(out=ef[:], in0=ps[:, b, 0:NT * 16].rearrange("p (t h) -> p t h", t=NT),
                                                    in1=bcast_mid(self.bf_bc[:], NT), op=ALU.add), waits=[tm])
        banks.release(b, t1)
        t2 = ph.op("act", lambda e: e.activation(out=ef[:], in_=ef[:], func=AF.Exp, scale=-1.0), waits=[t1])
        t3 = ph.op("act", lambda e: e.activation(out=ef[:], in_=ef[:], func=AF.Ln, bias=self.onec[:], scale=1.0), waits=[t2])
        t4 = ph.op("dve", lambda e: e.tensor_scalar(out=self.logf[:, g0:g0 + NT, :], in0=ef[:], scalar1=-1.0, scalar2=None, op0=ALU.mult),
                   waits=[t3])
        b2, b2f = banks.next()
        tm = None
        for t in range(NT):
            T = g0 + t
            for tp in range(T + 1):
                tm = ph.op("pe", lambda e, b2=b2, t=t, tp=tp, T=T: e.matmul(
                    ps[:, b2, t * 16:(t + 1) * 16], lhsT=(self.tri_f[:] if tp == T else self.ones_f[:]), rhs=self.logf[:, tp, :],
                    start=(tp == 0), stop=(tp == T)), waits=[b2f, t4], sig=(tp == T and t == NT - 1))
        t5 = ph.op("dve", lambda e: e.tensor_copy(out=self.Fcum[:, g0:g0 + NT, :],
                                                  in_=ps[:, b2, 0:NT * 16].rearrange("p (t h) -> p t h", t=NT)), waits=[tm])
        banks.release(b2, t5)
        ph.emit()

    def ph_mixer_b(self, st):
        I = self.I
        ph = self.phase("mixB")
        self.norm_state()
        ps = ph.psum()
        banks = Banks(ps, range(6))
        xn = ph.alloc("xn", [128, KC, ST], BF16)
        rstd = [ph.alloc(f"rstd{i}", [128, 512], F32) for i in range(NTT)]
        QT = ph.alloc("QT", [128, KC, ST], BF16)
        SG = ph.alloc("SG", [128, KC, ST], BF16)
        OT = ph.alloc("OT", [128, KC, ST], BF16)
        sq = OT[:, 0:4, :].rearrange("p a (b n) -> p (a b) n", b=2)
        Vraw = [ph.alloc(f"Vraw{i}", [128, SEQ // 128, 128], BF16) for i in range(2)]
        raws = [ph.alloc(f"raw{i}", [128, 512], F32) for i in range(2)]
        sqbs = [ph.alloc(f"sqb{i}", [128, 512], BF16) for i in range(2)]
        rsbs = [ph.alloc(f"rsb{i}", [128, 512], F32) for i in range(2)]
        ntk = (st + 1) * NT
        L = ntk * 128
        Kb = [ph.alloc(f"Kb{i}", [128, SEQ], BF16) for i in range(2)]
        Vb = [ph.alloc(f"Vb{i}", [128, SEQ // 128, 2, 128], BF16) for i in range(2)]
        nb = ph.alloc("nb", [128, NTT, SEQ // 128, 16], F32)
        cq = ph.alloc("cq", [128, NTT, 16], F32)
        PT = [ph.alloc(f"PT{i}", [128, 512], BF16) for i in range(6)]
        Rt = [ph.alloc(f"Rt{i}", [128, 512], F32) for i in range(2)]
        Rs = [ph.alloc(f"Rs{i}", [128, 512], F32) for i in range(2)]
        ws = WStream(ph, "w", [128, KC, 512], 2)
        wo = ph.alloc("wo", [128, KC, D], BF16)
        w_in = I["b_w_in"][0]
        xtoks, _ = self.emit_norm(ph, ps, banks, 3, xn, sq, rstd)
        xall = xtoks[-1]
        t_wo = ph.dma("pool", lambda e: e.dma_start(out=wo[:], in_=I["b_w_out"][0].rearrange("(k p) n -> p k n", p=128)), "wo")
        tvo = None
        for i in range(2):
            ph.op("dve", lambda e, i=i: e.memset(Vb[i][:, :, 0, 64:128], 1.0), sig=False)
            tvo = ph.op("dve", lambda e, i=i: e.memset(Vb[i][:, :, 1, 0:64], 1.0))

        def load(col0):
            return ws.load([lambda e, buf, col0=col0: e.dma_start(
                out=buf[:], in_=w_in[:, col0:col0 + 512].rearrange("(k p) n -> p k n", p=128))])
        pend = load(0)
        hfree = [None, None]
        hi = 0
        q_last = None
        g_last = None
        for piece in range(4):
            slot, wb, wt = pend
            if piece + 1 < 4:
                pend = load((piece + 1) * 512)
            tm = None
            for mm in range(4):
                m = (piece % 2) * 4 + mm
                for tt in range(NTT):
                    cs = slice(tt * 512, (tt + 1) * 512)
                    b, bf = banks.next()
                    for k in range(KC):
                        tm = ph.op("pe", lambda e, b=b, k=k, mm=mm, cs=cs, wb=wb: e.matmul(
                            ps[:, b, :], lhsT=wb[:, k, mm * 128:(mm + 1) * 128], rhs=xn[:, k, cs],
                            start=(k == 0), stop=(k == KC - 1)), waits=[bf, xall] + wt, sig=(k == KC - 1))
                    if piece < 2:
                        s = hi % 2
                        hi += 1
                        q_last = self.emit_headnorm(ph, ps, banks, b, tm, 1, QT[:, m, cs], raws[s], sqbs[s], rsbs[s], hfree[s])
                        hfree[s] = q_last
                    else:
                        g_last = ph.op("act", lambda e, b=b, m=m, cs=cs: e.activation(out=SG[:, m, cs], in_=ps[:, b, :], func=AF.Sigmoid),
                                       waits=[tm])
                        banks.release(b, g_last)
            ws.release(slot, tm)
        bq, bqf = banks.next()
        tm = None
        for Q in range(NTT):
            Qa = st * NTT + Q
            npre = Qa * 4
            if npre == 0:
                continue
            for tp in range(npre):
                tm = ph.op("pe", lambda e, Q=Q, tp=tp, npre=npre: e.matmul(
                    ps[:, bq, Q * 16:(Q + 1) * 16], lhsT=self.ones_f[:], rhs=self.logf[:, tp, :], start=(tp == 0), stop=(tp == npre - 1)),
                    waits=[bqf])
        tcq = None
        for Q in range(NTT):
            Qa = st * NTT + Q
            if Qa == 0:
                tcq = ph.op("dve", lambda e, Q=Q: e.memset(cq[:, Q, :], 0.0))
            else:
                tcq = ph.op("dve", lambda e, Q=Q: e.tensor_copy(out=cq[:, Q, :], in_=ps[:, bq, Q * 16:(Q + 1) * 16]), waits=[tm])
        banks.release(bq, tcq)
        tnb = None
        for Q in range(NTT):
            tnb0 = ph.op("dve", lambda e, Q=Q: e.tensor_scalar(out=nb[:, Q, 0:ntk, :], in0=self.Fcum[:, 0:ntk, :], scalar1=-1.0,
                                                               scalar2=-SM_BOUND, op0=ALU.mult, op1=ALU.add), waits=[tcq])
            if st * NTT + Q == 0:
                tnb = tnb0
                continue
            tnb = ph.op("dve", lambda e, Q=Q: e.tensor_tensor(out=nb[:, Q, 0:ntk, :], in0=nb[:, Q, 0:ntk, :],
                                                              in1=bcast_mid(cq[:, Q, :], ntk), op=ALU.add), waits=[tnb0])
        NS, LA, DELAY = len(PT), 3, 8
        abanks = Banks(ps, range(4))
        for b_ in range(4):
            abanks.free[b_] = banks.free[b_]
        pvfree = {4: banks.free[4], 5: banks.free[5], 6: None, 7: None}
        kvfree = [None, None]
        pfree = [None] * NS
        work = []
        gi = 0
        for c in range(KC):
            for Q in range(NTT):
                Qa = st * NTT + Q
                nk = (Qa + 1) * 4
                for hh in range(2):
                    for t in range(nk):
                        work.append(dict(c=c, Q=Q, Qa=Qa, nk=nk, hh=hh, t=t, g=gi, par=gi % 2,
                                         first_pair=(Q == 0 and hh == 0 and t == 0),
                                         last_head=(t == nk - 1), last_g=(hh == 1 and t == nk - 1),
                                         last_pair=(Q == NTT - 1 and hh == 1 and t == nk - 1)))
                gi += 1
        n = len(work)
        ktok = {}
        vtok = {}
        tready = {}
        acc = {}
        pending = []
        o_last = None

        def finalize(c, Q, par, a0, a1):
            nonlocal o_last
            cs0 = Q * 512
            pa, pb_ = 4 + 2 * par, 5 + 2 * par
            ta = ph.op("act", lambda e: e.activation(out=Rt[par][0:64, :], in_=ps[0:64, pb_, :], func=AF.Ln), waits=[a1, self._rt_free[par]])
            tb = ph.op("act", lambda e: e.activation(out=Rt[par][64:128, :], in_=ps[64:128, pa, :], func=AF.Ln), waits=[a0, self._rt_free[par]])
            b, bf = abanks.next()
            tsw = ph.op("pe", lambda e: e.matmul(ps[:, b, :], lhsT=self.swap_f[:], rhs=Rt[par][:], start=True, stop=True),
                        waits=[ta, tb, bf])
            self._rt_free[par] = tsw
            tcp = ph.op("act", lambda e: e.activation(out=Rs[par][:], in_=ps[:, b, :], func=AF.Exp, scale=-1.0),
                        waits=[tsw, self._rs2_free[par]])
            abanks.release(b, tcp)
            tg = ph.op("dve", lambda e: e.tensor_tensor(out=Rs[par][:], in0=Rs[par][:], in1=SG[:, c, cs0:cs0 + 512], op=ALU.mult),
                       waits=[tcp, g_last])
            to0 = ph.op("dve", lambda e: e.tensor_tensor(out=OT[0:64, c, cs0:cs0 + 512], in0=ps[0:64, pa, :],
                                                         in1=Rs[par][0:64, :], op=ALU.mult), waits=[tg])
            to1 = ph.op("dve", lambda e: e.tensor_tensor(out=OT[64:128, c, cs0:cs0 + 512], in0=ps[64:128, pb_, :],
                                                         in1=Rs[par][64:128, :], op=ALU.mult), waits=[tg])
            pvfree[pa] = to0
            pvfree[pb_] = to1
            self._rs2_free[par] = to1
            o_last = to1

        for i in range(n + LA):
            if i < n:
                w = work[i]
                c, Q, Qa, hh, t = w["c"], w["Q"], w["Qa"], w["hh"], w["t"]
                sl = c % 2
                if w["first_pair"]:
                    ktok[c] = ph.dma("sp", lambda e, c=c, sl=sl: e.dma_start(out=Kb[sl][:, 0:L], in_=self.KT_d[:, c, 0:L]), f"kb{sl}",
                                     waits=[kvfree[sl]])
                    tvr = ph.dma("sp", lambda e, c=c, sl=sl: e.dma_start(out=Vraw[sl][:, 0:ntk, :], in_=self.V_d[:, c, 0:ntk, :]),
                                 f"vb{sl}", waits=[kvfree[sl]])
                    for h2 in range(2):
                        off = 0 if h2 == 0 else 64
                        vtok[c] = ph.op("pool", lambda e, sl=sl, h2=h2, off=off: e.tensor_copy(
                            out=Vb[sl][:, 0:ntk, h2, off:off + 64], in_=Vraw[sl][:, 0:ntk, off:off + 64]),
                            waits=[tvr, tvo, kvfree[sl]])
                h = c * 2 + hh
                rows = slice(hh * 64, (hh + 1) * 64)
                r = t - Qa * 4
                c0 = max(r, 0) * 128
                cs0 = Q * 512
                b, bf = abanks.next()
                tqk = ph.op("pe", lambda e, b=b, t=t, c0=c0, rows=rows, sl=sl, c=c, cs0=cs0: e.matmul(
                    ps[:, b, c0:512], lhsT=Kb[sl][rows, t * 128:(t + 1) * 128], rhs=QT[rows, c, cs0 + c0:cs0 + 512],
                    start=True, stop=True), waits=[bf, ktok[c], q_last])
                s_ = i % NS
                tex = ph.op("act", lambda e, b=b, t=t, c0=c0, s_=s_, Q=Q, h=h: e.activation(
                    out=PT[s_][:, c0:512], in_=ps[:, b, c0:512], func=AF.Exp, bias=nb[:, Q, t, h:h + 1], scale=1.0),
                    waits=[tqk, pfree[s_], tnb])
                abanks.release(b, tex)
                tr_ = tex
                if r >= 0:
                    tr_ = ph.op("dve", lambda e, s_=s_, c0=c0: e.tensor_tensor(
                        out=PT[s_][:, c0:c0 + 128], in0=PT[s_][:, c0:c0 + 128], in1=self.trimask[:], op=ALU.mult), waits=[tex])
                tready[i] = (tr_, c0, s_)
            j = i - LA
            if j >= 0:
                w = work[j]
                c, Q, hh, t, nk, par = w["c"], w["Q"], w["hh"], w["t"], w["nk"], w["par"]
                sl = c % 2
                pb = 4 + 2 * par + hh
                tr_, c0, s_ = tready.pop(j)
                tpv = ph.op("pe", lambda e, pb=pb, t=t, c0=c0, s_=s_, sl=sl, hh=hh, nk=nk: e.matmul(
                    ps[:, pb, c0:512], lhsT=Vb[sl][:, t, hh, :], rhs=PT[s_][:, c0:512], start=(t == 0), stop=(t == nk - 1)),
                    waits=[tr_, vtok[c], pvfree[pb] if t == 0 else None])
                pfree[s_] = tpv
                if w["last_head"]:
                    acc[(w["g"], hh)] = tpv
                if w["last_pair"]:
                    kvfree[sl] = tpv
                if w["last_g"]:
                    pending.append((j + DELAY, c, Q, par, acc.pop((w["g"], 0)), acc.pop((w["g"], 1))))
                while pending and pending[0][0] <= j:
                    _, c_, Q_, par_, a0, a1 = pending.pop(0)
                    finalize(c_, Q_, par_, a0, a1)
        while pending:
            _, c_, Q_, par_, a0, a1 = pending.pop(0)
            finalize(c_, Q_, par_, a0, a1)
        banks = abanks
        if self.cfg.get("dbg") and st == 0:
            tdd = None
            for c in range(KC):
                for tt in range(NTT):
                    cs = slice(tt * 512, (tt + 1) * 512)
                    td = ph.op("act", lambda e, c=c, cs=cs: e.activation(out=raws[0][:], in_={'OT': OT, 'QT': QT, 'SG': SG, 'XN': xn}[self.cfg['dbg']][:, c, cs], func=AF.Copy), waits=[o_last, tdd])
                    tdd = ph.dma("sp", lambda e, c=c, cs=cs: e.dma_start(out=self.dbg[:, c, cs], in_=raws[0][:]), "dbg", waits=[td])
        tm = None
        for mo in range(KC):
            for tt in range(NTT):
                cs = slice(tt * 512, (tt + 1) * 512)
                b, bf = banks.next()
                for k in range(KC):
                    tm = ph.op("pe", lambda e, b=b, k=k, mo=mo, cs=cs: e.matmul(
                        ps[:, b, :], lhsT=wo[:, k, mo * 128:(mo + 1) * 128], rhs=OT[:, k, cs], start=(k == 0), stop=(k == KC - 1)),
                        waits=[bf, o_last, t_wo], sig=(k == KC - 1))
                ta = ph.op("dve", lambda e, b=b, mo=mo, cs=cs: e.tensor_tensor(
                    out=self.hT[:, mo, cs], in0=self.hT[:, mo, cs], in1=ps[:, b, :], op=ALU.add), waits=[tm])
                banks.release(b, ta)
        ph.emit()

    def ph_moe(self):
        I = self.I
        ph = self.phase("moe")
        self.norm_state()
        ps = ph.psum()
        banks = Banks(ps, range(8))
        xn = ph.alloc("xn", [128, KC, ST], BF16)
        sq = ph.alloc("sq", [128, KC, 512], BF16)
        rstd = [ph.alloc(f"rstd{i}", [128, 512], F32) for i in range(NTT)]
        hbuf = ph.alloc("hbuf", [128, D_EXP // 128, ST], BF16)
        self._silu = [ph.alloc(f"silu{i}", [128, 512], F32) for i in range(2)]
        self._silu_free = [None, None]
        self._h_free = None
        tmpb = [ph.alloc(f"tmpb{i}", [128, 512], F32) for i in range(2)]
        self._tmp_free = [None, None]
        self._tmp_i = 0
        lgT = ph.alloc("lgT", [8, ST], F32)
        lg = ph.alloc("lg", [128, NT, NEXP], F32)
        m1 = ph.alloc("m1", [128, NT], F32)
        m2 = ph.alloc("m2", [128, NT], F32)
        mk1 = ph.alloc("mk1", [128, NT, NEXP], F32)
        mk2 = ph.alloc("mk2", [128, NT, NEXP], F32)
        lg2 = ph.alloc("lg2", [128, NT, NEXP], F32)
        g1 = ph.alloc("g1", [128, NT], F32)
        g2 = ph.alloc("g2", [128, NT], F32)
        gates = ph.alloc("gates", [128, NT, NEXP], F32)
        gT = ph.alloc("gT", [8, ST], F32)
        gbc = [ph.alloc(f"gbc{i}", [128, ST], F32) for i in range(2)]
        ws = WStream(ph, "w", [128, KC, 512], 2)
        ws2 = WStream(ph, "w2", [128, D_EXP // 128, 256], 2)
        xtoks, rtoks = self.emit_norm(ph, ps, banks, 4, xn, sq, rstd)
        xall = xtoks[-1]
        tl = None
        for tt in range(NTT):
            cs = slice(tt * 512, (tt + 1) * 512)
            b, bf = banks.next()
            tm = None
            for k in range(KC):
                tm = ph.op("pe", lambda e, b=b, k=k, cs=cs: e.matmul(ps[0:8, b, :], lhsT=self.gwr[:, k, :], rhs=self.hT[:, k, cs],
                                                                     start=(k == 0), stop=(k == KC - 1)), waits=[bf], sig=(k == KC - 1))
            tl = ph.op("dve", lambda e, b=b, cs=cs, tt=tt: e.tensor_tensor(out=lgT[:, cs], in0=ps[0:8, b, :], in1=rstd[tt][0:8, :],
                                                                           op=ALU.mult), waits=[tm, rtoks[tt]])
            banks.release(b, tl)
        b, bf = banks.next()
        tp = None
        for t in range(NT):
            tp = ph.op("pe", lambda e, b=b, t=t: e.transpose(ps[:, b, t * 8:(t + 1) * 8], lgT[:, t * 128:(t + 1) * 128], self.ident[0:8, 0:8]),
                       waits=[bf, tl], sig=(t == NT - 1))
        t0 = ph.op("dve", lambda e, b=b: e.tensor_copy(out=lg[:], in_=ps[:, b, 0:NT * 8].rearrange("p (t x) -> p t x", t=NT)), waits=[tp])
        banks.release(b, t0)
        t1 = ph.op("dve", lambda e: e.tensor_reduce(out=m1[:], in_=lg[:], axis=AX.X, op=ALU.max), waits=[t0])
        tk1 = None
        for t in range(NT):
            tk1 = ph.op("dve", lambda e, t=t: e.tensor_scalar(out=mk1[:, t, :], in0=lg[:, t, :], scalar1=m1[:, t:t + 1], scalar2=None,
                                                              op0=ALU.is_equal), waits=[t1])
        t2 = ph.op("dve", lambda e: e.scalar_tensor_tensor(out=lg2[:], in0=mk1[:], scalar=-1e30, in1=lg[:], op0=ALU.mult, op1=ALU.add),
                   waits=[tk1])
        t3 = ph.op("dve", lambda e: e.tensor_reduce(out=m2[:], in_=lg2[:], axis=AX.X, op=ALU.max), waits=[t2])
        tk2 = None
        for t in range(NT):
            tk2 = ph.op("dve", lambda e, t=t: e.tensor_scalar(out=mk2[:, t, :], in0=lg2[:, t, :], scalar1=m2[:, t:t + 1], scalar2=None,
                                                              op0=ALU.is_equal), waits=[t3])
        t4 = ph.op("dve", lambda e: e.tensor_tensor(out=g1[:], in0=m1[:], in1=m2[:], op=ALU.subtract), waits=[t3])
        t5 = ph.op("act", lambda e: e.activation(out=g1[:], in_=g1[:], func=AF.Sigmoid), waits=[t4])
        t6 = ph.op("dve", lambda e: e.tensor_scalar(out=g2[:], in0=g1[:], scalar1=-1.0, scalar2=1.0, op0=ALU.mult, op1=ALU.add), waits=[t5])
        tg = None
        for t in range(NT):
            ta = ph.op("dve", lambda e, t=t: e.tensor_scalar(out=mk1[:, t, :], in0=mk1[:, t, :], scalar1=g1[:, t:t + 1], scalar2=None,
                                                             op0=ALU.mult), waits=[t5, tk2])
            tb = ph.op("dve", lambda e, t=t: e.scalar_tensor_tensor(out=gates[:, t, :], in0=mk2[:, t, :], scalar=g2[:, t:t + 1],
                                                                    in1=mk1[:, t, :], op0=ALU.mult, op1=ALU.add), waits=[ta, t6])
            tg = tb
        tgt = None
        for half in range(2):
            b, bf = banks.next()
            tp = None
            for q in range(4):
                t = half * 4 + q
                tp = ph.op("pe", lambda e, b=b, q=q, t=t: e.transpose(ps[0:8, b, q * 128:(q + 1) * 128], gates[:, t, :], self.ident[:]),
                           waits=[bf, tg], sig=(q == 3))
            tgt = ph.op("dve", lambda e, b=b, half=half: e.tensor_copy(out=gT[:, half * 512:(half + 1) * 512], in_=ps[0:8, b, :]), waits=[tp])
            banks.release(b, tgt)
        gfree = [None, None]
        for ex in range(NEXP):
            gs = ex % 2
            tgb = None
            for tt in range(NTT):
                cs = slice(tt * 512, (tt + 1) * 512)
                b, bf = banks.next()
                tm = ph.op("pe", lambda e, b=b, ex=ex, cs=cs: e.matmul(ps[:, b, :], lhsT=self.sel8[:, ex, :], rhs=gT[:, cs], start=True, stop=True),
                           waits=[bf, tgt])
                tgb = ph.op("act", lambda e, b=b, gs=gs, cs=cs: e.activation(out=gbc[gs][:, cs], in_=ps[:, b, :], func=AF.Copy),
                            waits=[tm, gfree[gs]])
                banks.release(b, tgb)
            last = self.emit_swiglu(ph, ps, banks, xn, xall, I["m_w_in"][0, ex], I["m_w_out"][0, ex], D_EXP, hbuf, ws, ws2,
                                    gate_bc=gbc[gs], gate_tok=tgb, tmpb=tmpb,
                                    next_w_in=(I["m_w_in"][0, ex + 1] if ex + 1 < NEXP else None))
            gfree[gs] = self._tmp_free[(self._tmp_i - 1) % 2]
        ph.emit()

    def build(self):
        cfg = self.cfg
        self.declare()
        with ExitStack() as es:
            self.alloc_persist(es)
            self._rt_free = [None, None]
            self._rs2_free = [None, None]
            self.ph_setup()
            for seq in range(cfg.get("nseq", NSEQ)):
                for st in range(cfg.get("nst", SEQ // ST)):
                    self._rt_free = [None, None]
                    self._rs2_free = [None, None]
                    self.ph_load(seq, st)
                    if cfg.get("mixa", True):
                        self.ph_mixer_a()
                    if cfg.get("ffn", True):
                        self.ph_ffn()
                    if cfg.get("kv", True):
                        self.ph_kv(st)
                    if cfg.get("mixb", True):
                        self._rt_free = [None, None]
                        self._rs2_free = [None, None]
                        self.ph_mixer_b(st)
                    if cfg.get("moe", True):
                        self.ph_moe()
                    self.ph_store(seq, st)
        return self.nc


INPUT_NAMES = ["x", "a_norm_g", "a_w_in", "a_v_norm_g", "a_w_spatial", "a_b_spatial", "a_w_out",
               "f_norm_g", "f_w_in", "f_w_out", "kv_norm_g", "kv_w", "kv_b_f", "k_norm_g",
               "b_norm_g", "b_w_in", "q_norm_g", "b_w_out", "m_norm_g", "m_w_router", "m_w_in", "m_w_out"]


def kernel(**inputs):
    n = 8
    k = Kern({})
    nc = k.build()
    shared = {nm: np.ascontiguousarray(np.asarray(inputs[nm], dtype=np.float32)) for nm in INPUT_NAMES if nm != "x"}
    x = np.asarray(inputs["x"], dtype=np.float32)
    in_maps = []
    for i in range(n):
        m = dict(shared)
        m["x"] = np.ascontiguousarray(x[i * NSEQ:(i + 1) * NSEQ])
        in_maps.append(m)
    res = run_bass_kernel_spmd(nc, in_maps, core_ids=list(range(n)))
    return np.concatenate([r["out"] for r in res.results], axis=0)
```

```python
from contextlib import ExitStack

import numpy as np
import concourse.bass as bass
import concourse.mybir as mybir
from concourse.bass_utils import run_bass_kernel_spmd

F32 = mybir.dt.float32
BF16 = mybir.dt.bfloat16
AF = mybir.ActivationFunctionType
ALU = mybir.AluOpType
AX = mybir.AxisListType

ENG = ("pe", "act", "dve", "pool", "sp")
D = 1024
KC = 8
ST = 1024
NTT = ST // 512
NT = ST // 128
SEQ = 2048
NSEQ = 2
A_HALF = 3072
D_FF = 2816
D_EXP = 3584
NEXP = 8
EPS = 1e-6
SM_BOUND = 12.0


class Phase:
    def __init__(self, nc, name):
        self.nc = nc
        self.name = name
        self.es = ExitStack()
        self.ops = {e: [] for e in ENG}
        self.sem = {}
        self.cnt = {}
        self.all_sems = []
        for e in ENG:
            self.sem[e] = nc.alloc_semaphore(name=f"{name}_{e}")
            self.all_sems.append(self.sem[e])
            self.cnt[e] = 0
        self.dsem = {}
        self.waited = {e: {} for e in ENG}

    def alloc(self, name, shape, dt):
        return self.es.enter_context(self.nc.sbuf_tensor(f"{self.name}_{name}", shape, dt))

    def psum(self, name="ps"):
        return self.es.enter_context(self.nc.psum_tensor(f"{self.name}_{name}", [128, 8, 512], F32))

    def _waits(self, eng, waits):
        out = []
        for w in waits:
            if w is None:
                continue
            sem, val = w
            key = sem.num
            if self.waited[eng].get(key, 0) >= val:
                continue
            self.waited[eng][key] = val
            out.append((sem, val))
        return out

    def op(self, eng, fn, waits=(), sig=True):
        ws = self._waits(eng, waits)
        tok = None
        inc = None
        if sig:
            self.cnt[eng] += 1
            tok = (self.sem[eng], self.cnt[eng])
            inc = (self.sem[eng], 1)
        self.ops[eng].append((ws, fn, inc))
        return tok

    def dma(self, eng, fn, key, waits=()):
        if key not in self.dsem:
            s = self.nc.alloc_semaphore(name=f"{self.name}_d{len(self.dsem)}")
            self.all_sems.append(s)
            self.dsem[key] = [s, 0, eng]
        d = self.dsem[key]
        assert d[2] == eng
        d[1] += 16
        ws = self._waits(eng, waits)
        self.ops[eng].append((ws, fn, (d[0], 16)))
        return (d[0], d[1])

    def emit(self):
        nc = self.nc
        for key, (s, v, eng) in self.dsem.items():
            ws = self._waits(eng, [(s, v)])
            if ws:
                self.ops[eng].append((ws, None, None))

        def run(engobj, name):
            for ws, fn, inc in self.ops[name]:
                for sem, val in ws:
                    engobj.wait_ge(sem, val)
                if fn is None:
                    continue
                ins = fn(engobj)
                if inc is not None:
                    ins.then_inc(inc[0], inc[1])

        with nc.Block() as block:
            block.tensor(lambda e: run(e, "pe"))
            block.scalar(lambda e: run(e, "act"))
            block.vector(lambda e: run(e, "dve"))
            block.gpsimd(lambda e: run(e, "pool"))
            block.sync(lambda e: run(e, "sp"))
        nc.all_engine_barrier()
        nc.clear_and_free_semaphores(self.all_sems)
        nc.all_engine_barrier()
        self.es.close()


class Banks:
    def __init__(self, ps, ids):
        self.ps = ps
        self.ids = list(ids)
        self.free = {b: None for b in self.ids}
        self.i = 0

    def next(self):
        b = self.ids[self.i % len(self.ids)]
        self.i += 1
        return b, self.free[b]

    def release(self, b, tok):
        self.free[b] = tok


class WStream:
    def __init__(self, ph, name, shape, nbuf, dt=BF16, eng="pool", bufs=None, free0=None):
        self.ph = ph
        self.name = name
        self.eng = eng
        self.bufs = bufs if bufs is not None else [ph.alloc(f"{name}{i}", shape, dt) for i in range(nbuf)]
        self.free = [free0] * len(self.bufs)
        self.i = 0

    def load(self, fns):
        slot = self.i % len(self.bufs)
        self.i += 1
        buf = self.bufs[slot]
        toks = []
        for j, fn in enumerate(fns):
            toks.append(self.ph.dma(self.eng, (lambda e, fn=fn, buf=buf: fn(e, buf)), f"{self.name}{slot}",
                                    waits=[self.free[slot]]))
        return slot, buf, toks[-1:]

    def release(self, slot, tok):
        self.free[slot] = tok


def bcast_mid(ap2d, rep):
    a = ap2d.ap
    return bass.AP(ap2d.tensor, ap2d.offset, [list(a[0]), [0, rep], list(a[1])])


def bcast_last(ap2d, n):
    a = ap2d.ap
    return bass.AP(ap2d.tensor, ap2d.offset, [list(a[0]), [0, n]])


class Kern:
    def __init__(self, cfg):
        self.cfg = cfg
        self.nc = bass.Bass("TRN2", target_bir_lowering=False)
        self.pid = 0

    def phase(self, name):
        self.pid += 1
        return Phase(self.nc, f"p{self.pid}{name}")

    def declare(self):
        nc = self.nc
        I = {}

        def inp(name, shape):
            I[name] = nc.dram_tensor(name, list(shape), F32, kind="ExternalInput").ap()

        inp("x", (NSEQ, SEQ, D))
        inp("a_norm_g", (1, D)); inp("a_w_in", (1, D, 2 * A_HALF)); inp("a_v_norm_g", (1, A_HALF))
        inp("a_w_spatial", (1, 8, 128, 128)); inp("a_b_spatial", (1, 8, 128)); inp("a_w_out", (1, A_HALF, D))
        inp("f_norm_g", (1, D)); inp("f_w_in", (1, D, 2 * D_FF)); inp("f_w_out", (1, D_FF, D))
        inp("kv_norm_g", (D,)); inp("kv_w", (D, 2 * D + 16)); inp("kv_b_f", (16,)); inp("k_norm_g", (64,))
        inp("b_norm_g", (1, D)); inp("b_w_in", (1, D, 2 * D)); inp("q_norm_g", (1, 64)); inp("b_w_out", (1, D, D))
        inp("m_norm_g", (1, D)); inp("m_w_router", (1, D, NEXP)); inp("m_w_in", (1, NEXP, D, 2 * D_EXP))
        inp("m_w_out", (1, NEXP, D_EXP, D))
        self.I = I
        self.out = nc.dram_tensor("out", [NSEQ, SEQ, D], F32, kind="ExternalOutput").ap()
        if self.cfg.get("dbg"):
            self.dbg = nc.dram_tensor("dbg", [128, KC, ST], F32, kind="ExternalOutput").ap()
        self.KT_d = nc.dram_tensor("kt_scr", [128, KC, SEQ], BF16).ap()
        self.V_d = nc.dram_tensor("v_scr", [128, KC, SEQ // 128, 128], BF16).ap()

    def alloc_persist(self, es):
        nc = self.nc

        def sb(name, shape, dt=F32):
            return es.enter_context(nc.sbuf_tensor(name, shape, dt))

        self.hT = sb("hT", [128, KC, ST])
        self.ident = sb("ident", [128, 128])
        self.ones_bf = sb("ones_bf", [128, 128], BF16)
        self.blk_bf = sb("blk_bf", [128, 128], BF16)
        self.tri_f = sb("tri_f", [128, 128])
        self.ones_f = sb("ones_f", [128, 128])
        self.trimask = sb("trimask", [128, 128], BF16)
        self.swap_f = sb("swap_f", [128, 128])
        self.gains = sb("gains", [128, 5, KC])
        self.vgain = sb("vgain", [128, 24])
        self.kqg = sb("kqg", [128, 2])
        self.biasbc = sb("biasbc", [128, 8, 128])
        self.bf_bc = sb("bf_bc", [128, 16])
        self.wTsp = sb("wTsp", [128, 8, 128])
        self.gwr = sb("gwr", [128, KC, NEXP])
        self.sel8 = sb("sel8", [8, NEXP, 128])
        self.epsc = sb("epsc", [128, 1])
        self.onec = sb("onec", [128, 1])
        self.logf = sb("logf", [128, SEQ // 128, 16])
        self.Fcum = sb("Fcum", [128, SEQ // 128, 16])

    def ph_setup(self):
        I = self.I
        ph = self.phase("setup")
        nc = self.nc
        wsp = ph.alloc("wsp", [128, 8, 128], F32)
        rt = ph.alloc("rt", [128, KC, NEXP], F32)
        ps = ph.psum()
        toks = []
        with nc.allow_non_contiguous_dma(reason="tiny one-time parameter loads"):
            def ld(out, in_, key):
                return ph.dma("sp", lambda e: e.dma_start(out=out, in_=in_, allow_slow_non_contiguous=True), key)
            for i, nm in enumerate(["a_norm_g", "f_norm_g", "kv_norm_g", "b_norm_g", "m_norm_g"]):
                src = I[nm] if nm == "kv_norm_g" else I[nm][0]
                toks.append(ld(self.gains[:, i, :], src.rearrange("(c p) -> p c", p=128), "g"))
            toks.append(ld(self.vgain[:], I["a_v_norm_g"][0].rearrange("(c p) -> p c", p=128), "g"))
            for half in range(2):
                toks.append(ld(self.kqg[half * 64:(half + 1) * 64, 0:1], I["k_norm_g"].rearrange("(p o) -> p o", o=1), "g"))
                toks.append(ld(self.kqg[half * 64:(half + 1) * 64, 1:2], I["q_norm_g"][0].rearrange("(p o) -> p o", o=1), "g"))
            toks.append(ld(self.biasbc[:], bass.AP(I["a_b_spatial"].tensor, 0, [[0, 128], [128, 8], [1, 128]]), "g"))
            toks.append(ld(self.bf_bc[:], bass.AP(I["kv_b_f"].tensor, 0, [[0, 128], [1, 16]]), "g"))
            toks.append(ld(wsp[:], I["a_w_spatial"][0].rearrange("g i j -> i g j"), "g"))
            toks.append(ld(rt[:], I["m_w_router"][0].rearrange("(c p) e -> p c e", p=128), "g"))
        tl = toks[-1]
        pl = []
        def P(fn, waits=()):
            t = ph.op("pool", fn, waits=waits)
            pl.append(t)
            return t
        P(lambda e: e.memset(self.ident[:], 0.0))
        t_id = P(lambda e: e.affine_select(out=self.ident[:], in_=self.ident[:], pattern=[[-1, 128]],
                                           compare_op=ALU.not_equal, fill=1.0, base=0, channel_multiplier=1), waits=[pl[-1]])
        P(lambda e: e.memset(self.ones_bf[:], 1.0))
        P(lambda e: e.memset(self.epsc[:], EPS))
        P(lambda e: e.memset(self.onec[:], 1.0))
        P(lambda e: e.memset(self.ones_f[:], 1.0))
        P(lambda e: e.memset(self.blk_bf[:], 0.0))
        P(lambda e: e.memset(self.blk_bf[0:64, 0:64], 1.0), waits=[pl[-1]])
        P(lambda e: e.memset(self.blk_bf[64:128, 64:128], 1.0), waits=[pl[-2]])
        P(lambda e: e.memset(self.tri_f[:], 1.0))
        P(lambda e: e.affine_select(out=self.tri_f[:], in_=self.tri_f[:], pattern=[[1, 128]],
                                    compare_op=ALU.is_ge, fill=0.0, base=0, channel_multiplier=-1), waits=[pl[-1]])
        t_tri = pl[-1]
        P(lambda e: e.tensor_copy(out=self.trimask[:], in_=self.tri_f[:]), waits=[t_tri])
        P(lambda e: e.memset(self.swap_f[:], 0.0))
        t0 = pl[-1]
        P(lambda e: e.affine_select(out=self.swap_f[:, 0:64], in_=self.swap_f[:, 0:64], pattern=[[-1, 64]],
                                    compare_op=ALU.not_equal, fill=1.0, base=-64, channel_multiplier=1), waits=[t0])
        P(lambda e: e.affine_select(out=self.swap_f[:, 64:128], in_=self.swap_f[:, 64:128], pattern=[[-1, 64]],
                                    compare_op=ALU.not_equal, fill=1.0, base=0, channel_multiplier=1), waits=[t0])
        P(lambda e: e.memset(self.sel8[:], 0.0))
        P(lambda e: e.affine_select(out=self.sel8[:], in_=self.sel8[:], pattern=[[-1, NEXP], [0, 128]],
                                    compare_op=ALU.not_equal, fill=1.0, base=0, channel_multiplier=1), waits=[pl[-1]])
        t_pool = pl[-1]
        t1 = ph.op("dve", lambda e: e.tensor_scalar(out=self.kqg[:, 1:2], in0=self.kqg[:, 1:2], scalar1=0.125, scalar2=None,
                                                    op0=ALU.mult), waits=[tl])
        tg = None
        for c in range(KC):
            tg = ph.op("dve", lambda e, c=c: e.tensor_scalar(out=self.gwr[:, c, :], in0=rt[:, c, :],
                                                             scalar1=self.gains[:, 4, c:c + 1], scalar2=None, op0=ALU.mult),
                       waits=[tl])
        tp = None
        for g in range(8):
            tp = ph.op("pe", lambda e, g=g: e.transpose(ps[:, g // 4, (g % 4) * 128:(g % 4 + 1) * 128], wsp[:, g, :], self.ident[:]),
                       waits=[tl, t_id])
        tc = None
        for b in range(2):
            tc = ph.op("dve", lambda e, b=b: e.tensor_copy(out=self.wTsp[:, b * 4:(b + 1) * 4, :],
                                                           in_=ps[:, b, :].rearrange("p (g i) -> p g i", g=4)), waits=[tp])
        ph.op("dve", lambda e: e.memset(self.wTsp[64:128, :, 0:64], 0.0), waits=[tc])
        ph.emit()

    def ph_load(self, seq, st):
        ph = self.phase("load")
        xt = [ph.alloc(f"xt{i}", [128, D], F32) for i in range(2)]
        ps = ph.psum()
        banks = Banks(ps, range(8))
        xfree = [None, None]
        for t in range(NT):
            sl = t % 2
            r0 = st * ST + t * 128
            tl = ph.dma("sp", lambda e, sl=sl, r0=r0: e.dma_start(out=xt[sl][:], in_=self.I["x"][seq, r0:r0 + 128, :]),
                        f"x{sl}", waits=[xfree[sl]])
            for half in range(2):
                b, bf = banks.next()
                tp = None
                for j in range(4):
                    c = half * 4 + j
                    tp = ph.op("pe", lambda e, b=b, j=j, c=c, sl=sl: e.transpose(ps[:, b, j * 128:(j + 1) * 128],
                                                                                 xt[sl][:, c * 128:(c + 1) * 128], self.ident[:]),
                               waits=[tl, bf], sig=(j == 3))
                eng = "act" if half == 0 else "dve"
                if eng == "act":
                    tcp = ph.op("act", lambda e, b=b, half=half, t=t: e.activation(
                        out=self.hT[:, half * 4:(half + 1) * 4, t * 128:(t + 1) * 128],
                        in_=ps[:, b, :].rearrange("p (c i) -> p c i", c=4), func=AF.Copy), waits=[tp])
                else:
                    tcp = ph.op("dve", lambda e, b=b, half=half, t=t: e.tensor_copy(
                        out=self.hT[:, half * 4:(half + 1) * 4, t * 128:(t + 1) * 128],
                        in_=ps[:, b, :].rearrange("p (c i) -> p c i", c=4)), waits=[tp])
                banks.release(b, tcp)
                if half == 1:
                    xfree[sl] = tp
        ph.emit()

    def ph_store(self, seq, st):
        ph = self.phase("store")
        ot = [ph.alloc(f"ot{i}", [128, D], F32) for i in range(2)]
        ps = ph.psum()
        banks = Banks(ps, range(8))
        ofree = [None, None]
        for t in range(NT):
            sl = t % 2
            r0 = st * ST + t * 128
            cps = []
            for half in range(2):
                b, bf = banks.next()
                tp = None
                for j in range(4):
                    c = half * 4 + j
                    tp = ph.op("pe", lambda e, b=b, j=j, c=c, t=t: e.transpose(ps[:, b, j * 128:(j + 1) * 128],
                                                                               self.hT[:, c, t * 128:(t + 1) * 128], self.ident[:]),
                               waits=[bf], sig=(j == 3))
                if half == 0:
                    tcp = ph.op("act", lambda e, b=b, sl=sl: e.activation(out=ot[sl][:, 0:512], in_=ps[:, b, :], func=AF.Copy),
                                waits=[tp, ofree[sl]])
                else:
                    tcp = ph.op("dve", lambda e, b=b, sl=sl: e.tensor_copy(out=ot[sl][:, 512:1024], in_=ps[:, b, :]),
                                waits=[tp, ofree[sl]])
                banks.release(b, tcp)
                cps.append(tcp)
            ofree[sl] = ph.dma("sp", lambda e, sl=sl, r0=r0: e.dma_start(out=self.out[seq, r0:r0 + 128, :], in_=ot[sl][:]),
                               f"o{sl}", waits=cps)
        ph.emit()

    def emit_norm(self, ph, ps, banks, gidx, xn, sq, rstd_bufs, want_rstd=False):
        toks = []
        rstd_toks = []
        for tt in range(NTT):
            cs = slice(tt * 512, (tt + 1) * 512)
            b, bf = banks.next()
            tm = None
            for c in range(KC):
                eng = "act" if c % 2 == 0 else "dve"
                if eng == "act":
                    tsq = ph.op("act", lambda e, c=c, cs=cs: e.activation(out=sq[:, c, :], in_=self.hT[:, c, cs], func=AF.Square),
                                waits=[self._sq_free.get(c)])
                else:
                    tsq = ph.op("dve", lambda e, c=c, cs=cs: e.tensor_tensor(out=sq[:, c, :], in0=self.hT[:, c, cs], in1=self.hT[:, c, cs],
                                                                            op=ALU.mult), waits=[self._sq_free.get(c)])
                tm = ph.op("pe", lambda e, c=c, b=b: e.matmul(ps[:, b, :], lhsT=self.ones_bf[:], rhs=sq[:, c, :],
                                                              start=(c == 0), stop=(c == KC - 1)), waits=[tsq, bf])
                self._sq_free[c] = tm
            rs = rstd_bufs[tt]
            t1, t2 = self.emit_rsqrt(ph, rs[:], ps[:, b, :], 1.0 / D, [tm, self._rs_free.get(tt)])
            banks.release(b, t1)
            rstd_toks.append(t2)
            last = []
            for c in range(KC):
                tx = ph.op("dve", lambda e, c=c, cs=cs, rs=rs: e.scalar_tensor_tensor(
                    out=xn[:, c, cs], in0=self.hT[:, c, cs], scalar=self.gains[:, gidx, c:c + 1], in1=rs[:],
                    op0=ALU.mult, op1=ALU.mult), waits=[t2, self._xn_free])
                last.append(tx)
            toks.append(last[-1])
            self._rs_free[tt] = last[-1]
        return toks, rstd_toks

    def emit_rsqrt(self, ph, out_ap, in_ap, scale, waits):
        n = out_ap.shape[0]
        t1 = ph.op("act", lambda e: e.activation(out=out_ap, in_=in_ap, func=AF.Ln, bias=self.epsc[0:n, :], scale=scale), waits=waits)
        t2 = ph.op("act", lambda e: e.activation(out=out_ap, in_=out_ap, func=AF.Exp, scale=-0.5), waits=[t1])
        return t1, t2

    def norm_state(self):
        self._sq_free = {}
        self._rs_free = {}
        self._xn_free = None

    def ph_mixer_a(self):
        I = self.I
        ph = self.phase("mixA")
        self.norm_state()
        ps = ph.psum()
        banks = Banks(ps, range(8))
        xn = ph.alloc("xn", [128, KC, ST], BF16)
        rstd = [ph.alloc(f"rstd{i}", [128, 512], F32) for i in range(NTT)]
        big1 = ph.alloc("big1", [128, NT * A_HALF], BF16)
        big2 = ph.alloc("big2", [128, 24 * ST], BF16)
        vg = big1[:].rearrange("p (t n) -> p t n", t=NT)
        pT = big2[:].rearrange("p (m t) -> p m t", m=24)
        sq = big2[:, 0:KC * 512].rearrange("p (c n) -> p c n", c=KC)
        junk = big2[:, 8192:8192 + A_HALF]
        wts = ph.alloc("wts", [128, NT, 8 * 128], BF16)
        ssv = ph.alloc("ssv", [128, NT], F32)
        rsv = ph.alloc("rsv", [128, NT], F32)
        usb = [ph.alloc(f"usb{i}", [128, 512], BF16) for i in range(4)]
        t1b = [ph.alloc(f"t1b{i}", [128, 512], F32) for i in range(2)]
        ws = WStream(ph, "w", [128, KC, 512], 2)
        w_in = I["a_w_in"][0]
        w_out = I["a_w_out"][0]

        xtoks, _ = self.emit_norm(ph, ps, banks, 0, xn, sq, rstd)
        xall = xtoks[-1]

        def load_in(col0):
            return ws.load([lambda e, buf, col0=col0: e.dma_start(
                out=buf[:], in_=w_in[:, col0:col0 + 512].rearrange("(k p) n -> p k n", p=128))])

        pend = load_in(A_HALF)
        for n in range(6):
            slot, wb, wt = pend
            if n + 1 < 6:
                pend = load_in(A_HALF + (n + 1) * 512)
            else:
                pend = load_in(0)
            tm = None
            for t in range(NT):
                b, bf = banks.next()
                for k in range(KC):
                    tm = ph.op("pe", lambda e, b=b, k=k, t=t, wb=wb: e.matmul(
                        ps[:, b, :], lhsT=xn[:, k, t * 128:(t + 1) * 128], rhs=wb[:, k, :], start=(k == 0), stop=(k == KC - 1)),
                        waits=[xall, bf] + wt, sig=(k == KC - 1))
                tg = ph.op("act", lambda e, b=b, t=t, n=n: e.activation(out=vg[:, t, n * 512:(n + 1) * 512], in_=ps[:, b, :],
                                                                        func=AF.Gelu), waits=[tm])
                banks.release(b, tg)
                vg_last = tg
            ws.release(slot, tm)
        tz = ph.op("dve", lambda e: e.memset(ssv[:], 0.0))
        tsq = None
        for t in range(NT):
            tsq = ph.op("act", lambda e, t=t: e.activation(out=junk, in_=vg[:, t, :], func=AF.Square,
                                                           accum_out=ssv[:, t:t + 1]), waits=[vg_last, tz, tsq])
        _, tr = self.emit_rsqrt(ph, rsv[:], ssv[:], 1.0 / A_HALF, [tsq])
        tw = None
        for t in range(NT):
            tw = ph.op("dve", lambda e, t=t: e.tensor_scalar(out=wts[:, t, :], in0=self.wTsp[:].rearrange("p g i -> p (g i)"),
                                                             scalar1=rsv[:, t:t + 1], scalar2=None, op0=ALU.mult), waits=[tr])
        ufree = [None] * 4
        t1free = [None] * 2
        ui = 0
        ti = 0
        for piece in range(6):
            slot, wb, wt = pend
            if piece + 1 < 6:
                pend = load_in((piece + 1) * 512)
            tlast = None
            for mm in range(4):
                m = piece * 4 + mm
                g = m // 3
                for tt in range(NTT):
                    cs = slice(tt * 512, (tt + 1) * 512)
                    bu, buf_ = banks.next()
                    tm = None
                    for k in range(KC):
                        tm = ph.op("pe", lambda e, bu=bu, k=k, mm=mm, cs=cs, wb=wb: e.matmul(
                            ps[:, bu, :], lhsT=wb[:, k, mm * 128:(mm + 1) * 128], rhs=xn[:, k, cs],
                            start=(k == 0), stop=(k == KC - 1)), waits=[buf_] + wt, sig=(k == KC - 1))
                    tlast = tm
                    us = ui % 4
                    ui += 1
                    tu = ph.op("act", lambda e, bu=bu, us=us: e.activation(out=usb[us][:], in_=ps[:, bu, :], func=AF.Gelu),
                               waits=[tm, ufree[us]])
                    banks.release(bu, tu)
                    bs, bsf = banks.next()
                    tsm = None
                    for q in range(4):
                        t = tt * 4 + q
                        tsm = ph.op("pe", lambda e, bs=bs, q=q, t=t, m=m, g=g: e.matmul(
                            ps[:, bs, q * 128:(q + 1) * 128], lhsT=vg[:, t, m * 128:(m + 1) * 128],
                            rhs=wts[:, t, g * 128:(g + 1) * 128], start=True, stop=True), waits=[bsf, tw], sig=(q == 3))
                    tlast = tsm
                    t1s = ti % 2
                    ti += 1
                    ta = ph.op("dve", lambda e, bs=bs, m=m, g=g, t1s=t1s: e.scalar_tensor_tensor(
                        out=t1b[t1s][:].rearrange("p (r i) -> p r i", r=4), in0=ps[:, bs, :].rearrange("p (r i) -> p r i", r=4),
                        scalar=self.vgain[:, m:m + 1], in1=bcast_mid(self.biasbc[:, g, :], 4), op0=ALU.mult, op1=ALU.add),
                        waits=[tsm, t1free[t1s]])
                    banks.release(bs, ta)
                    tb = ph.op("dve", lambda e, m=m, cs=cs, t1s=t1s, us=us: e.tensor_tensor(
                        out=pT[:, m, cs], in0=t1b[t1s][:], in1=usb[us][:], op=ALU.mult), waits=[ta, tu])
                    ufree[us] = tb
                    t1free[t1s] = tb
                    p_last = tb
            ws.release(slot, tlast)
        ws2 = WStream(ph, "w2", None, 2, bufs=[big1[:, i * 6144:(i + 1) * 6144].rearrange("p (k n) -> p k n", k=24) for i in range(2)],
                      free0=tlast)

        def load_out(c0):
            return ws2.load([lambda e, buf, c0=c0: e.dma_start(
                out=buf, in_=w_out[:, c0:c0 + 256].rearrange("(k p) n -> p k n", p=128))])
        pend2 = load_out(0)
        for piece in range(4):
            slot, wb, wt = pend2
            if piece + 1 < 4:
                pend2 = load_out((piece + 1) * 256)
            tm = None
            for mm in range(2):
                mo = piece * 2 + mm
                for tt in range(NTT):
                    cs = slice(tt * 512, (tt + 1) * 512)
                    b, bf = banks.next()
                    for k in range(24):
                        tm = ph.op("pe", lambda e, b=b, k=k, mm=mm, cs=cs, wb=wb: e.matmul(
                            ps[:, b, :], lhsT=wb[:, k, mm * 128:(mm + 1) * 128], rhs=pT[:, k, cs],
                            start=(k == 0), stop=(k == 23)), waits=[bf, p_last] + wt, sig=(k == 23))
                    ta = ph.op("dve", lambda e, b=b, mo=mo, cs=cs: e.tensor_tensor(
                        out=self.hT[:, mo, cs], in0=self.hT[:, mo, cs], in1=ps[:, b, :], op=ALU.add), waits=[tm])
                    banks.release(b, ta)
            ws2.release(slot, tm)
        ph.emit()

    def emit_swiglu(self, ph, ps, banks, xn, xall, w_in, w_out, dff, hbuf, ws, ws2, gate_bc=None, gate_tok=None, tmpb=None,
                    next_w_in=None):
        nj = dff // 128
        npiece = nj // 2

        def load_in(p, w_in=w_in):
            c0 = p * 256
            return ws.load([
                lambda e, buf, c0=c0: e.dma_start(out=buf[:, :, 0:256], in_=w_in[:, c0:c0 + 256].rearrange("(k p) n -> p k n", p=128)),
                lambda e, buf, c0=c0: e.dma_start(out=buf[:, :, 256:512],
                                                  in_=w_in[:, dff + c0:dff + c0 + 256].rearrange("(k p) n -> p k n", p=128)),
            ])

        def load_out(c0):
            return ws2.load([lambda e, buf, c0=c0: e.dma_start(
                out=buf[:, 0:nj, :], in_=w_out[:, c0:c0 + 256].rearrange("(k p) n -> p k n", p=128))])
        pre = getattr(self, "_pre_in", None)
        self._pre_in = None
        pend = pre if pre is not None else load_in(0)
        pend2 = None
        si = 0
        h_last = None
        for p in range(npiece):
            slot, wb, wt = pend
            if p + 1 < npiece:
                pend = load_in(p + 1)
            if p == 0:
                pend2 = load_out(0)
            tlast = None
            for jj in range(2):
                j = p * 2 + jj
                for tt in range(NTT):
                    cs = slice(tt * 512, (tt + 1) * 512)
                    ba, baf = banks.next()
                    bb, bbf = banks.next()
                    ta_ = tb_ = None
                    for k in range(KC):
                        ta_ = ph.op("pe", lambda e, ba=ba, k=k, jj=jj, cs=cs, wb=wb: e.matmul(
                            ps[:, ba, :], lhsT=wb[:, k, jj * 128:(jj + 1) * 128], rhs=xn[:, k, cs],
                            start=(k == 0), stop=(k == KC - 1)), waits=[baf, xall] + wt, sig=(k == KC - 1))
                    for k in range(KC):
                        tb_ = ph.op("pe", lambda e, bb=bb, k=k, jj=jj, cs=cs, wb=wb: e.matmul(
                            ps[:, bb, :], lhsT=wb[:, k, 256 + jj * 128:256 + (jj + 1) * 128], rhs=xn[:, k, cs],
                            start=(k == 0), stop=(k == KC - 1)), waits=[bbf], sig=(k == KC - 1))
                    tlast = tb_
                    s = si % 2
                    si += 1
                    tsl = ph.op("act", lambda e, ba=ba, s=s: e.activation(out=self._silu[s][:], in_=ps[:, ba, :], func=AF.Silu),
                                waits=[ta_, self._silu_free[s]])
                    banks.release(ba, tsl)
                    th = ph.op("dve", lambda e, bb=bb, s=s, j=j, cs=cs: e.tensor_tensor(
                        out=hbuf[:, j, cs], in0=self._silu[s][:], in1=ps[:, bb, :], op=ALU.mult),
                        waits=[tsl, tb_, self._h_free])
                    banks.release(bb, th)
                    self._silu_free[s] = th
                    h_last = th
            ws.release(slot, tlast)
        if next_w_in is not None:
            self._pre_in = load_in(0, next_w_in)
        tm = None
        for piece in range(4):
            slot, wb, wt = pend2
            if piece + 1 < 4:
                pend2 = load_out((piece + 1) * 256)
            for mm in range(2):
                mo = piece * 2 + mm
                for tt in range(NTT):
                    cs = slice(tt * 512, (tt + 1) * 512)
                    b, bf = banks.next()
                    for k in range(nj):
                        tm = ph.op("pe", lambda e, b=b, k=k, mm=mm, cs=cs, wb=wb: e.matmul(
                            ps[:, b, :], lhsT=wb[:, k, mm * 128:(mm + 1) * 128], rhs=hbuf[:, k, cs],
                            start=(k == 0), stop=(k == nj - 1)), waits=[bf, h_last] + wt, sig=(k == nj - 1))
                    if gate_bc is None:
                        ta = ph.op("dve", lambda e, b=b, mo=mo, cs=cs: e.tensor_tensor(
                            out=self.hT[:, mo, cs], in0=self.hT[:, mo, cs], in1=ps[:, b, :], op=ALU.add), waits=[tm])
                        banks.release(b, ta)
                    else:
                        s = self._tmp_i % 2
                        self._tmp_i += 1
                        tq = ph.op("dve", lambda e, b=b, cs=cs, s=s: e.tensor_tensor(
                            out=tmpb[s][:], in0=ps[:, b, :], in1=gate_bc[:, cs], op=ALU.mult),
                            waits=[tm, gate_tok, self._tmp_free[s]])
                        banks.release(b, tq)
                        ta = ph.op("dve", lambda e, mo=mo, cs=cs, s=s: e.tensor_tensor(
                            out=self.hT[:, mo, cs], in0=self.hT[:, mo, cs], in1=tmpb[s][:], op=ALU.add), waits=[tq])
                        self._tmp_free[s] = ta
            ws2.release(slot, tm)
        self._h_free = tm
        return tm

    def ph_ffn(self):
        I = self.I
        ph = self.phase("ffn")
        self.norm_state()
        ps = ph.psum()
        banks = Banks(ps, range(8))
        xn = ph.alloc("xn", [128, KC, ST], BF16)
        sq = ph.alloc("sq", [128, KC, 512], BF16)
        rstd = [ph.alloc(f"rstd{i}", [128, 512], F32) for i in range(NTT)]
        hbuf = ph.alloc("hbuf", [128, D_FF // 128, ST], BF16)
        self._silu = [ph.alloc(f"silu{i}", [128, 512], F32) for i in range(2)]
        self._silu_free = [None, None]
        self._h_free = None
        ws = WStream(ph, "w", [128, KC, 512], 2)
        ws2 = WStream(ph, "w2", [128, D_FF // 128, 256], 2)
        xtoks, _ = self.emit_norm(ph, ps, banks, 1, xn, sq, rstd)
        self.emit_swiglu(ph, ps, banks, xn, xtoks[-1], I["f_w_in"][0], I["f_w_out"][0], D_FF, hbuf, ws, ws2)
        ph.emit()

    def emit_headnorm(self, ph, ps, banks, src_bank, src_tok, gcol, out_ap, raw, sqb, rsb, free_tok):
        t_raw = ph.op("act", lambda e: e.activation(out=raw[:], in_=ps[:, src_bank, :], func=AF.Copy), waits=[src_tok, free_tok])
        t_sq = ph.op("act", lambda e: e.activation(out=sqb[:], in_=ps[:, src_bank, :], func=AF.Square), waits=[src_tok, free_tok])
        banks.release(src_bank, t_sq)
        b, bf = banks.next()
        tm = ph.op("pe", lambda e: e.matmul(ps[:, b, :], lhsT=self.blk_bf[:], rhs=sqb[:], start=True, stop=True), waits=[t_sq, bf])
        t1, t2 = self.emit_rsqrt(ph, rsb[:], ps[:, b, :], 1.0 / 64, [tm, free_tok])
        banks.release(b, t1)
        t3 = ph.op("dve", lambda e: e.scalar_tensor_tensor(out=out_ap, in0=raw[:], scalar=self.kqg[:, gcol:gcol + 1], in1=rsb[:],
                                                           op0=ALU.mult, op1=ALU.mult), waits=[t2, t_raw])
        return t3

    def ph_kv(self, st):
        I = self.I
        ph = self.phase("kv")
        self.norm_state()
        ps = ph.psum()
        banks = Banks(ps, range(8))
        xn = ph.alloc("xn", [128, KC, ST], BF16)
        sq = ph.alloc("sq", [128, KC, 512], BF16)
        rstd = [ph.alloc(f"rstd{i}", [128, 512], F32) for i in range(NTT)]
        KT = ph.alloc("KT", [128, KC, ST], BF16)
        Vs = ph.alloc("Vs", [128, NT, D], BF16)
        wf = ph.alloc("wf", [128, KC, 16], BF16)
        raws = [ph.alloc(f"raw{i}", [128, 512], F32) for i in range(2)]
        sqbs = [ph.alloc(f"sqb{i}", [128, 512], BF16) for i in range(2)]
        rsbs = [ph.alloc(f"rsb{i}", [128, 512], F32) for i in range(2)]
        ef = ph.alloc("ef", [128, NT, 16], F32)
        ws = WStream(ph, "w", [128, KC, 512], 2)
        kv_w = I["kv_w"]
        xtoks, _ = self.emit_norm(ph, ps, banks, 2, xn, sq, rstd)
        xall = xtoks[-1]
        tf_w = ph.dma("pool", lambda e: e.dma_start(out=wf[:], in_=kv_w[:, 2 * D:2 * D + 16].rearrange("(k p) n -> p k n", p=128)), "wf")

        def load(col0):
            return ws.load([lambda e, buf, col0=col0: e.dma_start(
                out=buf[:], in_=kv_w[:, col0:col0 + 512].rearrange("(k p) n -> p k n", p=128))])
        pend = load(0)
        hfree = [None, None]
        hi = 0
        k_last = None
        for piece in range(2):
            slot, wb, wt = pend
            pend = load((piece + 1) * 512) if piece == 0 else load(D)
            tm = None
            for mm in range(4):
                m = piece * 4 + mm
                for tt in range(NTT):
                    cs = slice(tt * 512, (tt + 1) * 512)
                    b, bf = banks.next()
                    for k in range(KC):
                        tm = ph.op("pe", lambda e, b=b, k=k, mm=mm, cs=cs, wb=wb: e.matmul(
                            ps[:, b, :], lhsT=wb[:, k, mm * 128:(mm + 1) * 128], rhs=xn[:, k, cs],
                            start=(k == 0), stop=(k == KC - 1)), waits=[bf, xall] + wt, sig=(k == KC - 1))
                    s = hi % 2
                    hi += 1
                    k_last = self.emit_headnorm(ph, ps, banks, b, tm, 0, KT[:, m, cs], raws[s], sqbs[s], rsbs[s], hfree[s])
                    hfree[s] = k_last
            ws.release(slot, tm)
        tk_st = ph.dma("sp", lambda e: e.dma_start(out=self.KT_d[:, :, st * ST:(st + 1) * ST], in_=KT[:]), "kst", waits=[k_last])
        v_last = None
        for n in range(2):
            slot, wb, wt = pend
            if n == 0:
                pend = load(D + 512)
            tm = None
            for t in range(NT):
                b, bf = banks.next()
                for k in range(KC):
                    tm = ph.op("pe", lambda e, b=b, k=k, t=t, wb=wb: e.matmul(
                        ps[:, b, :], lhsT=xn[:, k, t * 128:(t + 1) * 128], rhs=wb[:, k, :], start=(k == 0), stop=(k == KC - 1)),
                        waits=[bf, xall] + wt, sig=(k == KC - 1))
                if t % 2 == 0:
                    v_last = ph.op("act", lambda e, b=b, t=t, n=n: e.activation(out=Vs[:, t, n * 512:(n + 1) * 512], in_=ps[:, b, :],
                                                                                func=AF.Copy), waits=[tm])
                else:
                    v_last = ph.op("dve", lambda e, b=b, t=t, n=n: e.tensor_copy(out=Vs[:, t, n * 512:(n + 1) * 512], in_=ps[:, b, :]),
                                   waits=[tm])
                banks.release(b, v_last)
                if t == NT - 2:
                    v_prev = v_last
            ws.release(slot, tm)
        for c in range(KC):
            ph.dma("sp", lambda e, c=c: e.dma_start(out=self.V_d[:, c, st * NT:(st + 1) * NT, :], in_=Vs[:, :, c * 128:(c + 1) * 128]),
                   "vst", waits=[v_last, v_prev])
        b, bf = banks.next()
        tm = None
        for t in range(NT):
            for k in range(KC):
                tm = ph.op("pe", lambda e, b=b, k=k, t=t: e.matmul(
                    ps[:, b, t * 16:(t + 1) * 16], lhsT=xn[:, k, t * 128:(t + 1) * 128], rhs=wf[:, k, :],
                    start=(k == 0), stop=(k == KC - 1)), waits=[bf, xall, tf_w], sig=(k == KC - 1 and t == NT - 1))
        g0 = st * NT
        t1 = ph.op("dve", lambda e: e.tensor_tensor(out=ef[:], in0=ps[:, b, 0:NT * 16].rearrange("p (t h) -> p t h", t=NT),
                                                    in1=bcast_mid(self.bf_bc[:], NT), op=ALU.add), waits=[tm])
        banks.release(b, t1)
        t2 = ph.op("act", lambda e: e.activation(out=ef[:], in_=ef[:], func=AF.Exp, scale=-1.0), waits=[t1])
        t3 = ph.op("act", lambda e: e.activation(out=ef[:], in_=ef[:], func=AF.Ln, bias=self.onec[:], scale=1.0), waits=[t2])
        t4 = ph.op("dve", lambda e: e.tensor_scalar(out=self.logf[:, g0:g0 + NT, :], in0=ef[:], scalar1=-1.0, scalar2=None, op0=ALU.mult),
                   waits=[t3])
        b2, b2f = banks.next()
        tm = None
        for t in range(NT):
            T = g0 + t
            for tp in range(T + 1):
                tm = ph.op("pe", lambda e, b2=b2, t=t, tp=tp, T=T: e.matmul(
                    ps[:, b2, t * 16:(t + 1) * 16], lhsT=(self.tri_f[:] if tp == T else self.ones_f[:]), rhs=self.logf[:, tp, :],
                    start=(tp == 0), stop=(tp == T)), waits=[b2f, t4], sig=(tp == T and t == NT - 1))
        t5 = ph.op("dve", lambda e: e.tensor_copy(out=self.Fcum[:, g0:g0 + NT, :],
                                                  in_=ps[:, b2, 0:NT * 16].rearrange("p (t h) -> p t h", t=NT)), waits=[tm])
        banks.release(b2, t5)
        ph.emit()

    def ph_mixer_b(self, st):
        I = self.I
        ph = self.phase("mixB")
        self.norm_state()
        ps = ph.psum()
        banks = Banks(ps, range(6))
        xn = ph.alloc("xn", [128, KC, ST], BF16)
        rstd = [ph.alloc(f"rstd{i}", [128, 512], F32) for i in range(NTT)]
        QT = ph.alloc("QT", [128, KC, ST], BF16)
        SG = ph.alloc("SG", [128, KC, ST], BF16)
        OT = ph.alloc("OT", [128, KC, ST], BF16)
        sq = OT[:, 0:4, :].rearrange("p a (b n) -> p (a b) n", b=2)
        Vraw = [ph.alloc(f"Vraw{i}", [128, SEQ // 128, 128], BF16) for i in range(2)]
        raws = [ph.alloc(f"raw{i}", [128, 512], F32) for i in range(2)]
        sqbs = [ph.alloc(f"sqb{i}", [128, 512], BF16) for i in range(2)]
        rsbs = [ph.alloc(f"rsb{i}", [128, 512], F32) for i in range(2)]
        ntk = (st + 1) * NT
        L = ntk * 128
        Kb = [ph.alloc(f"Kb{i}", [128, SEQ], BF16) for i in range(2)]
        Vb = [ph.alloc(f"Vb{i}", [128, SEQ // 128, 2, 128], BF16) for i in range(2)]
        nb = ph.alloc("nb", [128, NTT, SEQ // 128, 16], F32)
        cq = ph.alloc("cq", [128, NTT, 16], F32)
        PT = [ph.alloc(f"PT{i}", [128, 512], BF16) for i in range(6)]
        Rt = [ph.alloc(f"Rt{i}", [128, 512], F32) for i in range(2)]
        Rs = [ph.alloc(f"Rs{i}", [128, 512], F32) for i in range(2)]
        ws = WStream(ph, "w", [128, KC, 512], 2)
        wo = ph.alloc("wo", [128, KC, D], BF16)
        w_in = I["b_w_in"][0]
        xtoks, _ = self.emit_norm(ph, ps, banks, 3, xn, sq, rstd)
        xall = xtoks[-1]
        t_wo = ph.dma("pool", lambda e: e.dma_start(out=wo[:], in_=I["b_w_out"][0].rearrange("(k p) n -> p k n", p=128)), "wo")
        tvo = None
        for i in range(2):
            ph.op("dve", lambda e, i=i: e.memset(Vb[i][:, :, 0, 64:128], 1.0), sig=False)
            tvo = ph.op("dve", lambda e, i=i: e.memset(Vb[i][:, :, 1, 0:64], 1.0))

        def load(col0):
            return ws.load([lambda e, buf, col0=col0: e.dma_start(
                out=buf[:], in_=w_in[:, col0:col0 + 512].rearrange("(k p) n -> p k n", p=128))])
        pend = load(0)
        hfree = [None, None]
        hi = 0
        q_last = None
        g_last = None
        for piece in range(4):
            slot, wb, wt = pend
            if piece + 1 < 4:
                pend = load((piece + 1) * 512)
            tm = None
            for mm in range(4):
                m = (piece % 2) * 4 + mm
                for tt in range(NTT):
                    cs = slice(tt * 512, (tt + 1) * 512)
                    b, bf = banks.next()
                    for k in range(KC):
                        tm = ph.op("pe", lambda e, b=b, k=k, mm=mm, cs=cs, wb=wb: e.matmul(
                            ps[:, b, :], lhsT=wb[:, k, mm * 128:(mm + 1) * 128], rhs=xn[:, k, cs],
                            start=(k == 0), stop=(k == KC - 1)), waits=[bf, xall] + wt, sig=(k == KC - 1))
                    if piece < 2:
                        s = hi % 2
                        hi += 1
                        q_last = self.emit_headnorm(ph, ps, banks, b, tm, 1, QT[:, m, cs], raws[s], sqbs[s], rsbs[s], hfree[s])
                        hfree[s] = q_last
                    else:
                        g_last = ph.op("act", lambda e, b=b, m=m, cs=cs: e.activation(out=SG[:, m, cs], in_=ps[:, b, :], func=AF.Sigmoid),
                                       waits=[tm])
                        banks.release(b, g_last)
            ws.release(slot, tm)
        bq, bqf = banks.next()
        tm = None
        for Q in range(NTT):
            Qa = st * NTT + Q
            npre = Qa * 4
            if npre == 0:
                continue
            for tp in range(npre):
                tm = ph.op("pe", lambda e, Q=Q, tp=tp, npre=npre: e.matmul(
                    ps[:, bq, Q * 16:(Q + 1) * 16], lhsT=self.ones_f[:], rhs=self.logf[:, tp, :], start=(tp == 0), stop=(tp == npre - 1)),
                    waits=[bqf])
        tcq = None
        for Q in range(NTT):
            Qa = st * NTT + Q
            if Qa == 0:
                tcq = ph.op("dve", lambda e, Q=Q: e.memset(cq[:, Q, :], 0.0))
            else:
                tcq = ph.op("dve", lambda e, Q=Q: e.tensor_copy(out=cq[:, Q, :], in_=ps[:, bq, Q * 16:(Q + 1) * 16]), waits=[tm])
        banks.release(bq, tcq)
        tnb = None
        for Q in range(NTT):
            tnb0 = ph.op("dve", lambda e, Q=Q: e.tensor_scalar(out=nb[:, Q, 0:ntk, :], in0=self.Fcum[:, 0:ntk, :], scalar1=-1.0,
                                                               scalar2=-SM_BOUND, op0=ALU.mult, op1=ALU.add), waits=[tcq])
            if st * NTT + Q == 0:
                tnb = tnb0
                continue
            tnb = ph.op("dve", lambda e, Q=Q: e.tensor_tensor(out=nb[:, Q, 0:ntk, :], in0=nb[:, Q, 0:ntk, :],
                                                              in1=bcast_mid(cq[:, Q, :], ntk), op=ALU.add), waits=[tnb0])
        NS, LA, DELAY = len(PT), 3, 8
        abanks = Banks(ps, range(4))
        for b_ in range(4):
            abanks.free[b_] = banks.free[b_]
        pvfree = {4: banks.free[4], 5: banks.free[5], 6: None, 7: None}
        kvfree = [None, None]
        pfree = [None] * NS
        work = []
        gi = 0
        for c in range(KC):
            for Q in range(NTT):
                Qa = st * NTT + Q
                nk = (Qa + 1) * 4
                for hh in range(2):
                    for t in range(nk):
                        work.append(dict(c=c, Q=Q, Qa=Qa, nk=nk, hh=hh, t=t, g=gi, par=gi % 2,
                                         first_pair=(Q == 0 and hh == 0 and t == 0),
                                         last_head=(t == nk - 1), last_g=(hh == 1 and t == nk - 1),
                                         last_pair=(Q == NTT - 1 and hh == 1 and t == nk - 1)))
                gi += 1
        n = len(work)
        ktok = {}
        vtok = {}
        tready = {}
        acc = {}
        pending = []
        o_last = None

        def finalize(c, Q, par, a0, a1):
            nonlocal o_last
            cs0 = Q * 512
            pa, pb_ = 4 + 2 * par, 5 + 2 * par
            ta = ph.op("act", lambda e: e.activation(out=Rt[par][0:64, :], in_=ps[0:64, pb_, :], func=AF.Ln), waits=[a1, self._rt_free[par]])
            tb = ph.op("act", lambda e: e.activation(out=Rt[par][64:128, :], in_=ps[64:128, pa, :], func=AF.Ln), waits=[a0, self._rt_free[par]])
            b, bf = abanks.next()
            tsw = ph.op("pe", lambda e: e.matmul(ps[:, b, :], lhsT=self.swap_f[:], rhs=Rt[par][:], start=True, stop=True),
                        waits=[ta, tb, bf])
            self._rt_free[par] = tsw
            tcp = ph.op("act", lambda e: e.activation(out=Rs[par][:], in_=ps[:, b, :], func=AF.Exp, scale=-1.0),
                        waits=[tsw, self._rs2_free[par]])
            abanks.release(b, tcp)
            tg = ph.op("dve", lambda e: e.tensor_tensor(out=Rs[par][:], in0=Rs[par][:], in1=SG[:, c, cs0:cs0 + 512], op=ALU.mult),
                       waits=[tcp, g_last])
            to0 = ph.op("dve", lambda e: e.tensor_tensor(out=OT[0:64, c, cs0:cs0 + 512], in0=ps[0:64, pa, :],
                                                         in1=Rs[par][0:64, :], op=ALU.mult), waits=[tg])
            to1 = ph.op("dve", lambda e: e.tensor_tensor(out=OT[64:128, c, cs0:cs0 + 512], in0=ps[64:128, pb_, :],
                                                         in1=Rs[par][64:128, :], op=ALU.mult), waits=[tg])
            pvfree[pa] = to0
            pvfree[pb_] = to1
            self._rs2_free[par] = to1
            o_last = to1

        for i in range(n + LA):
            if i < n:
                w = work[i]
                c, Q, Qa, hh, t = w["c"], w["Q"], w["Qa"], w["hh"], w["t"]
                sl = c % 2
                if w["first_pair"]:
                    ktok[c] = ph.dma("sp", lambda e, c=c, sl=sl: e.dma_start(out=Kb[sl][:, 0:L], in_=self.KT_d[:, c, 0:L]), f"kb{sl}",
                                     waits=[kvfree[sl]])
                    tvr = ph.dma("sp", lambda e, c=c, sl=sl: e.dma_start(out=Vraw[sl][:, 0:ntk, :], in_=self.V_d[:, c, 0:ntk, :]),
                                 f"vb{sl}", waits=[kvfree[sl]])
                    for h2 in range(2):
                        off = 0 if h2 == 0 else 64
                        vtok[c] = ph.op("pool", lambda e, sl=sl, h2=h2, off=off: e.tensor_copy(
                            out=Vb[sl][:, 0:ntk, h2, off:off + 64], in_=Vraw[sl][:, 0:ntk, off:off + 64]),
                            waits=[tvr, tvo, kvfree[sl]])
                h = c * 2 + hh
                rows = slice(hh * 64, (hh + 1) * 64)
                r = t - Qa * 4
                c0 = max(r, 0) * 128
                cs0 = Q * 512
                b, bf = abanks.next()
                tqk = ph.op("pe", lambda e, b=b, t=t, c0=c0, rows=rows, sl=sl, c=c, cs0=cs0: e.matmul(
                    ps[:, b, c0:512], lhsT=Kb[sl][rows, t * 128:(t + 1) * 128], rhs=QT[rows, c, cs0 + c0:cs0 + 512],
                    start=True, stop=True), waits=[bf, ktok[c], q_last])
                s_ = i % NS
                tex = ph.op("act", lambda e, b=b, t=t, c0=c0, s_=s_, Q=Q, h=h: e.activation(
                    out=PT[s_][:, c0:512], in_=ps[:, b, c0:512], func=AF.Exp, bias=nb[:, Q, t, h:h + 1], scale=1.0),
                    waits=[tqk, pfree[s_], tnb])
                abanks.release(b, tex)
                tr_ = tex
                if r >= 0:
                    tr_ = ph.op("dve", lambda e, s_=s_, c0=c0: e.tensor_tensor(
                        out=PT[s_][:, c0:c0 + 128], in0=PT[s_][:, c0:c0 + 128], in1=self.trimask[:], op=ALU.mult), waits=[tex])
                tready[i] = (tr_, c0, s_)
            j = i - LA
            if j >= 0:
                w = work[j]
                c, Q, hh, t, nk, par = w["c"], w["Q"], w["hh"], w["t"], w["nk"], w["par"]
                sl = c % 2
                pb = 4 + 2 * par + hh
                tr_, c0, s_ = tready.pop(j)
                tpv = ph.op("pe", lambda e, pb=pb, t=t, c0=c0, s_=s_, sl=sl, hh=hh, nk=nk: e.matmul(
                    ps[:, pb, c0:512], lhsT=Vb[sl][:, t, hh, :], rhs=PT[s_][:, c0:512], start=(t == 0), stop=(t == nk - 1)),
                    waits=[tr_, vtok[c], pvfree[pb] if t == 0 else None])
                pfree[s_] = tpv
                if w["last_head"]:
                    acc[(w["g"], hh)] = tpv
                if w["last_pair"]:
                    kvfree[sl] = tpv
                if w["last_g"]:
                    pending.append((j + DELAY, c, Q, par, acc.pop((w["g"], 0)), acc.pop((w["g"], 1))))
                while pending and pending[0][0] <= j:
                    _, c_, Q_, par_, a0, a1 = pending.pop(0)
                    finalize(c_, Q_, par_, a0, a1)
        while pending:
            _, c_, Q_, par_, a0, a1 = pending.pop(0)
            finalize(c_, Q_, par_, a0, a1)
        banks = abanks
        if self.cfg.get("dbg") and st == 0:
            tdd = None
            for c in range(KC):
                for tt in range(NTT):
                    cs = slice(tt * 512, (tt + 1) * 512)
                    td = ph.op("act", lambda e, c=c, cs=cs: e.activation(out=raws[0][:], in_={'OT': OT, 'QT': QT, 'SG': SG, 'XN': xn}[self.cfg['dbg']][:, c, cs], func=AF.Copy), waits=[o_last, tdd])
                    tdd = ph.dma("sp", lambda e, c=c, cs=cs: e.dma_start(out=self.dbg[:, c, cs], in_=raws[0][:]), "dbg", waits=[td])
        tm = None
        for mo in range(KC):
            for tt in range(NTT):
                cs = slice(tt * 512, (tt + 1) * 512)
                b, bf = banks.next()
                for k in range(KC):
                    tm = ph.op("pe", lambda e, b=b, k=k, mo=mo, cs=cs: e.matmul(
                        ps[:, b, :], lhsT=wo[:, k, mo * 128:(mo + 1) * 128], rhs=OT[:, k, cs], start=(k == 0), stop=(k == KC - 1)),
                        waits=[bf, o_last, t_wo], sig=(k == KC - 1))
                ta = ph.op("dve", lambda e, b=b, mo=mo, cs=cs: e.tensor_tensor(
                    out=self.hT[:, mo, cs], in0=self.hT[:, mo, cs], in1=ps[:, b, :], op=ALU.add), waits=[tm])
                banks.release(b, ta)
        ph.emit()

    def ph_moe(self):
        I = self.I
        ph = self.phase("moe")
        self.norm_state()
        ps = ph.psum()
        banks = Banks(ps, range(8))
        xn = ph.alloc("xn", [128, KC, ST], BF16)
        sq = ph.alloc("sq", [128, KC, 512], BF16)
        rstd = [ph.alloc(f"rstd{i}", [128, 512], F32) for i in range(NTT)]
        hbuf = ph.alloc("hbuf", [128, D_EXP // 128, ST], BF16)
        self._silu = [ph.alloc(f"silu{i}", [128, 512], F32) for i in range(2)]
        self._silu_free = [None, None]
        self._h_free = None
        tmpb = [ph.alloc(f"tmpb{i}", [128, 512], F32) for i in range(2)]
        self._tmp_free = [None, None]
        self._tmp_i = 0
        lgT = ph.alloc("lgT", [8, ST], F32)
        lg = ph.alloc("lg", [128, NT, NEXP], F32)
        m1 = ph.alloc("m1", [128, NT], F32)
        m2 = ph.alloc("m2", [128, NT], F32)
        mk1 = ph.alloc("mk1", [128, NT, NEXP], F32)
        mk2 = ph.alloc("mk2", [128, NT, NEXP], F32)
        lg2 = ph.alloc("lg2", [128, NT, NEXP], F32)
        g1 = ph.alloc("g1", [128, NT], F32)
        g2 = ph.alloc("g2", [128, NT], F32)
        gates = ph.alloc("gates", [128, NT, NEXP], F32)
        gT = ph.alloc("gT", [8, ST], F32)
        gbc = [ph.alloc(f"gbc{i}", [128, ST], F32) for i in range(2)]
        ws = WStream(ph, "w", [128, KC, 512], 2)
        ws2 = WStream(ph, "w2", [128, D_EXP // 128, 256], 2)
        xtoks, rtoks = self.emit_norm(ph, ps, banks, 4, xn, sq, rstd)
        xall = xtoks[-1]
        tl = None
        for tt in range(NTT):
            cs = slice(tt * 512, (tt + 1) * 512)
            b, bf = banks.next()
            tm = None
            for k in range(KC):
                tm = ph.op("pe", lambda e, b=b, k=k, cs=cs: e.matmul(ps[0:8, b, :], lhsT=self.gwr[:, k, :], rhs=self.hT[:, k, cs],
                                                                     start=(k == 0), stop=(k == KC - 1)), waits=[bf], sig=(k == KC - 1))
            tl = ph.op("dve", lambda e, b=b, cs=cs, tt=tt: e.tensor_tensor(out=lgT[:, cs], in0=ps[0:8, b, :], in1=rstd[tt][0:8, :],
                                                                           op=ALU.mult), waits=[tm, rtoks[tt]])
            banks.release(b, tl)
        b, bf = banks.next()
        tp = None
        for t in range(NT):
            tp = ph.op("pe", lambda e, b=b, t=t: e.transpose(ps[:, b, t * 8:(t + 1) * 8], lgT[:, t * 128:(t + 1) * 128], self.ident[0:8, 0:8]),
                       waits=[bf, tl], sig=(t == NT - 1))
        t0 = ph.op("dve", lambda e, b=b: e.tensor_copy(out=lg[:], in_=ps[:, b, 0:NT * 8].rearrange("p (t x) -> p t x", t=NT)), waits=[tp])
        banks.release(b, t0)
        t1 = ph.op("dve", lambda e: e.tensor_reduce(out=m1[:], in_=lg[:], axis=AX.X, op=ALU.max), waits=[t0])
        tk1 = None
        for t in range(NT):
            tk1 = ph.op("dve", lambda e, t=t: e.tensor_scalar(out=mk1[:, t, :], in0=lg[:, t, :], scalar1=m1[:, t:t + 1], scalar2=None,
                                                              op0=ALU.is_equal), waits=[t1])
        t2 = ph.op("dve", lambda e: e.scalar_tensor_tensor(out=lg2[:], in0=mk1[:], scalar=-1e30, in1=lg[:], op0=ALU.mult, op1=ALU.add),
                   waits=[tk1])
        t3 = ph.op("dve", lambda e: e.tensor_reduce(out=m2[:], in_=lg2[:], axis=AX.X, op=ALU.max), waits=[t2])
        tk2 = None
        for t in range(NT):
            tk2 = ph.op("dve", lambda e, t=t: e.tensor_scalar(out=mk2[:, t, :], in0=lg2[:, t, :], scalar1=m2[:, t:t + 1], scalar2=None,
                                                              op0=ALU.is_equal), waits=[t3])
        t4 = ph.op("dve", lambda e: e.tensor_tensor(out=g1[:], in0=m1[:], in1=m2[:], op=ALU.subtract), waits=[t3])
        t5 = ph.op("act", lambda e: e.activation(out=g1[:], in_=g1[:], func=AF.Sigmoid), waits=[t4])
        t6 = ph.op("dve", lambda e: e.tensor_scalar(out=g2[:], in0=g1[:], scalar1=-1.0, scalar2=1.0, op0=ALU.mult, op1=ALU.add), waits=[t5])
        tg = None
        for t in range(NT):
            ta = ph.op("dve", lambda e, t=t: e.tensor_scalar(out=mk1[:, t, :], in0=mk1[:, t, :], scalar1=g1[:, t:t + 1], scalar2=None,
                                                             op0=ALU.mult), waits=[t5, tk2])
            tb = ph.op("dve", lambda e, t=t: e.scalar_tensor_tensor(out=gates[:, t, :], in0=mk2[:, t, :], scalar=g2[:, t:t + 1],
                                                                    in1=mk1[:, t, :], op0=ALU.mult, op1=ALU.add), waits=[ta, t6])
            tg = tb
        tgt = None
        for half in range(2):
            b, bf = banks.next()
            tp = None
            for q in range(4):
                t = half * 4 + q
                tp = ph.op("pe", lambda e, b=b, q=q, t=t: e.transpose(ps[0:8, b, q * 128:(q + 1) * 128], gates[:, t, :], self.ident[:]),
                           waits=[bf, tg], sig=(q == 3))
            tgt = ph.op("dve", lambda e, b=b, half=half: e.tensor_copy(out=gT[:, half * 512:(half + 1) * 512], in_=ps[0:8, b, :]), waits=[tp])
            banks.release(b, tgt)
        gfree = [None, None]
        for ex in range(NEXP):
            gs = ex % 2
            tgb = None
            for tt in range(NTT):
                cs = slice(tt * 512, (tt + 1) * 512)
                b, bf = banks.next()
                tm = ph.op("pe", lambda e, b=b, ex=ex, cs=cs: e.matmul(ps[:, b, :], lhsT=self.sel8[:, ex, :], rhs=gT[:, cs], start=True, stop=True),
                           waits=[bf, tgt])
                tgb = ph.op("act", lambda e, b=b, gs=gs, cs=cs: e.activation(out=gbc[gs][:, cs], in_=ps[:, b, :], func=AF.Copy),
                            waits=[tm, gfree[gs]])
                banks.release(b, tgb)
            last = self.emit_swiglu(ph, ps, banks, xn, xall, I["m_w_in"][0, ex], I["m_w_out"][0, ex], D_EXP, hbuf, ws, ws2,
                                    gate_bc=gbc[gs], gate_tok=tgb, tmpb=tmpb,
                                    next_w_in=(I["m_w_in"][0, ex + 1] if ex + 1 < NEXP else None))
            gfree[gs] = self._tmp_free[(self._tmp_i - 1) % 2]
        ph.emit()

    def build(self):
        cfg = self.cfg
        self.declare()
        with ExitStack() as es:
            self.alloc_persist(es)
            self._rt_free = [None, None]
            self._rs2_free = [None, None]
            self.ph_setup()
            for seq in range(cfg.get("nseq", NSEQ)):
                for st in range(cfg.get("nst", SEQ // ST)):
                    self._rt_free = [None, None]
                    self._rs2_free = [None, None]
                    self.ph_load(seq, st)
                    if cfg.get("mixa", True):
                        self.ph_mixer_a()
                    if cfg.get("ffn", True):
                        self.ph_ffn()
                    if cfg.get("kv", True):
                        self.ph_kv(st)
                    if cfg.get("mixb", True):
                        self._rt_free = [None, None]
                        self._rs2_free = [None, None]
                        self.ph_mixer_b(st)
                    if cfg.get("moe", True):
                        self.ph_moe()
                    self.ph_store(seq, st)
        return self.nc


INPUT_NAMES = ["x", "a_norm_g", "a_w_in", "a_v_norm_g", "a_w_spatial", "a_b_spatial", "a_w_out",
               "f_norm_g", "f_w_in", "f_w_out", "kv_norm_g", "kv_w", "kv_b_f", "k_norm_g",
               "b_norm_g", "b_w_in", "q_norm_g", "b_w_out", "m_norm_g", "m_w_router", "m_w_in", "m_w_out"]


def kernel(**inputs):
    n = 8
    k = Kern({})
    nc = k.build()
    shared = {nm: np.ascontiguousarray(np.asarray(inputs[nm], dtype=np.float32)) for nm in INPUT_NAMES if nm != "x"}
    x = np.asarray(inputs["x"], dtype=np.float32)
    in_maps = []
    for i in range(n):
        m = dict(shared)
        m["x"] = np.ascontiguousarray(x[i * NSEQ:(i + 1) * NSEQ])
        in_maps.append(m)
    res = run_bass_kernel_spmd(nc, in_maps, core_ids=list(range(n)))
    return np.concatenate([r["out"] for r in res.results], axis=0)
```

```python
from contextlib import ExitStack

import numpy as np
import concourse.bass as bass
import concourse.mybir as mybir
from concourse.bass_utils import run_bass_kernel_spmd

F32 = mybir.dt.float32
BF16 = mybir.dt.bfloat16
AF = mybir.ActivationFunctionType
ALU = mybir.AluOpType
AX = mybir.AxisListType

ENG = ("pe", "act", "dve", "pool", "sp")
D = 1024
KC = 8
ST = 1024
NTT = ST // 512
NT = ST // 128
SEQ = 2048
NSEQ = 2
A_HALF = 3072
D_FF = 2816
D_EXP = 3584
NEXP = 8
EPS = 1e-6
SM_BOUND = 12.0


class Phase:
    def __init__(self, nc, name):
        self.nc = nc
        self.name = name
        self.es = ExitStack()
        self.ops = {e: [] for e in ENG}
        self.sem = {}
        self.cnt = {}
        self.all_sems = []
        for e in ENG:
            self.sem[e] = nc.alloc_semaphore(name=f"{name}_{e}")
            self.all_sems.append(self.sem[e])
            self.cnt[e] = 0
        self.dsem = {}
        self.waited = {e: {} for e in ENG}

    def alloc(self, name, shape, dt):
        return self.es.enter_context(self.nc.sbuf_tensor(f"{self.name}_{name}", shape, dt))

    def psum(self, name="ps"):
        return self.es.enter_context(self.nc.psum_tensor(f"{self.name}_{name}", [128, 8, 512], F32))

    def _waits(self, eng, waits):
        out = []
        for w in waits:
            if w is None:
                continue
            sem, val = w
            key = sem.num
            if self.waited[eng].get(key, 0) >= val:
                continue
            self.waited[eng][key] = val
            out.append((sem, val))
        return out

    def op(self, eng, fn, waits=(), sig=True):
        ws = self._waits(eng, waits)
        tok = None
        inc = None
        if sig:
            self.cnt[eng] += 1
            tok = (self.sem[eng], self.cnt[eng])
            inc = (self.sem[eng], 1)
        self.ops[eng].append((ws, fn, inc))
        return tok

    def dma(self, eng, fn, key, waits=()):
        if key not in self.dsem:
            s = self.nc.alloc_semaphore(name=f"{self.name}_d{len(self.dsem)}")
            self.all_sems.append(s)
            self.dsem[key] = [s, 0, eng]
        d = self.dsem[key]
        assert d[2] == eng
        d[1] += 16
        ws = self._waits(eng, waits)
        self.ops[eng].append((ws, fn, (d[0], 16)))
        return (d[0], d[1])

    def emit(self):
        nc = self.nc
        for key, (s, v, eng) in self.dsem.items():
            ws = self._waits(eng, [(s, v)])
            if ws:
                self.ops[eng].append((ws, None, None))

        def run(engobj, name):
            for ws, fn, inc in self.ops[name]:
                for sem, val in ws:
                    engobj.wait_ge(sem, val)
                if fn is None:
                    continue
                ins = fn(engobj)
                if inc is not None:
                    ins.then_inc(inc[0], inc[1])

        with nc.Block() as block:
            block.tensor(lambda e: run(e, "pe"))
            block.scalar(lambda e: run(e, "act"))
            block.vector(lambda e: run(e, "dve"))
            block.gpsimd(lambda e: run(e, "pool"))
            block.sync(lambda e: run(e, "sp"))
        nc.all_engine_barrier()
        nc.clear_and_free_semaphores(self.all_sems)
        nc.all_engine_barrier()
        self.es.close()


class Banks:
    def __init__(self, ps, ids):
        self.ps = ps
        self.ids = list(ids)
        self.free = {b: None for b in self.ids}
        self.i = 0

    def next(self):
        b = self.ids[self.i % len(self.ids)]
        self.i += 1
        return b, self.free[b]

    def release(self, b, tok):
        self.free[b] = tok


class WStream:
    def __init__(self, ph, name, shape, nbuf, dt=BF16, eng="pool", bufs=None, free0=None):
        self.ph = ph
        self.name = name
        self.eng = eng
        self.bufs = bufs if bufs is not None else [ph.alloc(f"{name}{i}", shape, dt) for i in range(nbuf)]
        self.free = [free0] * len(self.bufs)
        self.i = 0

    def load(self, fns):
        slot = self.i % len(self.bufs)
        self.i += 1
        buf = self.bufs[slot]
        toks = []
        for j, fn in enumerate(fns):
            toks.append(self.ph.dma(self.eng, (lambda e, fn=fn, buf=buf: fn(e, buf)), f"{self.name}{slot}",
                                    waits=[self.free[slot]]))
        return slot, buf, toks[-1:]

    def release(self, slot, tok):
        self.free[slot] = tok


def bcast_mid(ap2d, rep):
    a = ap2d.ap
    return bass.AP(ap2d.tensor, ap2d.offset, [list(a[0]), [0, rep], list(a[1])])


def bcast_last(ap2d, n):
    a = ap2d.ap
    return bass.AP(ap2d.tensor, ap2d.offset, [list(a[0]), [0, n]])


class Kern:
    def __init__(self, cfg):
        self.cfg = cfg
        self.nc = bass.Bass("TRN2", target_bir_lowering=False)
        self.pid = 0

    def phase(self, name):
        self.pid += 1
        return Phase(self.nc, f"p{self.pid}{name}")

    def declare(self):
        nc = self.nc
        I = {}

        def inp(name, shape):
            I[name] = nc.dram_tensor(name, list(shape), F32, kind="ExternalInput").ap()

        inp("x", (NSEQ, SEQ, D))
        inp("a_norm_g", (1, D)); inp("a_w_in", (1, D, 2 * A_HALF)); inp("a_v_norm_g", (1, A_HALF))
        inp("a_w_spatial", (1, 8, 128, 128)); inp("a_b_spatial", (1, 8, 128)); inp("a_w_out", (1, A_HALF, D))
        inp("f_norm_g", (1, D)); inp("f_w_in", (1, D, 2 * D_FF)); inp("f_w_out", (1, D_FF, D))
        inp("kv_norm_g", (D,)); inp("kv_w", (D, 2 * D + 16)); inp("kv_b_f", (16,)); inp("k_norm_g", (64,))
        inp("b_norm_g", (1, D)); inp("b_w_in", (1, D, 2 * D)); inp("q_norm_g", (1, 64)); inp("b_w_out", (1, D, D))
        inp("m_norm_g", (1, D)); inp("m_w_router", (1, D, NEXP)); inp("m_w_in", (1, NEXP, D, 2 * D_EXP))
        inp("m_w_out", (1, NEXP, D_EXP, D))
        self.I = I
        self.out = nc.dram_tensor("out", [NSEQ, SEQ, D], F32, kind="ExternalOutput").ap()
        if self.cfg.get("dbg"):
            self.dbg = nc.dram_tensor("dbg", [128, KC, ST], F32, kind="ExternalOutput").ap()
        self.KT_d = nc.dram_tensor("kt_scr", [128, KC, SEQ], BF16).ap()
        self.V_d = nc.dram_tensor("v_scr", [128, KC, SEQ // 128, 128], BF16).ap()

    def alloc_persist(self, es):
        nc = self.nc

        def sb(name, shape, dt=F32):
            return es.enter_context(nc.sbuf_tensor(name, shape, dt))

        self.hT = sb("hT", [128, KC, ST])
        self.ident = sb("ident", [128, 128])
        self.ones_bf = sb("ones_bf", [128, 128], BF16)
        self.blk_bf = sb("blk_bf", [128, 128], BF16)
        self.tri_f = sb("tri_f", [128, 128])
        self.ones_f = sb("ones_f", [128, 128])
        self.trimask = sb("trimask", [128, 128], BF16)
        self.swap_f = sb("swap_f", [128, 128])
        self.gains = sb("gains", [128, 5, KC])
        self.vgain = sb("vgain", [128, 24])
        self.kqg = sb("kqg", [128, 2])
        self.biasbc = sb("biasbc", [128, 8, 128])
        self.bf_bc = sb("bf_bc", [128, 16])
        self.wTsp = sb("wTsp", [128, 8, 128])
        self.gwr = sb("gwr", [128, KC, NEXP])
        self.sel8 = sb("sel8", [8, NEXP, 128])
        self.epsc = sb("epsc", [128, 1])
        self.onec = sb("onec", [128, 1])
        self.logf = sb("logf", [128, SEQ // 128, 16])
        self.Fcum = sb("Fcum", [128, SEQ // 128, 16])

    def ph_setup(self):
        I = self.I
        ph = self.phase("setup")
        nc = self.nc
        wsp = ph.alloc("wsp", [128, 8, 128], F32)
        rt = ph.alloc("rt", [128, KC, NEXP], F32)
        ps = ph.psum()
        toks = []
        with nc.allow_non_contiguous_dma(reason="tiny one-time parameter loads"):
            def ld(out, in_, key):
                return ph.dma("sp", lambda e: e.dma_start(out=out, in_=in_, allow_slow_non_contiguous=True), key)
            for i, nm in enumerate(["a_norm_g", "f_norm_g", "kv_norm_g", "b_norm_g", "m_norm_g"]):
                src = I[nm] if nm == "kv_norm_g" else I[nm][0]
                toks.append(ld(self.gains[:, i, :], src.rearrange("(c p) -> p c", p=128), "g"))
            toks.append(ld(self.vgain[:], I["a_v_norm_g"][0].rearrange("(c p) -> p c", p=128), "g"))
            for half in range(2):
                toks.append(ld(self.kqg[half * 64:(half + 1) * 64, 0:1], I["k_norm_g"].rearrange("(p o) -> p o", o=1), "g"))
                toks.append(ld(self.kqg[half * 64:(half + 1) * 64, 1:2], I["q_norm_g"][0].rearrange("(p o) -> p o", o=1), "g"))
            toks.append(ld(self.biasbc[:], bass.AP(I["a_b_spatial"].tensor, 0, [[0, 128], [128, 8], [1, 128]]), "g"))
            toks.append(ld(self.bf_bc[:], bass.AP(I["kv_b_f"].tensor, 0, [[0, 128], [1, 16]]), "g"))
            toks.append(ld(wsp[:], I["a_w_spatial"][0].rearrange("g i j -> i g j"), "g"))
            toks.append(ld(rt[:], I["m_w_router"][0].rearrange("(c p) e -> p c e", p=128), "g"))
        tl = toks[-1]
        pl = []
        def P(fn, waits=()):
            t = ph.op("pool", fn, waits=waits)
            pl.append(t)
            return t
        P(lambda e: e.memset(self.ident[:], 0.0))
        t_id = P(lambda e: e.affine_select(out=self.ident[:], in_=self.ident[:], pattern=[[-1, 128]],
                                           compare_op=ALU.not_equal, fill=1.0, base=0, channel_multiplier=1), waits=[pl[-1]])
        P(lambda e: e.memset(self.ones_bf[:], 1.0))
        P(lambda e: e.memset(self.epsc[:], EPS))
        P(lambda e: e.memset(self.onec[:], 1.0))
        P(lambda e: e.memset(self.ones_f[:], 1.0))
        P(lambda e: e.memset(self.blk_bf[:], 0.0))
        P(lambda e: e.memset(self.blk_bf[0:64, 0:64], 1.0), waits=[pl[-1]])
        P(lambda e: e.memset(self.blk_bf[64:128, 64:128], 1.0), waits=[pl[-2]])
        P(lambda e: e.memset(self.tri_f[:], 1.0))
        P(lambda e: e.affine_select(out=self.tri_f[:], in_=self.tri_f[:], pattern=[[1, 128]],
                                    compare_op=ALU.is_ge, fill=0.0, base=0, channel_multiplier=-1), waits=[pl[-1]])
        t_tri = pl[-1]
        P(lambda e: e.tensor_copy(out=self.trimask[:], in_=self.tri_f[:]), waits=[t_tri])
        P(lambda e: e.memset(self.swap_f[:], 0.0))
        t0 = pl[-1]
        P(lambda e: e.affine_select(out=self.swap_f[:, 0:64], in_=self.swap_f[:, 0:64], pattern=[[-1, 64]],
                                    compare_op=ALU.not_equal, fill=1.0, base=-64, channel_multiplier=1), waits=[t0])
        P(lambda e: e.affine_select(out=self.swap_f[:, 64:128], in_=self.swap_f[:, 64:128], pattern=[[-1, 64]],
                                    compare_op=ALU.not_equal, fill=1.0, base=0, channel_multiplier=1), waits=[t0])
        P(lambda e: e.memset(self.sel8[:], 0.0))
        P(lambda e: e.affine_select(out=self.sel8[:], in_=self.sel8[:], pattern=[[-1, NEXP], [0, 128]],
                                    compare_op=ALU.not_equal, fill=1.0, base=0, channel_multiplier=1), waits=[pl[-1]])
        t_pool = pl[-1]
        t1 = ph.op("dve", lambda e: e.tensor_scalar(out=self.kqg[:, 1:2], in0=self.kqg[:, 1:2], scalar1=0.125, scalar2=None,
                                                    op0=ALU.mult), waits=[tl])
        tg = None
        for c in range(KC):
            tg = ph.op("dve", lambda e, c=c: e.tensor_scalar(out=self.gwr[:, c, :], in0=rt[:, c, :],
                                                             scalar1=self.gains[:, 4, c:c + 1], scalar2=None, op0=ALU.mult),
                       waits=[tl])
        tp = None
        for g in range(8):
            tp = ph.op("pe", lambda e, g=g: e.transpose(ps[:, g // 4, (g % 4) * 128:(g % 4 + 1) * 128], wsp[:, g, :], self.ident[:]),
                       waits=[tl, t_id])
        tc = None
        for b in range(2):
            tc = ph.op("dve", lambda e, b=b: e.tensor_copy(out=self.wTsp[:, b * 4:(b + 1) * 4, :],
                                                           in_=ps[:, b, :].rearrange("p (g i) -> p g i", g=4)), waits=[tp])
        ph.op("dve", lambda e: e.memset(self.wTsp[64:128, :, 0:64], 0.0), waits=[tc])
        ph.emit()

    def ph_load(self, seq, st):
        ph = self.phase("load")
        xt = [ph.alloc(f"xt{i}", [128, D], F32) for i in range(2)]
        ps = ph.psum()
        banks = Banks(ps, range(8))
        xfree = [None, None]
        for t in range(NT):
            sl = t % 2
            r0 = st * ST + t * 128
            tl = ph.dma("sp", lambda e, sl=sl, r0=r0: e.dma_start(out=xt[sl][:], in_=self.I["x"][seq, r0:r0 + 128, :]),
                        f"x{sl}", waits=[xfree[sl]])
            for half in range(2):
                b, bf = banks.next()
                tp = None
                for j in range(4):
                    c = half * 4 + j
                    tp = ph.op("pe", lambda e, b=b, j=j, c=c, sl=sl: e.transpose(ps[:, b, j * 128:(j + 1) * 128],
                                                                                 xt[sl][:, c * 128:(c + 1) * 128], self.ident[:]),
                               waits=[tl, bf], sig=(j == 3))
                eng = "act" if half == 0 else "dve"
                if eng == "act":
                    tcp = ph.op("act", lambda e, b=b, half=half, t=t: e.activation(
                        out=self.hT[:, half * 4:(half + 1) * 4, t * 128:(t + 1) * 128],
                        in_=ps[:, b, :].rearrange("p (c i) -> p c i", c=4), func=AF.Copy), waits=[tp])
                else:
                    tcp = ph.op("dve", lambda e, b=b, half=half, t=t: e.tensor_copy(
                        out=self.hT[:, half * 4:(half + 1) * 4, t * 128:(t + 1) * 128],
                        in_=ps[:, b, :].rearrange("p (c i) -> p c i", c=4)), waits=[tp])
                banks.release(b, tcp)
                if half == 1:
                    xfree[sl] = tp
        ph.emit()

    def ph_store(self, seq, st):
        ph = self.phase("store")
        ot = [ph.alloc(f"ot{i}", [128, D], F32) for i in range(2)]
        ps = ph.psum()
        banks = Banks(ps, range(8))
        ofree = [None, None]
        for t in range(NT):
            sl = t % 2
            r0 = st * ST + t * 128
            cps = []
            for half in range(2):
                b, bf = banks.next()
                tp = None
                for j in range(4):
                    c = half * 4 + j
                    tp = ph.op("pe", lambda e, b=b, j=j, c=c, t=t: e.transpose(ps[:, b, j * 128:(j + 1) * 128],
                                                                               self.hT[:, c, t * 128:(t + 1) * 128], self.ident[:]),
                               waits=[bf], sig=(j == 3))
                if half == 0:
                    tcp = ph.op("act", lambda e, b=b, sl=sl: e.activation(out=ot[sl][:, 0:512], in_=ps[:, b, :], func=AF.Copy),
                                waits=[tp, ofree[sl]])
                else:
                    tcp = ph.op("dve", lambda e, b=b, sl=sl: e.tensor_copy(out=ot[sl][:, 512:1024], in_=ps[:, b, :]),
                                waits=[tp, ofree[sl]])
                banks.release(b, tcp)
                cps.append(tcp)
            ofree[sl] = ph.dma("sp", lambda e, sl=sl, r0=r0: e.dma_start(out=self.out[seq, r0:r0 + 128, :], in_=ot[sl][:]),
                               f"o{sl}", waits=cps)
        ph.emit()

    def emit_norm(self, ph, ps, banks, gidx, xn, sq, rstd_bufs, want_rstd=False):
        toks = []
        rstd_toks = []
        for tt in range(NTT):
            cs = slice(tt * 512, (tt + 1) * 512)
            b, bf = banks.next()
            tm = None
            for c in range(KC):
                eng = "act" if c % 2 == 0 else "dve"
                if eng == "act":
                    tsq = ph.op("act", lambda e, c=c, cs=cs: e.activation(out=sq[:, c, :], in_=self.hT[:, c, cs], func=AF.Square),
                                waits=[self._sq_free.get(c)])
                else:
                    tsq = ph.op("dve", lambda e, c=c, cs=cs: e.tensor_tensor(out=sq[:, c, :], in0=self.hT[:, c, cs], in1=self.hT[:, c, cs],
                                                                            op=ALU.mult), waits=[self._sq_free.get(c)])
                tm = ph.op("pe", lambda e, c=c, b=b: e.matmul(ps[:, b, :], lhsT=self.ones_bf[:], rhs=sq[:, c, :],
                                                              start=(c == 0), stop=(c == KC - 1)), waits=[tsq, bf])
                self._sq_free[c] = tm
            rs = rstd_bufs[tt]
            t1, t2 = self.emit_rsqrt(ph, rs[:], ps[:, b, :], 1.0 / D, [tm, self._rs_free.get(tt)])
            banks.release(b, t1)
            rstd_toks.append(t2)
            last = []
            for c in range(KC):
                tx = ph.op("dve", lambda e, c=c, cs=cs, rs=rs: e.scalar_tensor_tensor(
                    out=xn[:, c, cs], in0=self.hT[:, c, cs], scalar=self.gains[:, gidx, c:c + 1], in1=rs[:],
                    op0=ALU.mult, op1=ALU.mult), waits=[t2, self._xn_free])
                last.append(tx)
            toks.append(last[-1])
            self._rs_free[tt] = last[-1]
        return toks, rstd_toks

    def emit_rsqrt(self, ph, out_ap, in_ap, scale, waits):
        n = out_ap.shape[0]
        t1 = ph.op("act", lambda e: e.activation(out=out_ap, in_=in_ap, func=AF.Ln, bias=self.epsc[0:n, :], scale=scale), waits=waits)
        t2 = ph.op("act", lambda e: e.activation(out=out_ap, in_=out_ap, func=AF.Exp, scale=-0.5), waits=[t1])
        return t1, t2

    def norm_state(self):
        self._sq_free = {}
        self._rs_free = {}
        self._xn_free = None

    def ph_mixer_a(self):
        I = self.I
        ph = self.phase("mixA")
        self.norm_state()
        ps = ph.psum()
        banks = Banks(ps, range(8))
        xn = ph.alloc("xn", [128, KC, ST], BF16)
        rstd = [ph.alloc(f"rstd{i}", [128, 512], F32) for i in range(NTT)]
        big1 = ph.alloc("big1", [128, NT * A_HALF], BF16)
        big2 = ph.alloc("big2", [128, 24 * ST], BF16)
        vg = big1[:].rearrange("p (t n) -> p t n", t=NT)
        pT = big2[:].rearrange("p (m t) -> p m t", m=24)
        sq = big2[:, 0:KC * 512].rearrange("p (c n) -> p c n", c=KC)
        junk = big2[:, 8192:8192 + A_HALF]
        wts = ph.alloc("wts", [128, NT, 8 * 128], BF16)
        ssv = ph.alloc("ssv", [128, NT], F32)
        rsv = ph.alloc("rsv", [128, NT], F32)
        usb = [ph.alloc(f"usb{i}", [128, 512], BF16) for i in range(4)]
        t1b = [ph.alloc(f"t1b{i}", [128, 512], F32) for i in range(2)]
        ws = WStream(ph, "w", [128, KC, 512], 2)
        w_in = I["a_w_in"][0]
        w_out = I["a_w_out"][0]

        xtoks, _ = self.emit_norm(ph, ps, banks, 0, xn, sq, rstd)
        xall = xtoks[-1]

        def load_in(col0):
            return ws.load([lambda e, buf, col0=col0: e.dma_start(
                out=buf[:], in_=w_in[:, col0:col0 + 512].rearrange("(k p) n -> p k n", p=128))])

        pend = load_in(A_HALF)
        for n in range(6):
            slot, wb, wt = pend
            if n + 1 < 6:
                pend = load_in(A_HALF + (n + 1) * 512)
            else:
                pend = load_in(0)
            tm = None
            for t in range(NT):
                b, bf = banks.next()
                for k in range(KC):
                    tm = ph.op("pe", lambda e, b=b, k=k, t=t, wb=wb: e.matmul(
                        ps[:, b, :], lhsT=xn[:, k, t * 128:(t + 1) * 128], rhs=wb[:, k, :], start=(k == 0), stop=(k == KC - 1)),
                        waits=[xall, bf] + wt, sig=(k == KC - 1))
                tg = ph.op("act", lambda e, b=b, t=t, n=n: e.activation(out=vg[:, t, n * 512:(n + 1) * 512], in_=ps[:, b, :],
                                                                        func=AF.Gelu), waits=[tm])
                banks.release(b, tg)
                vg_last = tg
            ws.release(slot, tm)
        tz = ph.op("dve", lambda e: e.memset(ssv[:], 0.0))
        tsq = None
        for t in range(NT):
            tsq = ph.op("act", lambda e, t=t: e.activation(out=junk, in_=vg[:, t, :], func=AF.Square,
                                                           accum_out=ssv[:, t:t + 1]), waits=[vg_last, tz, tsq])
        _, tr = self.emit_rsqrt(ph, rsv[:], ssv[:], 1.0 / A_HALF, [tsq])
        tw = None
        for t in range(NT):
            tw = ph.op("dve", lambda e, t=t: e.tensor_scalar(out=wts[:, t, :], in0=self.wTsp[:].rearrange("p g i -> p (g i)"),
                                                             scalar1=rsv[:, t:t + 1], scalar2=None, op0=ALU.mult), waits=[tr])
        ufree = [None] * 4
        t1free = [None] * 2
        ui = 0
        ti = 0
        for piece in range(6):
            slot, wb, wt = pend
            if piece + 1 < 6:
                pend = load_in((piece + 1) * 512)
            tlast = None
            for mm in range(4):
                m = piece * 4 + mm
                g = m // 3
                for tt in range(NTT):
                    cs = slice(tt * 512, (tt + 1) * 512)
                    bu, buf_ = banks.next()
                    tm = None
                    for k in range(KC):
                        tm = ph.op("pe", lambda e, bu=bu, k=k, mm=mm, cs=cs, wb=wb: e.matmul(
                            ps[:, bu, :], lhsT=wb[:, k, mm * 128:(mm + 1) * 128], rhs=xn[:, k, cs],
                            start=(k == 0), stop=(k == KC - 1)), waits=[buf_] + wt, sig=(k == KC - 1))
                    tlast = tm
                    us = ui % 4
                    ui += 1
                    tu = ph.op("act", lambda e, bu=bu, us=us: e.activation(out=usb[us][:], in_=ps[:, bu, :], func=AF.Gelu),
                               waits=[tm, ufree[us]])
                    banks.release(bu, tu)
                    bs, bsf = banks.next()
                    tsm = None
                    for q in range(4):
                        t = tt * 4 + q
                        tsm = ph.op("pe", lambda e, bs=bs, q=q, t=t, m=m, g=g: e.matmul(
                            ps[:, bs, q * 128:(q + 1) * 128], lhsT=vg[:, t, m * 128:(m + 1) * 128],
                            rhs=wts[:, t, g * 128:(g + 1) * 128], start=True, stop=True), waits=[bsf, tw], sig=(q == 3))
                    tlast = tsm
                    t1s = ti % 2
                    ti += 1
                    ta = ph.op("dve", lambda e, bs=bs, m=m, g=g, t1s=t1s: e.scalar_tensor_tensor(
                        out=t1b[t1s][:].rearrange("p (r i) -> p r i", r=4), in0=ps[:, bs, :].rearrange("p (r i) -> p r i", r=4),
                        scalar=self.vgain[:, m:m + 1], in1=bcast_mid(self.biasbc[:, g, :], 4), op0=ALU.mult, op1=ALU.add),
                        waits=[tsm, t1free[t1s]])
                    banks.release(bs, ta)
                    tb = ph.op("dve", lambda e, m=m, cs=cs, t1s=t1s, us=us: e.tensor_tensor(
                        out=pT[:, m, cs], in0=t1b[t1s][:], in1=usb[us][:], op=ALU.mult), waits=[ta, tu])
                    ufree[us] = tb
                    t1free[t1s] = tb
                    p_last = tb
            ws.release(slot, tlast)
        ws2 = WStream(ph, "w2", None, 2, bufs=[big1[:, i * 6144:(i + 1) * 6144].rearrange("p (k n) -> p k n", k=24) for i in range(2)],
                      free0=tlast)

        def load_out(c0):
            return ws2.load([lambda e, buf, c0=c0: e.dma_start(
                out=buf, in_=w_out[:, c0:c0 + 256].rearrange("(k p) n -> p k n", p=128))])
        pend2 = load_out(0)
        for piece in range(4):
            slot, wb, wt = pend2
            if piece + 1 < 4:
                pend2 = load_out((piece + 1) * 256)
            tm = None
            for mm in range(2):
                mo = piece * 2 + mm
                for tt in range(NTT):
                    cs = slice(tt * 512, (tt + 1) * 512)
                    b, bf = banks.next()
                    for k in range(24):
                        tm = ph.op("pe", lambda e, b=b, k=k, mm=mm, cs=cs, wb=wb: e.matmul(
                            ps[:, b, :], lhsT=wb[:, k, mm * 128:(mm + 1) * 128], rhs=pT[:, k, cs],
                            start=(k == 0), stop=(k == 23)), waits=[bf, p_last] + wt, sig=(k == 23))
                    ta = ph.op("dve", lambda e, b=b, mo=mo, cs=cs: e.tensor_tensor(
                        out=self.hT[:, mo, cs], in0=self.hT[:, mo, cs], in1=ps[:, b, :], op=ALU.add), waits=[tm])
                    banks.release(b, ta)
            ws2.release(slot, tm)
        ph.emit()

    def emit_swiglu(self, ph, ps, banks, xn, xall, w_in, w_out, dff, hbuf, ws, ws2, gate_bc=None, gate_tok=None, tmpb=None,
                    next_w_in=None):
        nj = dff // 128
        npiece = nj // 2

        def load_in(p, w_in=w_in):
            c0 = p * 256
            return ws.load([
                lambda e, buf, c0=c0: e.dma_start(out=buf[:, :, 0:256], in_=w_in[:, c0:c0 + 256].rearrange("(k p) n -> p k n", p=128)),
                lambda e, buf, c0=c0: e.dma_start(out=buf[:, :, 256:512],
                                                  in_=w_in[:, dff + c0:dff + c0 + 256].rearrange("(k p) n -> p k n", p=128)),
            ])

        def load_out(c0):
            return ws2.load([lambda e, buf, c0=c0: e.dma_start(
                out=buf[:, 0:nj, :], in_=w_out[:, c0:c0 + 256].rearrange("(k p) n -> p k n", p=128))])
        pre = getattr(self, "_pre_in", None)
        self._pre_in = None
        pend = pre if pre is not None else load_in(0)
        pend2 = None
        si = 0
        h_last = None
        for p in range(npiece):
            slot, wb, wt = pend
            if p + 1 < npiece:
                pend = load_in(p + 1)
            if p == 0:
                pend2 = load_out(0)
            tlast = None
            for jj in range(2):
                j = p * 2 + jj
                for tt in range(NTT):
                    cs = slice(tt * 512, (tt + 1) * 512)
                    ba, baf = banks.next()
                    bb, bbf = banks.next()
                    ta_ = tb_ = None
                    for k in range(KC):
                        ta_ = ph.op("pe", lambda e, ba=ba, k=k, jj=jj, cs=cs, wb=wb: e.matmul(
                            ps[:, ba, :], lhsT=wb[:, k, jj * 128:(jj + 1) * 128], rhs=xn[:, k, cs],
                            start=(k == 0), stop=(k == KC - 1)), waits=[baf, xall] + wt, sig=(k == KC - 1))
                    for k in range(KC):
                        tb_ = ph.op("pe", lambda e, bb=bb, k=k, jj=jj, cs=cs, wb=wb: e.matmul(
                            ps[:, bb, :], lhsT=wb[:, k, 256 + jj * 128:256 + (jj + 1) * 128], rhs=xn[:, k, cs],
                            start=(k == 0), stop=(k == KC - 1)), waits=[bbf], sig=(k == KC - 1))
                    tlast = tb_
                    s = si % 2
                    si += 1
                    tsl = ph.op("act", lambda e, ba=ba, s=s: e.activation(out=self._silu[s][:], in_=ps[:, ba, :], func=AF.Silu),
                                waits=[ta_, self._silu_free[s]])
                    banks.release(ba, tsl)
                    th = ph.op("dve", lambda e, bb=bb, s=s, j=j, cs=cs: e.tensor_tensor(
                        out=hbuf[:, j, cs], in0=self._silu[s][:], in1=ps[:, bb, :], op=ALU.mult),
                        waits=[tsl, tb_, self._h_free])
                    banks.release(bb, th)
                    self._silu_free[s] = th
                    h_last = th
            ws.release(slot, tlast)
        if next_w_in is not None:
            self._pre_in = load_in(0, next_w_in)
        tm = None
        for piece in range(4):
            slot, wb, wt = pend2
            if piece + 1 < 4:
                pend2 = load_out((piece + 1) * 256)
            for mm in range(2):
                mo = piece * 2 + mm
                for tt in range(NTT):
                    cs = slice(tt * 512, (tt + 1) * 512)
                    b, bf = banks.next()
                    for k in range(nj):
                        tm = ph.op("pe", lambda e, b=b, k=k, mm=mm, cs=cs, wb=wb: e.matmul(
                            ps[:, b, :], lhsT=wb[:, k, mm * 128:(mm + 1) * 128], rhs=hbuf[:, k, cs],
                            start=(k == 0), stop=(k == nj - 1)), waits=[bf, h_last] + wt, sig=(k == nj - 1))
                    if gate_bc is None:
                        ta = ph.op("dve", lambda e, b=b, mo=mo, cs=cs: e.tensor_tensor(
                            out=self.hT[:, mo, cs], in0=self.hT[:, mo, cs], in1=ps[:, b, :], op=ALU.add), waits=[tm])
                        banks.release(b, ta)
                    else:
                        s = self._tmp_i % 2
                        self._tmp_i += 1
                        tq = ph.op("dve", lambda e, b=b, cs=cs, s=s: e.tensor_tensor(
                            out=tmpb[s][:], in0=ps[:, b, :], in1=gate_bc[:, cs], op=ALU.mult),
                            waits=[tm, gate_tok, self._tmp_free[s]])
                        banks.release(b, tq)
                        ta = ph.op("dve", lambda e, mo=mo, cs=cs, s=s: e.tensor_tensor(
                            out=self.hT[:, mo, cs], in0=self.hT[:, mo, cs], in1=tmpb[s][:], op=ALU.add), waits=[tq])
                        self._tmp_free[s] = ta
            ws2.release(slot, tm)
        self._h_free = tm
        return tm

    def ph_ffn(self):
        I = self.I
        ph = self.phase("ffn")
        self.norm_state()
        ps = ph.psum()
        banks = Banks(ps, range(8))
        xn = ph.alloc("xn", [128, KC, ST], BF16)
        sq = ph.alloc("sq", [128, KC, 512], BF16)
        rstd = [ph.alloc(f"rstd{i}", [128, 512], F32) for i in range(NTT)]
        hbuf = ph.alloc("hbuf", [128, D_FF // 128, ST], BF16)
        self._silu = [ph.alloc(f"silu{i}", [128, 512], F32) for i in range(2)]
        self._silu_free = [None, None]
        self._h_free = None
        ws = WStream(ph, "w", [128, KC, 512], 2)
        ws2 = WStream(ph, "w2", [128, D_FF // 128, 256], 2)
        xtoks, _ = self.emit_norm(ph, ps, banks, 1, xn, sq, rstd)
        self.emit_swiglu(ph, ps, banks, xn, xtoks[-1], I["f_w_in"][0], I["f_w_out"][0], D_FF, hbuf, ws, ws2)
        ph.emit()

    def emit_headnorm(self, ph, ps, banks, src_bank, src_tok, gcol, out_ap, raw, sqb, rsb, free_tok):
        t_raw = ph.op("act", lambda e: e.activation(out=raw[:], in_=ps[:, src_bank, :], func=AF.Copy), waits=[src_tok, free_tok])
        t_sq = ph.op("act", lambda e: e.activation(out=sqb[:], in_=ps[:, src_bank, :], func=AF.Square), waits=[src_tok, free_tok])
        banks.release(src_bank, t_sq)
        b, bf = banks.next()
        tm = ph.op("pe", lambda e: e.matmul(ps[:, b, :], lhsT=self.blk_bf[:], rhs=sqb[:], start=True, stop=True), waits=[t_sq, bf])
        t1, t2 = self.emit_rsqrt(ph, rsb[:], ps[:, b, :], 1.0 / 64, [tm, free_tok])
        banks.release(b, t1)
        t3 = ph.op("dve", lambda e: e.scalar_tensor_tensor(out=out_ap, in0=raw[:], scalar=self.kqg[:, gcol:gcol + 1], in1=rsb[:],
                                                           op0=ALU.mult, op1=ALU.mult), waits=[t2, t_raw])
        return t3

    def ph_kv(self, st):
        I = self.I
        ph = self.phase("kv")
        self.norm_state()
        ps = ph.psum()
        banks = Banks(ps, range(8))
        xn = ph.alloc("xn", [128, KC, ST], BF16)
        sq = ph.alloc("sq", [128, KC, 512], BF16)
        rstd = [ph.alloc(f"rstd{i}", [128, 512], F32) for i in range(NTT)]
        KT = ph.alloc("KT", [128, KC, ST], BF16)
        Vs = ph.alloc("Vs", [128, NT, D], BF16)
        wf = ph.alloc("wf", [128, KC, 16], BF16)
        raws = [ph.alloc(f"raw{i}", [128, 512], F32) for i in range(2)]
        sqbs = [ph.alloc(f"sqb{i}", [128, 512], BF16) for i in range(2)]
        rsbs = [ph.alloc(f"rsb{i}", [128, 512], F32) for i in range(2)]
        ef = ph.alloc("ef", [128, NT, 16], F32)
        ws = WStream(ph, "w", [128, KC, 512], 2)
        kv_w = I["kv_w"]
        xtoks, _ = self.emit_norm(ph, ps, banks, 2, xn, sq, rstd)
        xall = xtoks[-1]
        tf_w = ph.dma("pool", lambda e: e.dma_start(out=wf[:], in_=kv_w[:, 2 * D:2 * D + 16].rearrange("(k p) n -> p k n", p=128)), "wf")

        def load(col0):
            return ws.load([lambda e, buf, col0=col0: e.dma_start(
                out=buf[:], in_=kv_w[:, col0:col0 + 512].rearrange("(k p) n -> p k n", p=128))])
        pend = load(0)
        hfree = [None, None]
        hi = 0
        k_last = None
        for piece in range(2):
            slot, wb, wt = pend
            pend = load((piece + 1) * 512) if piece == 0 else load(D)
            tm = None
            for mm in range(4):
                m = piece * 4 + mm
                for tt in range(NTT):
                    cs = slice(tt * 512, (tt + 1) * 512)
                    b, bf = banks.next()
                    for k in range(KC):
                        tm = ph.op("pe", lambda e, b=b, k=k, mm=mm, cs=cs, wb=wb: e.matmul(
                            ps[:, b, :], lhsT=wb[:, k, mm * 128:(mm + 1) * 128], rhs=xn[:, k, cs],
                            start=(k == 0), stop=(k == KC - 1)), waits=[bf, xall] + wt, sig=(k == KC - 1))
                    s = hi % 2
                    hi += 1
                    k_last = self.emit_headnorm(ph, ps, banks, b, tm, 0, KT[:, m, cs], raws[s], sqbs[s], rsbs[s], hfree[s])
                    hfree[s] = k_last
            ws.release(slot, tm)
        tk_st = ph.dma("sp", lambda e: e.dma_start(out=self.KT_d[:, :, st * ST:(st + 1) * ST], in_=KT[:]), "kst", waits=[k_last])
        v_last = None
        for n in range(2):
            slot, wb, wt = pend
            if n == 0:
                pend = load(D + 512)
            tm = None
            for t in range(NT):
                b, bf = banks.next()
                for k in range(KC):
                    tm = ph.op("pe", lambda e, b=b, k=k, t=t, wb=wb: e.matmul(
                        ps[:, b, :], lhsT=xn[:, k, t * 128:(t + 1) * 128], rhs=wb[:, k, :], start=(k == 0), stop=(k == KC - 1)),
                        waits=[bf, xall] + wt, sig=(k == KC - 1))
                if t % 2 == 0:
                    v_last = ph.op("act", lambda e, b=b, t=t, n=n: e.activation(out=Vs[:, t, n * 512:(n + 1) * 512], in_=ps[:, b, :],
                                                                                func=AF.Copy), waits=[tm])
                else:
                    v_last = ph.op("dve", lambda e, b=b, t=t, n=n: e.tensor_copy(out=Vs[:, t, n * 512:(n + 1) * 512], in_=ps[:, b, :]),
                                   waits=[tm])
                banks.release(b, v_last)
                if t == NT - 2:
                    v_prev = v_last
            ws.release(slot, tm)
        for c in range(KC):
            ph.dma("sp", lambda e, c=c: e.dma_start(out=self.V_d[:, c, st * NT:(st + 1) * NT, :], in_=Vs[:, :, c * 128:(c + 1) * 128]),
                   "vst", waits=[v_last, v_prev])
        b, bf = banks.next()
        tm = None
        for t in range(NT):
            for k in range(KC):
                tm = ph.op("pe", lambda e, b=b, k=k, t=t: e.matmul(
                    ps[:, b, t * 16:(t + 1) * 16], lhsT=xn[:, k, t * 128:(t + 1) * 128], rhs=wf[:, k, :],
                    start=(k == 0), stop=(k == KC - 1)), waits=[bf, xall, tf_w], sig=(k == KC - 1 and t == NT - 1))
        g0 = st * NT
        t1 = ph.op("dve", lambda e: e.tensor_tensor(out=ef[:], in0=ps[:, b, 0:NT * 16].rearrange("p (t h) -> p t h", t=NT),
                                                    in1=bcast_mid(self.bf_bc[:], NT), op=ALU.add), waits=[tm])
        banks.release(b, t1)
        t2 = ph.op("act", lambda e: e.activation(out=ef[:], in_=ef[:], func=AF.Exp, scale=-1.0), waits=[t1])
        t3 = ph.op("act", lambda e: e.activation(out=ef[:], in_=ef[:], func=AF.Ln, bias=self.onec[:], scale=1.0), waits=[t2])
        t4 = ph.op("dve", lambda e: e.tensor_scalar(out=self.logf[:, g0:g0 + NT, :], in0=ef[:], scalar1=-1.0, scalar2=None, op0=ALU.mult),
                   waits=[t3])
        b2, b2f = banks.next()
        tm = None
        for t in range(NT):
            T = g0 + t
            for tp in range(T + 1):
                tm = ph.op("pe", lambda e, b2=b2, t=t, tp=tp, T=T: e.matmul(
                    ps[:, b2, t * 16:(t + 1) * 16], lhsT=(self.tri_f[:] if tp == T else self.ones_f[:]), rhs=self.logf[:, tp, :],
                    start=(tp == 0), stop=(tp == T)), waits=[b2f, t4], sig=(tp == T and t == NT - 1))
        t5 = ph.op("dve", lambda e: e.tensor_copy(out=self.Fcum[:, g0:g0 + NT, :],
                                                  in_=ps[:, b2, 0:NT * 16].rearrange("p (t h) -> p t h", t=NT)), waits=[tm])
        banks.release(b2, t5)
        ph.emit()

    def ph_mixer_b(self, st):
        I = self.I
        ph = self.phase("mixB")
        self.norm_state()
        ps = ph.psum()
        banks = Banks(ps, range(6))
        xn = ph.alloc("xn", [128, KC, ST], BF16)
        rstd = [ph.alloc(f"rstd{i}", [128, 512], F32) for i in range(NTT)]
        QT = ph.alloc("QT", [128, KC, ST], BF16)
        SG = ph.alloc("SG", [128, KC, ST], BF16)
        OT = ph.alloc("OT", [128, KC, ST], BF16)
        sq = OT[:, 0:4, :].rearrange("p a (b n) -> p (a b) n", b=2)
        raws = [ph.alloc(f"raw{i}", [128, 512], F32) for i in range(2)]
        sqbs = [ph.alloc(f"sqb{i}", [128, 512], BF16) for i in range(2)]
        rsbs = [ph.alloc(f"rsb{i}", [128, 512], F32) for i in range(2)]
        ntk = (st + 1) * NT
        L = ntk * 128
        KbA = [ph.alloc(f"KbA{i}", [128, SEQ], BF16) for i in range(2)]
        KbB = [ph.alloc(f"KbB{i}", [128, SEQ], BF16) for i in range(2)]
        Vb = [ph.alloc(f"Vb{i}", [128, SEQ // 128, 2, 128], BF16) for i in range(2)]
        nb = ph.alloc("nb", [128, NTT, SEQ // 128, 16], F32)
        cq = ph.alloc("cq", [128, NTT, 16], F32)
        PT = [ph.alloc(f"PT{i}", [128, 512], BF16) for i in range(6)]
        Rt = [ph.alloc(f"Rt{i}", [128, 512], F32) for i in range(2)]
        Rs = [ph.alloc(f"Rs{i}", [128, 512], F32) for i in range(2)]
        ws = WStream(ph, "w", [128, KC, 512], 2)
        wo = ph.alloc("wo", [128, KC, D], BF16)
        w_in = I["b_w_in"][0]
        xtoks, _ = self.emit_norm(ph, ps, banks, 3, xn, sq, rstd)
        xall = xtoks[-1]
        t_wo = ph.dma("pool", lambda e: e.dma_start(out=wo[:], in_=I["b_w_out"][0].rearrange("(k p) n -> p k n", p=128)), "wo")
        tvo = None
        for i in range(2):
            ph.op("dve", lambda e, i=i: e.memset(Vb[i][:, :, 0, 64:128], 1.0), sig=False)
            ph.op("dve", lambda e, i=i: e.memset(KbA[i][64:128, :], 0.0), sig=False)
            ph.op("dve", lambda e, i=i: e.memset(KbB[i][0:64, :], 0.0), sig=False)
            tvo = ph.op("dve", lambda e, i=i: e.memset(Vb[i][:, :, 1, 0:64], 1.0))

        def load(col0):
            return ws.load([lambda e, buf, col0=col0: e.dma_start(
                out=buf[:], in_=w_in[:, col0:col0 + 512].rearrange("(k p) n -> p k n", p=128))])
        pend = load(0)
        hfree = [None, None]
        hi = 0
        q_last = None
        g_last = None
        for piece in range(4):
            slot, wb, wt = pend
            if piece + 1 < 4:
                pend = load((piece + 1) * 512)
            tm = None
            for mm in range(4):
                m = (piece % 2) * 4 + mm
                for tt in range(NTT):
                    cs = slice(tt * 512, (tt + 1) * 512)
                    b, bf = banks.next()
                    for k in range(KC):
                        tm = ph.op("pe", lambda e, b=b, k=k, mm=mm, cs=cs, wb=wb: e.matmul(
                            ps[:, b, :], lhsT=wb[:, k, mm * 128:(mm + 1) * 128], rhs=xn[:, k, cs],
                            start=(k == 0), stop=(k == KC - 1)), waits=[bf, xall] + wt, sig=(k == KC - 1))
                    if piece < 2:
                        s = hi % 2
                        hi += 1
                        q_last = self.emit_headnorm(ph, ps, banks, b, tm, 1, QT[:, m, cs], raws[s], sqbs[s], rsbs[s], hfree[s])
                        hfree[s] = q_last
                    else:
                        g_last = ph.op("act", lambda e, b=b, m=m, cs=cs: e.activation(out=SG[:, m, cs], in_=ps[:, b, :], func=AF.Sigmoid),
                                       waits=[tm])
                        banks.release(b, g_last)
            ws.release(slot, tm)
        bq, bqf = banks.next()
        tm = None
        for Q in range(NTT):
            Qa = st * NTT + Q
            npre = Qa * 4
            if npre == 0:
                continue
            for tp in range(npre):
                tm = ph.op("pe", lambda e, Q=Q, tp=tp, npre=npre: e.matmul(
                    ps[:, bq, Q * 16:(Q + 1) * 16], lhsT=self.ones_f[:], rhs=self.logf[:, tp, :], start=(tp == 0), stop=(tp == npre - 1)),
                    waits=[bqf])
        tcq = None
        for Q in range(NTT):
            Qa = st * NTT + Q
            if Qa == 0:
                tcq = ph.op("dve", lambda e, Q=Q: e.memset(cq[:, Q, :], 0.0))
            else:
                tcq = ph.op("dve", lambda e, Q=Q: e.tensor_copy(out=cq[:, Q, :], in_=ps[:, bq, Q * 16:(Q + 1) * 16]), waits=[tm])
        banks.release(bq, tcq)
        tnb = None
        for Q in range(NTT):
            tnb0 = ph.op("dve", lambda e, Q=Q: e.tensor_scalar(out=nb[:, Q, 0:ntk, :], in0=self.Fcum[:, 0:ntk, :], scalar1=-1.0,
                                                               scalar2=-SM_BOUND, op0=ALU.mult, op1=ALU.add), waits=[tcq])
            if st * NTT + Q == 0:
                tnb = tnb0
                continue
            tnb = ph.op("dve", lambda e, Q=Q: e.tensor_tensor(out=nb[:, Q, 0:ntk, :], in0=nb[:, Q, 0:ntk, :],
                                                              in1=bcast_mid(cq[:, Q, :], ntk), op=ALU.add), waits=[tnb0])
        NS, LA, DELAY = len(PT), 3, 8
        abanks = Banks(ps, range(4))
        for b_ in range(4):
            abanks.free[b_] = banks.free[b_]
        pvfree = {4: banks.free[4], 5: banks.free[5], 6: None, 7: None}
        kvfree = [None, None]
        pfree = [None] * NS
        work = []
        gi = 0
        for c in range(KC):
            for Q in range(NTT):
                Qa = st * NTT + Q
                nk = (Qa + 1) * 4
                for t in range(nk):
                    for hh in range(2):
                        work.append(dict(c=c, Q=Q, Qa=Qa, nk=nk, hh=hh, t=t, g=gi, par=gi % 2,
                                         first_pair=(Q == 0 and hh == 0 and t == 0),
                                         last_head=(t == nk - 1), last_g=(hh == 1 and t == nk - 1),
                                         last_pair=(Q == NTT - 1 and hh == 1 and t == nk - 1)))
                gi += 1
        n = len(work)
        ktok = {}
        vtok = {}
        tready = {}
        acc = {}
        pending = []
        o_last = None

        def finalize(c, Q, par, a0, a1):
            nonlocal o_last
            cs0 = Q * 512
            pa, pb_ = 4 + 2 * par, 5 + 2 * par
            ta = ph.op("act", lambda e: e.activation(out=Rt[par][0:64, :], in_=ps[0:64, pb_, :], func=AF.Ln), waits=[a1, self._rt_free[par]])
            tb = ph.op("act", lambda e: e.activation(out=Rt[par][64:128, :], in_=ps[64:128, pa, :], func=AF.Ln), waits=[a0, self._rt_free[par]])
            b, bf = abanks.next()
            tsw = ph.op("pe", lambda e: e.matmul(ps[:, b, :], lhsT=self.swap_f[:], rhs=Rt[par][:], start=True, stop=True),
                        waits=[ta, tb, bf])
            self._rt_free[par] = tsw
            tcp = ph.op("act", lambda e: e.activation(out=Rs[par][:], in_=ps[:, b, :], func=AF.Exp, scale=-1.0),
                        waits=[tsw, self._rs2_free[par]])
            abanks.release(b, tcp)
            tg = ph.op("dve", lambda e: e.tensor_tensor(out=Rs[par][:], in0=Rs[par][:], in1=SG[:, c, cs0:cs0 + 512], op=ALU.mult),
                       waits=[tcp, g_last])
            to0 = ph.op("dve", lambda e: e.tensor_tensor(out=OT[0:64, c, cs0:cs0 + 512], in0=ps[0:64, pa, :],
                                                         in1=Rs[par][0:64, :], op=ALU.mult), waits=[tg])
            to1 = ph.op("dve", lambda e: e.tensor_tensor(out=OT[64:128, c, cs0:cs0 + 512], in0=ps[64:128, pb_, :],
                                                         in1=Rs[par][64:128, :], op=ALU.mult), waits=[tg])
            pvfree[pa] = to0
            pvfree[pb_] = to1
            self._rs2_free[par] = to1
            o_last = to1

        for i in range(n + LA):
            if i < n:
                w = work[i]
                c, Q, Qa, hh, t = w["c"], w["Q"], w["Qa"], w["hh"], w["t"]
                sl = c % 2
                if w["first_pair"]:
                    ph.dma("sp", lambda e, c=c, sl=sl: e.dma_start(out=KbA[sl][0:64, 0:L], in_=self.KT_d[0:64, c, 0:L]), f"kb{sl}",
                           waits=[kvfree[sl]])
                    ktok[c] = ph.dma("sp", lambda e, c=c, sl=sl: e.dma_start(out=KbB[sl][64:128, 0:L], in_=self.KT_d[64:128, c, 0:L]),
                                     f"kb{sl}", waits=[kvfree[sl]])
                    for h2 in range(2):
                        off = 0 if h2 == 0 else 64
                        vtok[c] = ph.dma("sp", lambda e, c=c, sl=sl, h2=h2, off=off: e.dma_start(
                            out=Vb[sl][:, 0:ntk, h2, off:off + 64], in_=self.V_d[:, c, 0:ntk, off:off + 64]),
                            f"vb{sl}", waits=[kvfree[sl], tvo])
                h = c * 2 + hh
                Kh = KbA if hh == 0 else KbB
                r = t - Qa * 4
                c0 = max(r, 0) * 128
                cs0 = Q * 512
                b, bf = abanks.next()
                tqk = ph.op("pe", lambda e, b=b, t=t, c0=c0, Kh=Kh, sl=sl, c=c, cs0=cs0: e.matmul(
                    ps[:, b, c0:512], lhsT=Kh[sl][:, t * 128:(t + 1) * 128], rhs=QT[:, c, cs0 + c0:cs0 + 512],
                    start=True, stop=True), waits=[bf, ktok[c], q_last, tvo])
                s_ = i % NS
                tex = ph.op("act", lambda e, b=b, t=t, c0=c0, s_=s_, Q=Q, h=h: e.activation(
                    out=PT[s_][:, c0:512], in_=ps[:, b, c0:512], func=AF.Exp, bias=nb[:, Q, t, h:h + 1], scale=1.0),
                    waits=[tqk, pfree[s_], tnb])
                abanks.release(b, tex)
                tr_ = tex
                if r >= 0:
                    tr_ = ph.op("dve", lambda e, s_=s_, c0=c0: e.tensor_tensor(
                        out=PT[s_][:, c0:c0 + 128], in0=PT[s_][:, c0:c0 + 128], in1=self.trimask[:], op=ALU.mult), waits=[tex])
                tready[i] = (tr_, c0, s_)
            j = i - LA
            if j >= 0:
                w = work[j]
                c, Q, hh, t, nk, par = w["c"], w["Q"], w["hh"], w["t"], w["nk"], w["par"]
                sl = c % 2
                pb = 4 + 2 * par + hh
                tr_, c0, s_ = tready.pop(j)
                tpv = ph.op("pe", lambda e, pb=pb, t=t, c0=c0, s_=s_, sl=sl, hh=hh, nk=nk: e.matmul(
                    ps[:, pb, c0:512], lhsT=Vb[sl][:, t, hh, :], rhs=PT[s_][:, c0:512], start=(t == 0), stop=(t == nk - 1)),
                    waits=[tr_, vtok[c], pvfree[pb] if t == 0 else None])
                pfree[s_] = tpv
                if w["last_head"]:
                    acc[(w["g"], hh)] = tpv
                if w["last_pair"]:
                    kvfree[sl] = tpv
                if w["last_g"]:
                    pending.append((j + DELAY, c, Q, par, acc.pop((w["g"], 0)), acc.pop((w["g"], 1))))
                while pending and pending[0][0] <= j:
                    _, c_, Q_, par_, a0, a1 = pending.pop(0)
                    finalize(c_, Q_, par_, a0, a1)
        while pending:
            _, c_, Q_, par_, a0, a1 = pending.pop(0)
            finalize(c_, Q_, par_, a0, a1)
        banks = abanks
        if self.cfg.get("dbg") and st == 0:
            tdd = None
            for c in range(KC):
                for tt in range(NTT):
                    cs = slice(tt * 512, (tt + 1) * 512)
                    td = ph.op("act", lambda e, c=c, cs=cs: e.activation(out=raws[0][:], in_={'OT': OT, 'QT': QT, 'SG': SG, 'XN': xn}[self.cfg['dbg']][:, c, cs], func=AF.Copy), waits=[o_last, tdd])
                    tdd = ph.dma("sp", lambda e, c=c, cs=cs: e.dma_start(out=self.dbg[:, c, cs], in_=raws[0][:]), "dbg", waits=[td])
        tm = None
        for mo in range(KC):
            for tt in range(NTT):
                cs = slice(tt * 512, (tt + 1) * 512)
                b, bf = banks.next()
                for k in range(KC):
                    tm = ph.op("pe", lambda e, b=b, k=k, mo=mo, cs=cs: e.matmul(
                        ps[:, b, :], lhsT=wo[:, k, mo * 128:(mo + 1) * 128], rhs=OT[:, k, cs], start=(k == 0), stop=(k == KC - 1)),
                        waits=[bf, o_last, t_wo], sig=(k == KC - 1))
                ta = ph.op("dve", lambda e, b=b, mo=mo, cs=cs: e.tensor_tensor(
                    out=self.hT[:, mo, cs], in0=self.hT[:, mo, cs], in1=ps[:, b, :], op=ALU.add), waits=[tm])
                banks.release(b, ta)
        ph.emit()

    def ph_moe(self):
        I = self.I
        ph = self.phase("moe")
        self.norm_state()
        ps = ph.psum()
        banks = Banks(ps, range(8))
        xn = ph.alloc("xn", [128, KC, ST], BF16)
        sq = ph.alloc("sq", [128, KC, 512], BF16)
        rstd = [ph.alloc(f"rstd{i}", [128, 512], F32) for i in range(NTT)]
        hbuf = ph.alloc("hbuf", [128, D_EXP // 128, ST], BF16)
        self._silu = [ph.alloc(f"silu{i}", [128, 512], F32) for i in range(2)]
        self._silu_free = [None, None]
        self._h_free = None
        tmpb = [ph.alloc(f"tmpb{i}", [128, 512], F32) for i in range(2)]
        self._tmp_free = [None, None]
        self._tmp_i = 0
        lgT = ph.alloc("lgT", [8, ST], F32)
        lg = ph.alloc("lg", [128, NT, NEXP], F32)
        m1 = ph.alloc("m1", [128, NT], F32)
        m2 = ph.alloc("m2", [128, NT], F32)
        mk1 = ph.alloc("mk1", [128, NT, NEXP], F32)
        mk2 = ph.alloc("mk2", [128, NT, NEXP], F32)
        lg2 = ph.alloc("lg2", [128, NT, NEXP], F32)
        g1 = ph.alloc("g1", [128, NT], F32)
        g2 = ph.alloc("g2", [128, NT], F32)
        gates = ph.alloc("gates", [128, NT, NEXP], F32)
        gT = ph.alloc("gT", [8, ST], F32)
        gbc = [ph.alloc(f"gbc{i}", [128, ST], F32) for i in range(2)]
        ws = WStream(ph, "w", [128, KC, 512], 2)
        ws2 = WStream(ph, "w2", [128, D_EXP // 128, 256], 2)
        xtoks, rtoks = self.emit_norm(ph, ps, banks, 4, xn, sq, rstd)
        xall = xtoks[-1]
        tl = None
        for tt in range(NTT):
            cs = slice(tt * 512, (tt + 1) * 512)
            b, bf = banks.next()
            tm = None
            for k in range(KC):
                tm = ph.op("pe", lambda e, b=b, k=k, cs=cs: e.matmul(ps[0:8, b, :], lhsT=self.gwr[:, k, :], rhs=self.hT[:, k, cs],
                                                                     start=(k == 0), stop=(k == KC - 1)), waits=[bf], sig=(k == KC - 1))
            tl = ph.op("dve", lambda e, b=b, cs=cs, tt=tt: e.tensor_tensor(out=lgT[:, cs], in0=ps[0:8, b, :], in1=rstd[tt][0:8, :],
                                                                           op=ALU.mult), waits=[tm, rtoks[tt]])
            banks.release(b, tl)
        b, bf = banks.next()
        tp = None
        for t in range(NT):
            tp = ph.op("pe", lambda e, b=b, t=t: e.transpose(ps[:, b, t * 8:(t + 1) * 8], lgT[:, t * 128:(t + 1) * 128], self.ident[0:8, 0:8]),
                       waits=[bf, tl], sig=(t == NT - 1))
        t0 = ph.op("dve", lambda e, b=b: e.tensor_copy(out=lg[:], in_=ps[:, b, 0:NT * 8].rearrange("p (t x) -> p t x", t=NT)), waits=[tp])
        banks.release(b, t0)
        t1 = ph.op("dve", lambda e: e.tensor_reduce(out=m1[:], in_=lg[:], axis=AX.X, op=ALU.max), waits=[t0])
        tk1 = None
        for t in range(NT):
            tk1 = ph.op("dve", lambda e, t=t: e.tensor_scalar(out=mk1[:, t, :], in0=lg[:, t, :], scalar1=m1[:, t:t + 1], scalar2=None,
                                                              op0=ALU.is_equal), waits=[t1])
        t2 = ph.op("dve", lambda e: e.scalar_tensor_tensor(out=lg2[:], in0=mk1[:], scalar=-1e30, in1=lg[:], op0=ALU.mult, op1=ALU.add),
                   waits=[tk1])
        t3 = ph.op("dve", lambda e: e.tensor_reduce(out=m2[:], in_=lg2[:], axis=AX.X, op=ALU.max), waits=[t2])
        tk2 = None
        for t in range(NT):
            tk2 = ph.op("dve", lambda e, t=t: e.tensor_scalar(out=mk2[:, t, :], in0=lg2[:, t, :], scalar1=m2[:, t:t + 1], scalar2=None,
                                                              op0=ALU.is_equal), waits=[t3])
        t4 = ph.op("dve", lambda e: e.tensor_tensor(out=g1[:], in0=m1[:], in1=m2[:], op=ALU.subtract), waits=[t3])
        t5 = ph.op("act", lambda e: e.activation(out=g1[:], in_=g1[:], func=AF.Sigmoid), waits=[t4])
        t6 = ph.op("dve", lambda e: e.tensor_scalar(out=g2[:], in0=g1[:], scalar1=-1.0, scalar2=1.0, op0=ALU.mult, op1=ALU.add), waits=[t5])
        tg = None
        for t in range(NT):
            ta = ph.op("dve", lambda e, t=t: e.tensor_scalar(out=mk1[:, t, :], in0=mk1[:, t, :], scalar1=g1[:, t:t + 1], scalar2=None,
                                                             op0=ALU.mult), waits=[t5, tk2])
            tb = ph.op("dve", lambda e, t=t: e.scalar_tensor_tensor(out=gates[:, t, :], in0=mk2[:, t, :], scalar=g2[:, t:t + 1],
                                                                    in1=mk1[:, t, :], op0=ALU.mult, op1=ALU.add), waits=[ta, t6])
            tg = tb
        tgt = None
        for half in range(2):
            b, bf = banks.next()
            tp = None
            for q in range(4):
                t = half * 4 + q
                tp = ph.op("pe", lambda e, b=b, q=q, t=t: e.transpose(ps[0:8, b, q * 128:(q + 1) * 128], gates[:, t, :], self.ident[:]),
                           waits=[bf, tg], sig=(q == 3))
            tgt = ph.op("dve", lambda e, b=b, half=half: e.tensor_copy(out=gT[:, half * 512:(half + 1) * 512], in_=ps[0:8, b, :]), waits=[tp])
            banks.release(b, tgt)
        gfree = [None, None]
        for ex in range(NEXP):
            gs = ex % 2
            tgb = None
            for tt in range(NTT):
                cs = slice(tt * 512, (tt + 1) * 512)
                b, bf = banks.next()
                tm = ph.op("pe", lambda e, b=b, ex=ex, cs=cs: e.matmul(ps[:, b, :], lhsT=self.sel8[:, ex, :], rhs=gT[:, cs], start=True, stop=True),
                           waits=[bf, tgt])
                tgb = ph.op("act", lambda e, b=b, gs=gs, cs=cs: e.activation(out=gbc[gs][:, cs], in_=ps[:, b, :], func=AF.Copy),
                            waits=[tm, gfree[gs]])
                banks.release(b, tgb)
            last = self.emit_swiglu(ph, ps, banks, xn, xall, I["m_w_in"][0, ex], I["m_w_out"][0, ex], D_EXP, hbuf, ws, ws2,
                                    gate_bc=gbc[gs], gate_tok=tgb, tmpb=tmpb,
                                    next_w_in=(I["m_w_in"][0, ex + 1] if ex + 1 < NEXP else None))
            gfree[gs] = self._tmp_free[(self._tmp_i - 1) % 2]
        ph.emit()

    def build(self):
        cfg = self.cfg
        self.declare()
        with ExitStack() as es:
            self.alloc_persist(es)
            self._rt_free = [None, None]
            self._rs2_free = [None, None]
            self.ph_setup()
            for seq in range(cfg.get("nseq", NSEQ)):
                for st in range(cfg.get("nst", SEQ // ST)):
                    self._rt_free = [None, None]
                    self._rs2_free = [None, None]
                    self.ph_load(seq, st)
                    if cfg.get("mixa", True):
                        self.ph_mixer_a()
                    if cfg.get("ffn", True):
                        self.ph_ffn()
                    if cfg.get("kv", True):
                        self.ph_kv(st)
                    if cfg.get("mixb", True):
                        self._rt_free = [None, None]
                        self._rs2_free = [None, None]
                        self.ph_mixer_b(st)
                    if cfg.get("moe", True):
                        self.ph_moe()
                    self.ph_store(seq, st)
        return self.nc


INPUT_NAMES = ["x", "a_norm_g", "a_w_in", "a_v_norm_g", "a_w_spatial", "a_b_spatial", "a_w_out",
               "f_norm_g", "f_w_in", "f_w_out", "kv_norm_g", "kv_w", "kv_b_f", "k_norm_g",
               "b_norm_g", "b_w_in", "q_norm_g", "b_w_out", "m_norm_g", "m_w_router", "m_w_in", "m_w_out"]


def kernel(**inputs):
    n = 8
    k = Kern({})
    nc = k.build()
    shared = {nm: np.ascontiguousarray(np.asarray(inputs[nm], dtype=np.float32)) for nm in INPUT_NAMES if nm != "x"}
    x = np.asarray(inputs["x"], dtype=np.float32)
    in_maps = []
    for i in range(n):
        m = dict(shared)
        m["x"] = np.ascontiguousarray(x[i * NSEQ:(i + 1) * NSEQ])
        in_maps.append(m)
    res = run_bass_kernel_spmd(nc, in_maps, core_ids=list(range(n)))
    return np.concatenate([r["out"] for r in res.results], axis=0)
```
